# Optimizing a Trainium2 kernel written in Bass

```python
import math
import jax, jax.numpy as jnp
from jax import lax
import numpy as np

D_MODEL = 1024
BATCH = 8
SEQ = 8192
DEPTH = 1

GM_GROUPS = 4
GM_DG = 128
GM_WIDTH = GM_GROUPS * GM_DG
CHUNK = 128
DA_HEADS = 4
DA_DH = 64
DA_VDIM = 2 * DA_DH
DA_QK = DA_HEADS * 2 * DA_DH
DA_WIDTH = DA_HEADS * DA_VDIM
ROT_DIM = DA_DH // 4
ROPE_THETA = 500000.0
Q_BLOCK = 128
N_BRANCH = 2
SPLIT_SIZES = (GM_WIDTH, GM_WIDTH, DA_QK, DA_QK, DA_WIDTH, N_BRANCH * D_MODEL)
IN_COLS = sum(SPLIT_SIZES)
SPLIT_IDX = tuple(int(i) for i in np.cumsum(SPLIT_SIZES)[:-1])
N_GROUPS = 4
EXPERTS_PER_GROUP = 8
N_EXPERTS = N_GROUPS * EXPERTS_PER_GROUP
TOP_K = 2
D_EXPERT = 512
MOE_BLOCK = 128
EPS = 1e-6
POS_OFFSET_MAX = 4096

kernel_name = "hybrid_gmlp_diffattn_hmoe"


def lambda_init(layer):
    return 0.8 - 0.6 * math.exp(-0.3 * layer)


def rmsnorm(x, g):
    xf = x.astype(jnp.float32)
    var = jnp.mean(xf * xf, axis=-1, keepdims=True)
    return (xf * lax.rsqrt(var + EPS) * g.astype(jnp.float32)).astype(x.dtype)


def layernorm(x, g, b):
    xf = x.astype(jnp.float32)
    mu = jnp.mean(xf, axis=-1, keepdims=True)
    xc = xf - mu
    var = jnp.mean(xc * xc, axis=-1, keepdims=True)
    return (xc * lax.rsqrt(var + EPS) * g.astype(jnp.float32) + b.astype(jnp.float32)).astype(x.dtype)


def partial_rope(x, pos):
    half = ROT_DIM // 2
    inv_freq = ROPE_THETA ** (-jnp.arange(half, dtype=jnp.float32) / half)
    ang = pos.astype(jnp.float32)[..., None] * inv_freq
    cos = jnp.cos(ang)[:, :, None, None, :]
    sin = jnp.sin(ang)[:, :, None, None, :]
    xr = x[..., :ROT_DIM].astype(jnp.float32)
    x1, x2 = xr[..., :half], xr[..., half:]
    rot = jnp.concatenate([x1 * cos - x2 * sin, x2 * cos + x1 * sin], axis=-1).astype(x.dtype)
    return jnp.concatenate([rot, x[..., ROT_DIM:]], axis=-1)


def gmlp_mixer(u, v, ln_g, ln_b, w_s, b_s):
    B, S, _ = u.shape
    nc = S // CHUNK
    u = u.reshape(B, nc, CHUNK, GM_GROUPS, GM_DG)
    v = layernorm(v.reshape(B, nc, CHUNK, GM_GROUPS, GM_DG), ln_g, ln_b)
    causal = jnp.tril(jnp.ones((CHUNK, CHUNK), dtype=bool))
    w = jnp.where(causal[None], w_s, 0.0).astype(v.dtype)
    sv = jnp.einsum('gts,bnsgd->bntgd', w, v) + b_s.T[:, :, None].astype(v.dtype)
    return (u * sv).reshape(B, S, GM_WIDTH)


def diff_attention(q, k, v, pos, lam_q1, lam_k1, lam_q2, lam_k2, subln_g, lam_init):
    B, S = q.shape[0], q.shape[1]
    q = partial_rope(q, pos)
    k = partial_rope(k, pos)
    lam = (jnp.exp(jnp.sum(lam_q1.astype(jnp.float32) * lam_k1.astype(jnp.float32)))
           - jnp.exp(jnp.sum(lam_q2.astype(jnp.float32) * lam_k2.astype(jnp.float32)))
           + lam_init)
    scale = DA_DH ** -0.5
    nb = S // Q_BLOCK
    qb = q.reshape(B, nb, Q_BLOCK, DA_HEADS, 2, DA_DH).transpose(1, 0, 2, 3, 4, 5)
    key_idx = jnp.arange(S)

    def block(args):
        qi, bi = args
        s = jnp.einsum('bqhmd,bkhmd->bhmqk', qi, k).astype(jnp.float32) * scale
        q_idx = bi * Q_BLOCK + jnp.arange(Q_BLOCK)
        mask = key_idx[None, :] <= q_idx[:, None]
        p = jax.nn.softmax(jnp.where(mask, s, -jnp.inf), axis=-1)
        a = p[:, :, 0] - lam * p[:, :, 1]
        return jnp.einsum('bhqk,bkhd->bqhd', a.astype(v.dtype), v)

    o = lax.map(block, (qb, jnp.arange(nb)))
    o = o.transpose(1, 0, 2, 3, 4).reshape(B, S, DA_HEADS, DA_VDIM)
    o = rmsnorm(o, subln_g) * (1.0 - lam_init)
    return o.reshape(B, S, DA_WIDTH)


def hier_moe(h, w_rg, b_rg, w_re, b_re, w1, w3, w2):
    B, S, D = h.shape
    T = B * S
    A = T * TOP_K
    hf = h.reshape(T, D)
    g_logits = (hf @ w_rg).astype(jnp.float32) + b_rg.astype(jnp.float32)
    g_prob = jax.nn.softmax(g_logits, axis=-1)
    grp = jnp.argmax(g_logits, axis=-1)
    g_w = jnp.take_along_axis(g_prob, grp[:, None], axis=1)
    e_logits = ((hf @ w_re).astype(jnp.float32) + b_re.astype(jnp.float32)).reshape(T, N_GROUPS, EXPERTS_PER_GROUP)
    e_logits = jnp.take_along_axis(e_logits, grp[:, None, None], axis=1)[:, 0]
    top_v, top_i = lax.top_k(e_logits, TOP_K)
    e_w = jax.nn.softmax(top_v, axis=-1) * g_w
    eid = (grp[:, None] * EXPERTS_PER_GROUP + top_i).reshape(A).astype(jnp.int32)
    wts = e_w.reshape(A)
    tok = jnp.arange(A, dtype=jnp.int32) // TOP_K
    order = jnp.argsort(eid)
    s_eid, s_tok, s_w = eid[order], tok[order], wts[order]
    counts = jax.ops.segment_sum(jnp.ones((A,), jnp.int32), eid, num_segments=N_EXPERTS)
    starts = jnp.cumsum(counts) - counts
    p_counts = (counts + MOE_BLOCK - 1) // MOE_BLOCK * MOE_BLOCK
    p_ends = jnp.cumsum(p_counts)
    p_starts = p_ends - p_counts
    dest = p_starts[s_eid] + (jnp.arange(A, dtype=jnp.int32) - starts[s_eid])
    P = (A + N_EXPERTS * (MOE_BLOCK - 1) + MOE_BLOCK - 1) // MOE_BLOCK * MOE_BLOCK
    nblk = P // MOE_BLOCK
    buf_tok = jnp.zeros((P,), jnp.int32).at[dest].set(s_tok)
    buf_w = jnp.zeros((P,), jnp.float32).at[dest].set(s_w)
    blk_e = jnp.clip(jnp.searchsorted(p_ends, jnp.arange(nblk, dtype=jnp.int32) * MOE_BLOCK, side='right'),
                     0, N_EXPERTS - 1)
    xs = hf[buf_tok].reshape(nblk, MOE_BLOCK, D)

    def expert_block(args):
        xb, e = args
        return (jax.nn.silu(xb @ w1[e]) * (xb @ w3[e])) @ w2[e]

    ys = lax.map(expert_block, (xs, blk_e)).reshape(P, D)
    ys = ys * buf_w[:, None].astype(ys.dtype)
    return jax.ops.segment_sum(ys, buf_tok, num_segments=T).reshape(B, S, D)


def setup_inputs(seed: int = 0) -> dict:
    key = jax.random.key(seed)
    ks = jax.random.split(key, 32)
    f32 = jnp.float32
    nrm = lambda k, shape, s: jax.random.normal(k, shape, f32) * s
    L, D = DEPTH, D_MODEL
    offset = jax.random.randint(ks[1], (BATCH, 1), 0, POS_OFFSET_MAX, dtype=jnp.int32)
    positions = offset + jnp.arange(SEQ, dtype=jnp.int32)[None, :]
    return {
        "x": nrm(ks[0], (BATCH, SEQ, D), 1.0),
        "positions": positions,
        "norm_mix_g": 1.0 + nrm(ks[2], (L, D), 0.02),
        "w_in": nrm(ks[3], (L, D, IN_COLS), D ** -0.5),
        "gm_ln_g": 1.0 + nrm(ks[4], (L, GM_GROUPS, GM_DG), 0.02),
        "gm_ln_b": nrm(ks[5], (L, GM_GROUPS, GM_DG), 0.02),
        "gm_w_s": nrm(ks[6], (L, GM_GROUPS, CHUNK, CHUNK), 0.5 * CHUNK ** -0.5),
        "gm_b_s": 1.0 + nrm(ks[7], (L, GM_GROUPS, CHUNK), 0.02),
        "lam_q1": nrm(ks[8], (L, DA_DH), 0.1),
        "lam_k1": nrm(ks[9], (L, DA_DH), 0.1),
        "lam_q2": nrm(ks[10], (L, DA_DH), 0.1),
        "lam_k2": nrm(ks[11], (L, DA_DH), 0.1),
        "da_subln_g": 1.0 + nrm(ks[12], (L, DA_VDIM), 0.02),
        "w_br_a": nrm(ks[13], (L, GM_WIDTH, D), GM_WIDTH ** -0.5),
        "w_br_b": nrm(ks[14], (L, DA_WIDTH, D), DA_WIDTH ** -0.5),
        "w_out": nrm(ks[15], (L, D, D), D ** -0.5),
        "norm_ffn_g": 1.0 + nrm(ks[16], (L, D), 0.02),
        "w_router_group": nrm(ks[17], (L, D, N_GROUPS), D ** -0.5),
        "b_router_group": nrm(ks[18], (L, N_GROUPS), 0.01),
        "w_router_expert": nrm(ks[19], (L, D, N_EXPERTS), D ** -0.5),
        "b_router_expert": nrm(ks[20], (L, N_EXPERTS), 0.01),
        "w_exp_gate": nrm(ks[21], (L, N_EXPERTS, D, D_EXPERT), D ** -0.5),
        "w_exp_up": nrm(ks[22], (L, N_EXPERTS, D, D_EXPERT), D ** -0.5),
        "w_exp_down": nrm(ks[23], (L, N_EXPERTS, D_EXPERT, D), D_EXPERT ** -0.5),
        "final_norm_g": 1.0 + nrm(ks[24], (D,), 0.02),
    }


def reference(x, positions, norm_mix_g, w_in, gm_ln_g, gm_ln_b, gm_w_s, gm_b_s,
              lam_q1, lam_k1, lam_q2, lam_k2, da_subln_g, w_br_a, w_br_b, w_out,
              norm_ffn_g, w_router_group, b_router_group, w_router_expert, b_router_expert,
              w_exp_gate, w_exp_up, w_exp_down, final_norm_g):
    B, S, D = x.shape
    for l in range(DEPTH):
        h = rmsnorm(x, norm_mix_g[l])
        z = h @ w_in[l]
        z_u, z_v, z_q, z_k, z_va, z_g = jnp.split(z, SPLIT_IDX, axis=-1)
        y_a = gmlp_mixer(jax.nn.gelu(z_u), jax.nn.gelu(z_v), gm_ln_g[l], gm_ln_b[l], gm_w_s[l], gm_b_s[l])
        q = z_q.reshape(B, S, DA_HEADS, 2, DA_DH)
        k = z_k.reshape(B, S, DA_HEADS, 2, DA_DH)
        va = z_va.reshape(B, S, DA_HEADS, DA_VDIM)
        y_b = diff_attention(q, k, va, positions, lam_q1[l], lam_k1[l], lam_q2[l], lam_k2[l],
                             da_subln_g[l], lambda_init(l))
        gates = jax.nn.sigmoid(z_g.astype(jnp.float32)).astype(x.dtype).reshape(B, S, N_BRANCH, D)
        merged = gates[:, :, 0] * (y_a @ w_br_a[l]) + gates[:, :, 1] * (y_b @ w_br_b[l])
        x = x + merged @ w_out[l]
        h2 = rmsnorm(x, norm_ffn_g[l])
        x = x + hier_moe(h2, w_router_group[l], b_router_group[l], w_router_expert[l], b_router_expert[l],
                         w_exp_gate[l], w_exp_up[l], w_exp_down[l])
    return rmsnorm(x, final_norm_g)
```

```python
import math
from contextlib import ExitStack

import numpy as np
import concourse.bass as bass
import concourse.mybir as mybir
from concourse.bass_utils import run_bass_kernel_spmd

F32 = mybir.dt.float32
BF16 = mybir.dt.bfloat16
I32 = mybir.dt.int32
U32 = mybir.dt.uint32
ALU = mybir.AluOpType
AF = mybir.ActivationFunctionType
AX = mybir.AxisListType

ENGS = ("pe", "act", "dve", "pool", "sp")
import os as _os
NOSELF = tuple(x for x in _os.environ.get('KNOSELF', '').split(',') if x)


class _Op:
    __slots__ = ("eng", "fn", "deps", "semkey", "sigval", "needs_sig", "is_dma", "idx")

    def __init__(self, eng, fn, semkey, is_dma):
        self.eng = eng
        self.fn = fn
        self.deps = {}
        self.semkey = semkey
        self.sigval = None
        self.needs_sig = is_dma
        self.is_dma = is_dma


class Sched:
    def __init__(self, nc, stack):
        self.nc = nc
        self.stack = stack
        self.ops = {e: [] for e in ENGS}
        self.lastw = {}
        self.readers = {}
        self.sems = {}
        self.semcount = {}
        self.all_ops = []
        self.keep_prefix = None
        self._uid = 0

    def uid(self):
        self._uid += 1
        return self._uid

    def _sem(self, key):
        if key not in self.sems:
            name = "s_" + str(key).replace(" ", "").replace("(", "").replace(")", "").replace(",", "_").replace("'", "")
            self.sems[key] = self.stack.enter_context(self.nc.semaphore(name[:40]))
            self.semcount[key] = 0
        return self.sems[key]

    def add(self, eng, fn, reads=(), writes=(), dma=None):
        is_dma = dma is not None
        semkey = ("dma", dma) if is_dma else ("eng", eng)
        op = _Op(eng, fn, semkey, is_dma)
        self._sem(semkey)
        deps = {}

        def dep_on(o):
            if o is None or o is op:
                return
            if (not o.is_dma) and o.eng == "pe" and eng == "pe" and not is_dma:
                return
            if NOSELF and (not o.is_dma) and (not is_dma) and o.eng == eng and eng in NOSELF:
                return
            cur = deps.get(o.semkey)
            if cur is None or cur.idx < o.idx:
                deps[o.semkey] = o

        for r in reads:
            dep_on(self.lastw.get(r))
        for r in writes:
            dep_on(self.lastw.get(r))
            for o in self.readers.get(r, {}).values():
                dep_on(o)
        op.idx = len(self.all_ops)
        self.all_ops.append(op)
        for r in reads:
            self.readers.setdefault(r, {})[semkey] = op
        for r in writes:
            self.lastw[r] = op
            self.readers[r] = {}
        for o in deps.values():
            o.needs_sig = True
        op.deps = deps
        self.ops[eng].append(op)
        return op

    def barrier(self):
        tok = ("__barrier__",)
        last = []
        for e in ENGS:
            for o in reversed(self.ops[e]):
                if o.fn is not None:
                    last.append(o)
                    break
        lastdma = {}
        for o in self.all_ops:
            if o.is_dma:
                lastdma[o.semkey] = o
        keep_res = {r: o for r, o in self.lastw.items() if r.startswith(self.keep_prefix)} if self.keep_prefix else {}
        excl = {o.semkey for o in keep_res.values()}
        every = {o.semkey: o for o in last if not o.is_dma}
        every.update({k: o for k, o in lastdma.items() if k not in excl})
        for e in ENGS:
            op = _Op(e, None, ("eng", e), False)
            op.idx = len(self.all_ops)
            self.all_ops.append(op)
            op.deps = {k: o for k, o in every.items() if not (k == ("eng", "pe") and e == "pe")}
            for o in op.deps.values():
                o.needs_sig = True
            self.ops[e].append(op)
        self.lastw = dict(keep_res)
        self.readers = {}

    def finalize_and_emit(self, final_waits=()):
        nc = self.nc
        for o in self.all_ops:
            if o.fn is None:
                continue
            if o.needs_sig:
                inc = 16 if o.is_dma else 1
                self.semcount[o.semkey] += inc
                o.sigval = self.semcount[o.semkey]
        engmap = {"pe": "tensor", "act": "scalar", "dve": "vector", "pool": "gpsimd", "sp": "sync"}
        final = [(self.sems[o.semkey], o.sigval) for o in final_waits]

        def run(engname, eng):
            known = {}
            for o in self.ops[engname]:
                for k, d in o.deps.items():
                    v = d.sigval
                    assert v is not None, (k, d.eng)
                    if known.get(k, 0) >= v:
                        continue
                    known[k] = v
                    eng.wait_ge(self.sems[k], v)
                if o.fn is None:
                    continue
                inst = o.fn(eng)
                if o.needs_sig:
                    assert inst is not None
                    inst.then_inc(self.sems[o.semkey], 16 if o.is_dma else 1)
            if engname == "sp":
                for s, v in final:
                    eng.wait_ge(s, v)

        with nc.Block() as block:
            for engname in ENGS:
                getattr(block, engmap[engname])(lambda eng, _n=engname: run(_n, eng))


D = 1024
T = 8192
NT = T // 128
NC_IN = 2560
NEXP = 32
CAP = 1024
NBLK = CAP // 128
NSLOT = NEXP * CAP
EPS = 1e-6
LAM_INIT = 0.8 - 0.6 * math.exp(0.0)
TWO_PI = 2.0 * math.pi
C1 = 6.28125
C2 = TWO_PI - C1


INPUT_NAMES = []
import os
STAGES = os.environ.get('KSTAGES', '123')


class Arena:
    def __init__(self, nc, st, nbytes):
        self.t = st.enter_context(nc.sbuf_tensor("arena", [128, nbytes // 4], F32))
        self.off = 0
        self.cap = nbytes

    def _take(self, nbytes):
        nbytes = (nbytes + 31) // 32 * 32
        o = self.off
        self.off += nbytes
        assert self.off <= self.cap, ("SBUF arena overflow", self.off, self.cap)
        return o

    def f32(self, n):
        o = self._take(n * 4)
        return self.t[:, o // 4:o // 4 + n]

    def i32(self, n):
        return self.f32(n).bitcast(I32)

    def bf16(self, n):
        n2 = (n + 1) // 2 * 2
        o = self._take(n2 * 2)
        return self.t[:, o // 4:o // 4 + n2 // 2].bitcast(BF16)[:, 0:n]

    def mark(self):
        return self.off

    def release(self, m):
        self.off = m


def build_program(debug=False, stop_after=None, nt_a=NT, nt_b=NT, nt_c=NT, n_exp=NEXP):
    nc = bass.Bass("TRN2", target_bir_lowering=False)
    okind = "ExternalOutput" if debug else "Internal"

    early = stop_after in ("setup", "win", "A", "B", "C")
    INPUT_NAMES.clear()

    def din(name, shape, dt=F32):
        if early and name in ("w1", "w3", "w2"):
            return None
        INPUT_NAMES.append(name)
        return nc.dram_tensor(name, list(shape), dt, kind="ExternalInput").ap()

    def dscr(name, shape, dt):
        return nc.dram_tensor(name, list(shape), dt, kind=okind).ap()

    x_d = din("x", [T, D])
    pos_d = din("pos", [128, NT], I32)
    invf_d = din("invf", [128, 8])
    gmix_d = din("g_mix", [128, 8])
    win_d = din("w_in", [D, 4608])
    lng_d = din("lng_bc", [128, 512])
    lnb_d = din("lnb_bc", [128, 512])
    wsT_d = din("wsT", [128, 4, 128])
    bs_d = din("bs", [128, 4])
    lamv_d = din("lamv", [128, 4, 64])
    subgc_d = din("subg_col", [128, 1])
    wa_d = din("w_br_a", [512, D])
    wb_d = din("w_br_b", [512, D])
    wo_d = din("w_out", [D, D])
    gffn_d = din("g_ffn_bc", [128, D])
    wr_d = din("w_r", [D, 36])
    br_d = din("b_r", [128, 36])
    w1_d = din("w1", [NEXP, D, 512])
    w3_d = din("w3", [NEXP, D, 512])
    w2_d = din("w2", [NEXP, 512, D])
    fg_d = din("fg_bc", [128, D])
    out_d = nc.dram_tensor("out", [T, D], F32, kind="ExternalOutput").ap()

    qT_d = dscr("qT_s", [128, 4, T], BF16)
    kT_d = dscr("kT_s", [128, 4, T], BF16)
    v_d = dscr("v_s", [128, NT, 4, 130], BF16)
    yaT_d = dscr("yaT_s", [NT, 128, 512], BF16)
    ybT_d = dscr("ybT_s", [NT, 128, 512], BF16)
    x1_d = dscr("x1_s", [T, D], F32)
    xs_d = dscr("xs_s", [NSLOT, D], BF16)
    ys_d = dscr("ys_s", [NSLOT, D], BF16)
    rt_d = dscr("rt_s", [128, NT, 4], F32) if debug else None

    with ExitStack() as st:
        S = Sched(nc, st)
        A = Arena(nc, st, 192 * 1024)
        ps = st.enter_context(nc.psum_tensor("ps", [128, 8, 512], F32))

        def bank(b):
            return ps[:, b, :]

        def finish():
            lastd = {}
            for o in S.all_ops:
                if o.is_dma:
                    lastd[o.semkey] = o
            S.finalize_and_emit(final_waits=list(lastd.values()))
            return nc

        def bankbf(b):
            return ps[:, b, :].bitcast(BF16)

        dve = lambda fn, r=(), w=(): S.add("dve", fn, r, w)
        act = lambda fn, r=(), w=(): S.add("act", fn, r, w)
        pool = lambda fn, r=(), w=(): S.add("pool", fn, r, w)
        pe = lambda fn, r=(), w=(): S.add("pe", fn, r, w)

        def dma(q, out, in_, r=(), w=(), key=None):
            return S.add(q, lambda e: e.dma_start(out=out, in_=in_), r, w, dma=key)

        ident = A.bf16(128)
        identf = A.f32(128)
        ustrict = A.bf16(128)
        onesm = A.bf16(128)
        maskb = A.bf16(128)
        ctmp = A.f32(128)
        nhalf = A.f32(8)
        pool(lambda e: e.memset(nhalf, -0.5), w=["nhalf"])
        pool(lambda e: e.memset(identf, 0.0), w=["identf"])
        pool(lambda e: e.affine_select(identf, identf, [[-1, 128]], ALU.not_equal, 1.0, base=0,
                                       channel_multiplier=1), r=["identf"], w=["identf"])
        dve(lambda e: e.tensor_copy(ident, identf), r=["identf"], w=["ident"])
        pool(lambda e: e.memset(ctmp, 1.0), w=["ctmp"])
        pool(lambda e: e.affine_select(ctmp, ctmp, [[1, 128]], ALU.is_gt, 0.0, base=0,
                                       channel_multiplier=-1), r=["ctmp"], w=["ctmp"])
        dve(lambda e: e.tensor_copy(ustrict, ctmp), r=["ctmp"], w=["ustrict"])
        pool(lambda e: e.memset(ctmp, 0.0), r=["ctmp"], w=["ctmp"])
        pool(lambda e: e.affine_select(ctmp, ctmp, [[1, 128]], ALU.is_ge, -30000.0, base=0,
                                       channel_multiplier=-1), r=["ctmp"], w=["ctmp"])
        dve(lambda e: e.tensor_copy(maskb, ctmp), r=["ctmp"], w=["maskb"])
        dve(lambda e: e.memset(onesm, 1.0), w=["onesm"])

        slots_i = A.i32(NT * 2)
        wts = A.f32(NT * 2)
        slots3 = slots_i.rearrange("p (t k) -> p t k", k=2)
        wts3 = wts.rearrange("p (t k) -> p t k", k=2)
        tokid = A.i32(NT)
        pool(lambda e: e.iota(tokid, [[128, NT]], base=0, channel_multiplier=1), w=["tokid"])
        eoff_i = A.i32(NEXP)
        eoff = A.f32(NEXP)
        pool(lambda e: e.iota(eoff_i, [[CAP, NEXP]], base=0, channel_multiplier=0), w=["eoff_i"])
        dve(lambda e: e.tensor_copy(eoff, eoff_i), r=["eoff_i"], w=["eoff"])
        base_cnt = A.f32(NEXP)
        dve(lambda e: e.memset(base_cnt, 0.0), w=["base_cnt"])
        S.keep_prefix = "xs_zero_"
        ztile = A.bf16(4 * D)
        dve(lambda e: e.memset(ztile, 0.0), w=["ztile"])
        for z_ in range(NSLOT // 512):
            dma("act", xs_d[z_ * 512:(z_ + 1) * 512, :].rearrange("(n p) d -> p n d", p=128),
                ztile.rearrange("p (n d) -> p n d", d=D), r=["ztile"], w=["xs_zero_" + str(z_ % 4)], key=f"zfill{z_ % 4}")

        lamv = A.f32(256)
        lamp = A.f32(128)
        lsum = A.f32(2)
        lam_e = A.f32(2)
        neglam = A.f32(1)
        dma("sp", lamv, lamv_d.rearrange("p a b -> p (a b)"), w=["lamv"], key="lamv")
        lamv3 = lamv.rearrange("p (a b) -> p a b", b=64)
        dve(lambda e: e.tensor_tensor(lamp[:, 0:64], lamv3[:, 0, :], lamv3[:, 1, :], ALU.mult), r=["lamv"], w=["lamp"])
        dve(lambda e: e.tensor_tensor(lamp[:, 64:128], lamv3[:, 2, :], lamv3[:, 3, :], ALU.mult), r=["lamp", "lamv"], w=["lamp"])
        dve(lambda e: e.reduce_sum(lsum, lamp.rearrange("p (a b) -> p a b", b=64), axis=AX.X), r=["lamp"], w=["lsum"])
        act(lambda e: e.activation(lam_e, lsum, AF.Exp), r=["lsum"], w=["lam_e"])
        dve(lambda e: e.scalar_tensor_tensor(neglam, lam_e[:, 1:2], -LAM_INIT, lam_e[:, 0:1], ALU.add, ALU.subtract),
            r=["lam_e"], w=["neglam"])

        cos_t = A.f32(NT * 8)
        sin_t = A.f32(NT * 8)
        m0 = A.mark()
        pos_i = A.i32(NT)
        posf = A.f32(NT)
        invf = A.f32(8)
        ang = A.f32(NT * 8)
        kf = A.f32(NT * 8)
        ki = A.i32(NT * 8)
        rr = A.f32(NT * 8)
        r2 = A.f32(NT * 8)
        msk = A.f32(NT * 8)
        dma("sp", pos_i, pos_d, w=["pos_i"], key="pos_i")
        dma("sp", invf, invf_d, w=["invf"], key="invf")
        dve(lambda e: e.tensor_copy(posf, pos_i), r=["pos_i"], w=["posf"])
        ang3 = ang.rearrange("p (t j) -> p t j", j=8)
        dve(lambda e: e.tensor_tensor(ang3, posf.unsqueeze(2).broadcast_to([128, NT, 8]),
                                      invf.unsqueeze(1).broadcast_to([128, NT, 8]), ALU.mult),
            r=["posf", "invf"], w=["ang"])
        dve(lambda e: e.tensor_scalar(kf, ang, 1.0 / TWO_PI, None, ALU.mult), r=["ang"], w=["kf"])
        dve(lambda e: e.tensor_copy(ki, kf), r=["kf"], w=["ki"])
        dve(lambda e: e.tensor_copy(kf, ki), r=["ki"], w=["kf"])
        dve(lambda e: e.scalar_tensor_tensor(rr, kf, -C1, ang, ALU.mult, ALU.add), r=["kf", "ang"], w=["rr"])
        dve(lambda e: e.scalar_tensor_tensor(rr, kf, -C2, rr, ALU.mult, ALU.add), r=["kf", "rr"], w=["rr"])
        dve(lambda e: e.tensor_scalar(r2, rr, math.pi / 2, None, ALU.add), r=["rr"], w=["r2"])
        dve(lambda e: e.tensor_scalar(msk, r2, math.pi, None, ALU.is_gt), r=["r2"], w=["msk"])
        dve(lambda e: e.scalar_tensor_tensor(r2, msk, -TWO_PI, r2, ALU.mult, ALU.add), r=["msk", "r2"], w=["r2"])
        PI_SAFE = 3.1415925
        dve(lambda e: e.tensor_scalar(rr, rr, PI_SAFE, -PI_SAFE, ALU.min, ALU.max), r=["rr"], w=["rr"])
        dve(lambda e: e.tensor_scalar(r2, r2, PI_SAFE, -PI_SAFE, ALU.min, ALU.max), r=["r2"], w=["r2"])
        act(lambda e: e.activation(sin_t, rr, AF.Sin), r=["rr"], w=["sin_t"])
        act(lambda e: e.activation(cos_t, r2, AF.Sin), r=["r2"], w=["cos_t"])
        if debug:
            dbg_cs = nc.dram_tensor("dbg_cs", [128, 2, NT * 8], F32, kind="ExternalOutput").ap()
            dma("sp", dbg_cs[:, 0, :], cos_t, r=["cos_t"], key="dbgc")
            dma("sp", dbg_cs[:, 1, :], sin_t, r=["sin_t"], key="dbgs")
        S.barrier()
        A.release(m0)
        if stop_after == "setup":
            return finish()
        cos3 = cos_t.rearrange("p (t j) -> p t j", j=8)
        sin3 = sin_t.rearrange("p (t j) -> p t j", j=8)
        mark_phase = A.mark()

        def emit_rstd(ss, vtmp, rstd, n, scale, rname):
            dve(lambda e: e.tensor_scalar(vtmp, ss, scale, EPS, ALU.mult, ALU.add), r=[rname + "ss"], w=[rname + "v"])
            pool(lambda e: e.tensor_tensor(rstd, vtmp, nhalf[:, 0:n], ALU.pow), r=[rname + "v", "nhalf"], w=[rname + "rstd"])

        win = A.bf16(8 * NC_IN)
        win3 = win.rearrange("p (c n) -> p c n", n=NC_IN)
        gmix = A.f32(8)
        dma("sp", gmix, gmix_d, w=["gmix"], key="gmix")
        mA = A.mark()
        wst = [A.f32(NC_IN), A.f32(NC_IN)]
        for c in range(8):
            sl = c % 2
            dma("sp", wst[sl], win_d[c * 128:(c + 1) * 128, 0:NC_IN], w=[f"wst{sl}"], key=f"wst{sl}")
            if c % 2 == 0:
                dve(lambda e, c=c, sl=sl: e.tensor_scalar(win3[:, c, :], wst[sl], gmix[:, c:c + 1], None, ALU.mult),
                    r=[f"wst{sl}", "gmix"], w=[f"win{c}"])
            else:
                act(lambda e, c=c, sl=sl: e.activation(win3[:, c, :], wst[sl], AF.Copy, scale=gmix[:, c:c + 1]),
                    r=[f"wst{sl}", "gmix"], w=[f"win{c}"])
        S.barrier()
        A.release(mA)
        if stop_after == "win":
            dbg_w = nc.dram_tensor("dbg_w", [128, 8 * NC_IN], BF16, kind="ExternalOutput").ap()
            dma("sp", dbg_w, win, r=[f"win{c}" for c in range(8)], key="dbgw")
            return finish()
        winres = [f"win{c}" for c in range(8)]

        lng = A.f32(512)
        lnb = A.f32(512)
        wsTf = A.f32(512)
        wsT = A.bf16(512)
        bs = A.f32(4)
        dma("sp", lng, lng_d, w=["lng"], key="lng")
        dma("sp", lnb, lnb_d, w=["lnb"], key="lnb")
        dma("sp", wsTf, wsT_d.rearrange("p g t -> p (g t)"), w=["wsTf"], key="wsTf")
        dma("sp", bs, bs_d, w=["bs"], key="bs")
        pool(lambda e: e.affine_select(wsTf, wsTf, [[0, 4], [1, 128]], ALU.is_ge, 0.0, base=0,
                                       channel_multiplier=-1), r=["wsTf"], w=["wsTf"])
        dve(lambda e: e.tensor_copy(wsT, wsTf), r=["wsTf"], w=["wsT"])
        wsT3 = wsT.rearrange("p (g t) -> p g t", t=128)

        NXS = 3
        xt = [A.f32(D) for _ in range(NXS)]
        junk = A.bf16(D)
        ssA = [A.f32(1) for _ in range(2)]
        vA = [A.f32(1) for _ in range(2)]
        rsA = [A.f32(1) for _ in range(2)]
        hb = [A.bf16(D) for _ in range(2)]
        hT = [A.bf16(D) for _ in range(2)]
        x2b = [A.f32(512) for _ in range(2)]
        xhb = [A.f32(512) for _ in range(2)]
        gu = [A.f32(512) for _ in range(2)]
        gv = [A.f32(512) for _ in range(2)]
        sq = A.f32(512)
        lst = [A.f32(16) for _ in range(2)]
        vn = A.f32(512)
        vnb = [A.bf16(512) for _ in range(2)]
        qb = [A.bf16(512) for _ in range(3)]
        kb = [A.bf16(512) for _ in range(3)]
        rt = [A.f32(64 * 4) for _ in range(2)]
        vb = [A.bf16(4 * 130) for _ in range(2)]
        yab = [A.bf16(512) for _ in range(2)]
        yaT = [A.bf16(512) for _ in range(2)]
        qT = [A.bf16(512) for _ in range(2)]
        kT = [A.bf16(512) for _ in range(2)]
        for s_ in range(2):
            dve(lambda e, s_=s_: e.memset(vb[s_], 1.0), w=[f"vb{s_}"])

        def stageA0(i):
            xs = i % NXS
            s2 = i % 2
            KA1 = int(os.environ.get("KA1", "9"))
            dma("sp", xt[xs], x_d[i * 128:(i + 1) * 128, :], w=[f"xt{xs}"], key=f"xt{xs}")
            if KA1 < 2: return
            act(lambda e: e.activation(junk, xt[xs], AF.Square, accum_out=ssA[s2]), r=[f"xt{xs}"], w=["junk", f"Ass{s2}"])
            if KA1 < 3: return
            dve(lambda e: e.tensor_scalar(vA[s2], ssA[s2], 1.0 / D, EPS, ALU.mult, ALU.add), r=[f"Ass{s2}"], w=[f"Av{s2}"])
            if KA1 < 4: return
            pool(lambda e: e.tensor_tensor(rsA[s2], vA[s2], nhalf[:, 0:1], ALU.pow), r=[f"Av{s2}", "nhalf"], w=[f"Ars{s2}"])
            if KA1 < 5: return
            dve(lambda e: e.tensor_scalar(hb[s2], xt[xs], rsA[s2], None, ALU.mult), r=[f"xt{xs}", f"Ars{s2}"], w=[f"hb{s2}"])
            if KA1 < 6: return

        def stageA1(i):
            s2 = i % 2
            KA1 = 9

            def tr(e):
                last = None
                for c in range(8):
                    last = e.transpose(bankbf(0)[:, c * 128:(c + 1) * 128], hb[s2][:, c * 128:(c + 1) * 128], ident)
                return last
            pe(tr, r=[f"hb{s2}", "ident"], w=["ps0"])
            if KA1 < 7: return
            act(lambda e: e.copy(hT[s2], bankbf(0)), r=["ps0"], w=[f"hT{s2}"])

        def zmm(i, cg, bk):
            s2 = i % 2
            hT3 = hT[s2].rearrange("p (c t) -> p c t", t=128)

            def mm(e):
                last = None
                for c in range(8):
                    last = e.matmul(bank(bk), hT3[:, c, :], win3[:, c, cg * 512:(cg + 1) * 512],
                                    start=(c == 0), stop=(c == 7))
                return last
            pe(mm, r=[f"hT{s2}"] + winres, w=[f"ps{bk}"])

        def gelu_chain(i, which, bk, outbuf, oname):
            x2 = x2b[which]
            xh = xhb[which]
            n2, nh = f"x2_{which}", f"xh_{which}"
            act(lambda e: e.activation(x2, bank(bk), AF.Square), r=[f"ps{bk}"], w=[n2])
            act(lambda e: e.activation(xh, bank(bk), AF.Copy, scale=0.5), r=[f"ps{bk}"], w=[nh])
            dve(lambda e: e.tensor_scalar(x2, x2, 0.044715, 1.0, ALU.mult, ALU.add), r=[n2], w=[n2])
            dve(lambda e: e.tensor_tensor(x2, x2, xh, ALU.mult), r=[n2, nh], w=[n2])
            act(lambda e: e.activation(x2, x2, AF.Tanh, scale=2.0 * 0.7978845608028654), r=[n2], w=[n2])
            dve(lambda e: e.scalar_tensor_tensor(outbuf, x2, 1.0, xh, ALU.add, ALU.mult), r=[n2, nh], w=[oname])

        def rope(i, bk, dst, dname, tmp):
            z3 = bank(bk).rearrange("p (s d) -> p s d", d=64)
            d3 = dst.rearrange("p (s d) -> p s d", d=64)
            cb = cos3[:, i, :].unsqueeze(1).broadcast_to([128, 8, 8])
            sb = sin3[:, i, :].unsqueeze(1).broadcast_to([128, 8, 8])
            t4 = tmp.rearrange("p (a s j) -> p a s j", a=4, j=8)
            tn = dname + "_rt"
            act(lambda e: e.copy(dst, bank(bk)), r=[f"ps{bk}"], w=[dname])
            dve(lambda e: e.tensor_tensor(t4[:, 0], z3[:, :, 0:8], cb, ALU.mult), r=[f"ps{bk}", "cos_t"], w=[tn + "0"])
            dve(lambda e: e.tensor_tensor(t4[:, 1], z3[:, :, 8:16], sb, ALU.mult), r=[f"ps{bk}", "sin_t"], w=[tn + "1"])
            dve(lambda e: e.tensor_tensor(t4[:, 2], z3[:, :, 8:16], cb, ALU.mult), r=[f"ps{bk}", "cos_t"], w=[tn + "2"])
            dve(lambda e: e.tensor_tensor(t4[:, 3], z3[:, :, 0:8], sb, ALU.mult), r=[f"ps{bk}", "sin_t"], w=[tn + "3"])
            dve(lambda e: e.tensor_tensor(d3[:, :, 0:8], t4[:, 0], t4[:, 1], ALU.subtract), r=[tn + "0", tn + "1"], w=[dname])
            dve(lambda e: e.tensor_tensor(d3[:, :, 8:16], t4[:, 2], t4[:, 3], ALU.add), r=[tn + "2", tn + "3"], w=[dname])

        def stageA2(i):
            s2 = i % 2
            zmm(i, 0, 1)
            gelu_chain(i, 0, 1, gu[s2], f"gu{s2}")
            zmm(i, 1, 2)
            gelu_chain(i, 1, 2, gv[s2], f"gv{s2}")
            s3 = i % 3
            zmm(i, 2, 3)
            rope(i, 3, qb[s3], f"qb{s3}", rt[0])
            zmm(i, 3, 1)
            rope(i, 1, kb[s3], f"kb{s3}", rt[1])
            zmm(i, 4, 2)
            vb3 = vb[s2].rearrange("p (h d) -> p h d", d=130)
            act(lambda e: e.copy(vb3[:, :, 0:128], bank(2).rearrange("p (h d) -> p h d", d=128)),
                r=["ps2"], w=[f"vb{s2}"])
            dma("sp", v_d[:, i, :, :], vb3, r=[f"vb{s2}"], w=[f"v_d_{S.uid()}"], key=f"vb{s2}")
            g_ = gv[s2]
            g3 = g_.rearrange("p (g d) -> p g d", d=128)
            L = lst[s2]
            ln = f"lst{s2}"
            dve(lambda e: e.reduce_sum(L[:, 0:4], g3, axis=AX.X), r=[f"gv{s2}"], w=[ln + "s"])
            dve(lambda e: e.tensor_tensor(sq, g_, g_, ALU.mult), r=[f"gv{s2}"], w=["sq"])
            dve(lambda e: e.reduce_sum(L[:, 4:8], sq.rearrange("p (g d) -> p g d", d=128), axis=AX.X), r=["sq"], w=[ln + "q"])
            dve(lambda e: e.tensor_scalar(L[:, 8:12], L[:, 0:4], 1.0 / 128, None, ALU.mult), r=[ln + "s"], w=[ln + "m"])
            dve(lambda e: e.tensor_tensor(L[:, 12:16], L[:, 8:12], L[:, 8:12], ALU.mult), r=[ln + "m"], w=[ln + "v"])
            dve(lambda e: e.scalar_tensor_tensor(L[:, 12:16], L[:, 4:8], 1.0 / 128, L[:, 12:16], ALU.mult, ALU.subtract),
                r=[ln + "q", ln + "v"], w=[ln + "v"])
            dve(lambda e: e.tensor_scalar(L[:, 12:16], L[:, 12:16], EPS, None, ALU.add), r=[ln + "v"], w=[ln + "v"])
            pool(lambda e: e.tensor_tensor(L[:, 4:8], L[:, 12:16], nhalf[:, 0:4], ALU.pow), r=[ln + "v", "nhalf", ln + "q"], w=[ln + "r"])
            for g in range(4):
                dve(lambda e, g=g: e.tensor_scalar(vn[:, g * 128:(g + 1) * 128], g_[:, g * 128:(g + 1) * 128],
                                                   L[:, 8 + g:9 + g], L[:, 4 + g:5 + g], ALU.subtract, ALU.mult),
                    r=[f"gv{s2}", ln + "m", ln + "r"], w=[f"vn{g}"])
            vnr = [f"vn{g}" for g in range(4)]
            dve(lambda e: e.tensor_tensor(vn, vn, lng, ALU.mult), r=vnr + ["lng"], w=vnr)
            dve(lambda e: e.tensor_tensor(vnb[s2], vn, lnb, ALU.add), r=vnr + ["lnb"], w=[f"vnb{s2}"])

        def stageA3(i):
            s2 = i % 2

            def sp_mm(e):
                last = None
                for g in range(4):
                    last = e.matmul(bank(4)[:, g * 128:(g + 1) * 128], wsT3[:, g, :], vnb[s2][:, g * 128:(g + 1) * 128],
                                    start=True, stop=True)
                return last
            KA3 = int(os.environ.get("KA3", "9"))
            pe(sp_mm, r=[f"vnb{s2}", "wsT"], w=["ps4"])
            if KA3 < 2: return
            for g in range(4):
                dve(lambda e, g=g: e.scalar_tensor_tensor(yab[s2][:, g * 128:(g + 1) * 128], bank(4)[:, g * 128:(g + 1) * 128],
                                                          bs[:, g:g + 1], gu[s2][:, g * 128:(g + 1) * 128], ALU.add, ALU.mult),
                    r=["ps4", "bs", f"gu{s2}"], w=[f"yab{s2}_{g}"])

        def stageA4(i):
            s2 = i % 2
            s3 = i % 3
            KA3 = 9
            yres = [f"yab{s2}_{g}" for g in range(4)]

            def tr(src, bk):
                def f(e):
                    last = None
                    for c in range(4):
                        last = e.transpose(bankbf(bk)[:, c * 128:(c + 1) * 128], src[:, c * 128:(c + 1) * 128], ident)
                    return last
                return f
            pe(tr(yab[s2], 5), r=yres + ["ident"], w=["ps5"])
            pe(tr(qb[s3], 6), r=[f"qb{s3}", "ident"], w=["ps6"])
            pe(tr(kb[s3], 7), r=[f"kb{s3}", "ident"], w=["ps7"])
            act(lambda e: e.copy(yaT[s2], bankbf(5)[:, 0:512]), r=["ps5"], w=[f"yaT{s2}"])
            dve(lambda e: e.tensor_copy(qT[s2], bankbf(6)[:, 0:512]), r=["ps6"], w=[f"qT{s2}"])
            act(lambda e: e.copy(kT[s2], bankbf(7)[:, 0:512]), r=["ps7"], w=[f"kT{s2}"])
            if KA3 < 5: return
            dma("sp", yaT_d[i], yaT[s2], r=[f"yaT{s2}"], w=[f"yaT_d_{S.uid()}"], key=f"yaT{s2}")
            dma("sp", qT_d[:, :, i * 128:(i + 1) * 128], qT[s2].rearrange("p (h t) -> p h t", t=128),
                r=[f"qT{s2}"], w=[f"qT_d_{S.uid()}"], key=f"qT{s2}")
            dma("sp", kT_d[:, :, i * 128:(i + 1) * 128], kT[s2].rearrange("p (h t) -> p h t", t=128),
                r=[f"kT{s2}"], w=[f"kT_d_{S.uid()}"], key=f"kT{s2}")

        stagesA = [stageA0, stageA1, stageA2, stageA3, stageA4]
        for s_ in range(nt_a + len(stagesA) - 1):
            for k_, fn in enumerate(stagesA):
                if 0 <= s_ - k_ < nt_a:
                    fn(s_ - k_)
        S.barrier()
        A.release(mark_phase)

        if stop_after == "A":
            return finish()

        KT_sb = A.bf16(4 * T)
        KT3 = KT_sb.rearrange("p (h t) -> p h t", t=T)
        V_sb = A.bf16(NT * 4 * 130)
        V4 = V_sb.rearrange("p (i h d) -> p i h d", h=4, d=130)
        for h in range(4):
            dma("sp", KT3[:, h, :], kT_d[:, h, :], r=["kT_d"], w=[f"KT{h}"], key=f"KTl{h}")
        for c in range(4):
            dma("sp", V4[:, c * 16:(c + 1) * 16], v_d[:, c * 16:(c + 1) * 16], r=["v_d"], w=[f"V{c}"], key=f"Vl{c}")
        subgc = A.f32(1)
        dma("sp", subgc, subgc_d, w=["subgc"], key="subgc")
        dve(lambda e: e.tensor_scalar(subgc, subgc, 1.0 - LAM_INIT, None, ALU.mult), r=["subgc"], w=["subgc"])
        onesf = A.f32(128)
        dve(lambda e: e.memset(onesf, 1.0), w=["onesf"])
        QTs = [A.bf16(4 * 512) for _ in range(2)]
        NPT = 4
        pTall = A.bf16(2 * NPT * 512)
        pT4 = pTall.rearrange("p (m s q) -> p m s q", m=2, s=NPT)
        pT = [[pT4[:, m_, s_, :] for s_ in range(NPT)] for m_ in range(2)]
        racc = [A.f32(512) for _ in range(2)]
        a0B = A.f32(512)
        a1B = A.f32(512)
        l1B = A.f32(512)
        rlb = [A.f32(512) for _ in range(2)]
        t1B = A.f32(512)
        t2B = A.f32(512)
        oB = A.f32(512)
        sqB = A.f32(512)
        v4B = A.f32(4)
        rs4B = A.f32(4)
        RmB = A.f32(512)
        ybT = [A.bf16(512) for _ in range(2)]
        n_st = nt_b // 4
        blocks = [(I_, h, j) for I_ in range(n_st) for h in range(4) for j in range(4 * I_ + 4)]

        def load_q(I_):
            sl = I_ % 2
            dma("sp", QTs[sl].rearrange("p (h t) -> p h t", t=512), qT_d[:, :, I_ * 512:(I_ + 1) * 512],
                r=["qT_d"], w=[f"QT{sl}"], key=f"QT{sl}")

        def emit_qk(n):
            I_, h, j = blocks[n]
            par = n % 2
            qlo = max(0, j - 4 * I_)
            ncol = 512 - qlo * 128
            Q3 = QTs[I_ % 2].rearrange("p (h t) -> p h t", t=512)
            diag = j >= 4 * I_

            def f(e):
                last = None
                for m in range(2):
                    last = e.matmul(bank(2 * m + par)[:, 0:ncol], KT3[m * 64:(m + 1) * 64, h, j * 128:(j + 1) * 128],
                                    Q3[m * 64:(m + 1) * 64, h, qlo * 128:512], start=True, stop=not diag)
                if diag:
                    for m in range(2):
                        last = e.matmul(bank(2 * m + par)[:, 0:128], ident, maskb, start=False, stop=True)
                return last
            pe(f, r=[f"KT{h}", f"QT{I_ % 2}", "ident", "maskb"], w=[f"ps{par}", f"ps{2 + par}"])
            sl_ = n % NPT
            act(lambda e: e.activation(pT4[:, :, sl_, 0:ncol], ps[:, par:par + 3:2, 0:ncol], AF.Exp, scale=0.125),
                r=[f"ps{par}", f"ps{2 + par}"], w=[f"pT0{sl_}", f"pT1{sl_}"])

        def emit_pv(n):
            I_, h, j = blocks[n]
            par = n % NPT
            qlo = max(0, j - 4 * I_)
            ncol = 512 - qlo * 128
            jlast = 4 * I_ + 3

            def f(e):
                last = None
                for m in range(2):
                    last = e.matmul(bank(4 + m)[:, qlo * 128:512], V4[:, j, h, 0:128], pT[m][par][:, 0:ncol],
                                    start=(j == 0), stop=(j == jlast))
                last = e.matmul(bank(6)[:, qlo * 128:512], onesm, pT[1][par][:, 0:ncol], start=(j == 0), stop=(j == jlast))
                return last
            pe(f, r=[f"pT0{par}", f"pT1{par}", f"V{j // 16}", "onesm"], w=["ps4", "ps5", "ps6"])
            rc = racc[(I_ * 4 + h) % 2]
            rn = f"racc{(I_ * 4 + h) % 2}"
            if j == 0:
                dve(lambda e: e.tensor_copy(rc, pT[0][par]), r=[f"pT0{par}"], w=[rn])
            else:
                dve(lambda e: e.tensor_tensor(rc[:, qlo * 128:512], rc[:, qlo * 128:512], pT[0][par][:, 0:ncol], ALU.add),
                    r=[f"pT0{par}", rn], w=[rn])
            if j == jlast:
                offs = [0, 1, 2, 4, 6, 9, 10, 12, 13] if I_ >= 3 else ([0, 1, 2, 3, 4, 5, 6, 7, 8] if I_ == 2 else [0] * 9)
                for k_, fn in enumerate(head_steps(I_, h)):
                    pending.append((n + offs[k_], fn))

        def head_steps(I_, h):
            sl = (I_ * 4 + h) % 2
            rc = racc[(I_ * 4 + h) % 2]
            rn = f"racc{(I_ * 4 + h) % 2}"

            def s0():
                dve(lambda e: e.tensor_copy(a1B, bank(5)), r=["ps5"], w=["a1B"])
                dve(lambda e: e.tensor_copy(a0B, bank(4)), r=["ps4"], w=["a0B"])
                dve(lambda e: e.tensor_copy(l1B, bank(6)), r=["ps6"], w=["l1B"])

            def s1():
                pe(lambda e: e.matmul(bank(7), onesf, rc, start=True, stop=True), r=["onesf", rn], w=["ps7"])

            def s2a():
                dve(lambda e: e.reciprocal(rlb[1], l1B), r=["l1B"], w=["rlb1"])

            def s2b():
                dve(lambda e: e.reciprocal(rlb[0], bank(7)), r=["ps7"], w=["rlb0"])

            def s2():
                dve(lambda e: e.tensor_tensor(t2B, a1B, rlb[1], ALU.mult), r=["a1B", "rlb1"], w=["t2B"])
                dve(lambda e: e.tensor_tensor(t1B, a0B, rlb[0], ALU.mult), r=["a0B", "rlb0"], w=["t1B"])
                dve(lambda e: e.scalar_tensor_tensor(oB, t2B, neglam, t1B, ALU.mult, ALU.add), r=["t1B", "t2B", "neglam"], w=["oB"])
                dve(lambda e: e.tensor_tensor(sqB, oB, oB, ALU.mult), r=["oB"], w=["sqB"])

            def s3():
                def ssq_mm(e):
                    last = None
                    for r_ in range(4):
                        last = e.matmul(bank(7)[:, r_:r_ + 1], sqB[:, r_ * 128:(r_ + 1) * 128], onesf[:, 0:1], start=True, stop=True)
                    return last
                pe(ssq_mm, r=["sqB", "onesf"], w=["ps7"])

            def s4():
                dve(lambda e: e.tensor_scalar(v4B, bank(7)[:, 0:4], 1.0 / 128, EPS, ALU.mult, ALU.add), r=["ps7"], w=["v4B"])
                pool(lambda e: e.tensor_tensor(rs4B, v4B, nhalf[:, 0:4], ALU.pow), r=["v4B", "nhalf"], w=["rs4B"])
                for r_ in range(4):
                    dve(lambda e, r_=r_: e.tensor_scalar(RmB[:, r_ * 128:(r_ + 1) * 128], identf, rs4B[:, r_:r_ + 1], None, ALU.mult),
                        r=["rs4B", "identf"], w=[f"RmB{r_}"])

            def s5():
                def bc_mm(e):
                    last = None
                    for r_ in range(4):
                        last = e.matmul(bank(7)[:, r_ * 128:(r_ + 1) * 128], onesf, RmB[:, r_ * 128:(r_ + 1) * 128], start=True, stop=True)
                    return last
                pe(bc_mm, r=[f"RmB{r_}" for r_ in range(4)] + ["onesf"], w=["ps7"])

            def s6():
                dve(lambda e: e.scalar_tensor_tensor(ybT[sl], oB, subgc, bank(7), ALU.mult, ALU.mult), r=["oB", "subgc", "ps7"], w=[f"ybT{sl}"])
                dma("sp", ybT_d[4 * I_:4 * I_ + 4, :, h * 128:(h + 1) * 128].rearrange("r p t -> p r t"),
                    ybT[sl].rearrange("p (r t) -> p r t", t=128), r=[f"ybT{sl}"], w=[f"ybT_d_{S.uid()}"], key=f"ybT{sl}")
            return [s0, s1, s2a, s2b, s2, s3, s4, s5, s6]

        def warmup(nmm, bk):
            def f(e):
                last = None
                for _ in range(nmm):
                    last = e.matmul(bank(bk), ident, KT3[:, 0, 0:512], start=True, stop=True)
                return last
            pe(f, r=["ident", "KT0"], w=[f"ps{bk}"])

        pending = []
        if n_st > 0:
            load_q(0)
        for n in range(len(blocks) + 16):
            if n < len(blocks):
                I_, h, j = blocks[n]
                if h == 0 and j == 0:
                    if I_ + 1 < n_st:
                        load_q(I_ + 1)
                    warmup(20, n % 2)
                emit_qk(n)
            if 1 <= n <= len(blocks):
                emit_pv(n - 1)
            due = [p for p in pending if p[0] <= n - 1]
            pending[:] = [p for p in pending if p[0] > n - 1]
            for _, fn in due:
                fn()
        assert not pending
        S.barrier()
        A.release(mark_phase)
        if stop_after == "B":
            return finish()

        wg = A.bf16(8 * 2048)
        wg3 = wg.rearrange("p (c n) -> p c n", n=2048)
        wa = A.bf16(4 * 1024)
        wa3 = wa.rearrange("p (c n) -> p c n", n=1024)
        wb = A.bf16(4 * 1024)
        wb3 = wb.rearrange("p (c n) -> p c n", n=1024)
        wo = A.bf16(8 * 1024)
        wo3 = wo.rearrange("p (c n) -> p c n", n=1024)
        wr = A.f32(8 * 36)
        wr3 = wr.rearrange("p (c n) -> p c n", n=36)
        gffn = A.f32(D)
        brt = A.f32(36)
        gmixC = A.f32(8)
        dma("sp", gmixC, gmix_d, w=["gmixC"], key="gmixC")
        dma("sp", gffn, gffn_d, w=["gffn"], key="gffn")
        dma("sp", brt, br_d, w=["brt"], key="brt")
        dma("sp", wr3, wr_d.rearrange("(c p) n -> p c n", p=128), w=["wr"], key="wr")
        mC = A.mark()
        stg = [A.f32(2048), A.f32(2048)]
        nld = [0]

        def wload(src, ncol, dst, scale_ap=None, scale_f=None, dname=None):
            sl = nld[0] % 2
            nld[0] += 1
            dma("sp", stg[sl][:, 0:ncol], src, w=[f"stg{sl}"], key=f"stg{sl}")
            if sl == 0:
                if scale_ap is not None:
                    dve(lambda e: e.tensor_scalar(dst, stg[sl][:, 0:ncol], scale_ap, None, ALU.mult), r=[f"stg{sl}", "gmixC"], w=[dname])
                elif scale_f is not None:
                    dve(lambda e: e.tensor_scalar(dst, stg[sl][:, 0:ncol], scale_f, None, ALU.mult), r=[f"stg{sl}"], w=[dname])
                else:
                    dve(lambda e: e.tensor_copy(dst, stg[sl][:, 0:ncol]), r=[f"stg{sl}"], w=[dname])
            else:
                sc = scale_ap if scale_ap is not None else (scale_f if scale_f is not None else 1.0)
                act(lambda e: e.activation(dst, stg[sl][:, 0:ncol], AF.Copy, scale=sc), r=[f"stg{sl}", "gmixC"], w=[dname])
        for c in range(8):
            wload(win_d[c * 128:(c + 1) * 128, NC_IN:4608], 2048, wg3[:, c, :], scale_ap=gmixC[:, c:c + 1], dname=f"wg{c}")
        for c in range(4):
            wload(wa_d[c * 128:(c + 1) * 128, :], 1024, wa3[:, c, :], dname=f"wa{c}")
            wload(wb_d[c * 128:(c + 1) * 128, :], 1024, wb3[:, c, :], dname=f"wb{c}")
        for c in range(8):
            wload(wo_d[c * 128:(c + 1) * 128, :], 1024, wo3[:, c, :], scale_f=0.5, dname=f"wo{c}")
        S.barrier()
        A.release(mC)
        wgres = [f"wg{c}" for c in range(8)]

        xtC = [A.f32(D) for _ in range(5)]
        junkC = A.bf16(D)
        ssC = [A.f32(1) for _ in range(2)]
        vC = [A.f32(1) for _ in range(2)]
        rsC = [A.f32(1) for _ in range(2)]
        hbC = [A.bf16(D) for _ in range(2)]
        hTC = [A.bf16(D) for _ in range(2)]
        yaL = [A.bf16(512) for _ in range(3)]
        ybL = [A.bf16(512) for _ in range(3)]
        th = A.f32(2048)
        m1 = A.f32(D)
        m2 = A.f32(D)
        mbs = [A.bf16(D) for _ in range(2)]
        mTs = [A.bf16(D) for _ in range(2)]
        x1t = [A.f32(D) for _ in range(2)]
        ss2 = A.f32(1)
        v2 = A.f32(1)
        rs2 = A.f32(1)
        h2fs = [A.f32(D) for _ in range(2)]
        h2b = [A.bf16(D) for _ in range(4)]
        h2Ts = [A.f32(D) for _ in range(2)]
        Lg = A.f32(36)
        sm = A.f32(16)
        goh = A.f32(4)
        gex = A.f32(4)
        pen = A.f32(4)
        elm = A.f32(32)
        top8 = A.f32(8)
        oh1s = [A.f32(32) for _ in range(2)]
        oh2s = [A.f32(32) for _ in range(2)]
        Mbs = [A.bf16(32) for _ in range(2)]
        posC = A.f32(32)
        tmp32 = A.f32(32)

        def stageC0(i):
            xs = i % 5
            s2 = i % 2
            s3 = i % 3
            dma("sp", xtC[xs], x_d[i * 128:(i + 1) * 128, :], w=[f"xtC{xs}"], key=f"xtC{xs}")
            dma("sp", yaL[s3], yaT_d[i], r=["yaT_d"], w=[f"yaL{s3}"], key=f"yaL{s3}")
            dma("sp", ybL[s3], ybT_d[i], r=["ybT_d"], w=[f"ybL{s3}"], key=f"ybL{s3}")
            act(lambda e: e.activation(junkC, xtC[xs], AF.Square, accum_out=ssC[s2]), r=[f"xtC{xs}"], w=["junkC", f"Css{s2}"])
            dve(lambda e: e.tensor_scalar(vC[s2], ssC[s2], 1.0 / D, EPS, ALU.mult, ALU.add), r=[f"Css{s2}"], w=[f"Cv{s2}"])
            pool(lambda e: e.tensor_tensor(rsC[s2], vC[s2], nhalf[:, 0:1], ALU.pow), r=[f"Cv{s2}", "nhalf"], w=[f"Crs{s2}"])
            dve(lambda e: e.tensor_scalar(hbC[s2], xtC[xs], rsC[s2], None, ALU.mult), r=[f"xtC{xs}", f"Crs{s2}"], w=[f"hbC{s2}"])

        def stageC1(i):
            s2 = i % 2

            def tr(e):
                last = None
                for c in range(8):
                    last = e.transpose(bankbf(0)[:, c * 128:(c + 1) * 128], hbC[s2][:, c * 128:(c + 1) * 128], ident)
                return last
            pe(tr, r=[f"hbC{s2}", "ident"], w=["ps0"])
            act(lambda e: e.copy(hTC[s2], bankbf(0)), r=["ps0"], w=[f"hTC{s2}"])

        def stageC2(i):
            s2 = i % 2
            s3 = i % 3
            hT3 = hTC[s2].rearrange("p (c t) -> p c t", t=128)
            for cg in range(4):
                bk = 1 + cg % 2

                def mm(e, cg=cg, bk=bk):
                    last = None
                    for c in range(8):
                        last = e.matmul(bank(bk), hT3[:, c, :], wg3[:, c, cg * 512:(cg + 1) * 512], start=(c == 0), stop=(c == 7))
                    return last
                pe(mm, r=[f"hTC{s2}"] + wgres, w=[f"ps{bk}"])
                act(lambda e, cg=cg, bk=bk: e.activation(th[:, cg * 512:(cg + 1) * 512], bank(bk), AF.Tanh, scale=0.5),
                    r=[f"ps{bk}"], w=[f"th{cg}"])
            yl3 = yaL[s3].rearrange("p (c t) -> p c t", t=128)
            bl3 = ybL[s3].rearrange("p (c t) -> p c t", t=128)
            for half in range(2):
                def mma(e, half=half):
                    last = None
                    for c in range(4):
                        last = e.matmul(bank(3 + half), yl3[:, c, :], wa3[:, c, half * 512:(half + 1) * 512], start=(c == 0), stop=(c == 3))
                    return last
                pe(mma, r=[f"yaL{s3}"] + [f"wa{c}" for c in range(4)], w=[f"ps{3 + half}"])

                def mmb(e, half=half):
                    last = None
                    for c in range(4):
                        last = e.matmul(bank(5 + half), bl3[:, c, :], wb3[:, c, half * 512:(half + 1) * 512], start=(c == 0), stop=(c == 3))
                    return last
                pe(mmb, r=[f"ybL{s3}"] + [f"wb{c}" for c in range(4)], w=[f"ps{5 + half}"])
            for half in range(2):
                dve(lambda e, half=half: e.scalar_tensor_tensor(m1[:, half * 512:(half + 1) * 512], th[:, half * 512:(half + 1) * 512], 1.0,
                                                                bank(3 + half), ALU.add, ALU.mult),
                    r=[f"th{half}", f"ps{3 + half}"], w=[f"m1{half}"])
                dve(lambda e, half=half: e.scalar_tensor_tensor(m2[:, half * 512:(half + 1) * 512], th[:, 1024 + half * 512:1024 + (half + 1) * 512], 1.0,
                                                                bank(5 + half), ALU.add, ALU.mult),
                    r=[f"th{2 + half}", f"ps{5 + half}"], w=[f"m2{half}"])
            dve(lambda e: e.tensor_tensor(mbs[s2], m1, m2, ALU.add), r=["m10", "m11", "m20", "m21"], w=[f"mb{s2}"])

        def stageC3(i):
            s2 = i % 2

            def trm(e):
                last = None
                for c in range(8):
                    last = e.transpose(bankbf(0)[:, c * 128:(c + 1) * 128], mbs[s2][:, c * 128:(c + 1) * 128], ident)
                return last
            pe(trm, r=[f"mb{s2}", "ident"], w=["ps0"])
            act(lambda e: e.copy(mTs[s2], bankbf(0)), r=["ps0"], w=[f"mT{s2}"])

        def stageC4o(i):
            xs = i % 5
            s2 = i % 2
            s4 = i % 4
            mT3 = mTs[s2].rearrange("p (c t) -> p c t", t=128)
            for half in range(2):
                def mmo(e, half=half):
                    last = None
                    for c in range(8):
                        last = e.matmul(bank(1 + half), mT3[:, c, :], wo3[:, c, half * 512:(half + 1) * 512], start=(c == 0), stop=(c == 7))
                    return last
                pe(mmo, r=[f"mT{s2}"] + [f"wo{c}" for c in range(8)], w=[f"ps{1 + half}"])
                dve(lambda e, half=half: e.tensor_tensor(x1t[s2][:, half * 512:(half + 1) * 512], xtC[xs][:, half * 512:(half + 1) * 512],
                                                         bank(1 + half), ALU.add),
                    r=[f"xtC{xs}", f"ps{1 + half}"], w=[f"x1t{s2}_{half}"])
            x1res = [f"x1t{s2}_0", f"x1t{s2}_1"]
            dma("sp", x1_d[i * 128:(i + 1) * 128, :], x1t[s2], r=x1res, w=[f"x1_d_{S.uid()}"], key=f"x1t{s2}")
            act(lambda e: e.activation(junkC, x1t[s2], AF.Square, accum_out=ss2), r=x1res, w=["junkC", "ss2"])
            dve(lambda e: e.tensor_scalar(v2, ss2, 1.0 / D, EPS, ALU.mult, ALU.add), r=["ss2"], w=["v2"])
            pool(lambda e: e.tensor_tensor(rs2, v2, nhalf[:, 0:1], ALU.pow), r=["v2", "nhalf"], w=["rs2"])
            dve(lambda e: e.scalar_tensor_tensor(h2fs[s2], x1t[s2], rs2, gffn, ALU.mult, ALU.mult), r=x1res + ["rs2", "gffn"], w=[f"h2f{s2}"])
            act(lambda e: e.copy(h2b[s4], h2fs[s2]), r=[f"h2f{s2}"], w=[f"h2b{s4}"])

        def stageC5(i):
            s2 = i % 2
            h2f = h2fs[s2]
            h2T = h2Ts[s2]

            def trr(e):
                last = None
                for c in range(8):
                    last = e.transpose(ps[:, 3 + c // 4, (c % 4) * 128:(c % 4 + 1) * 128], h2f[:, c * 128:(c + 1) * 128], identf)
                return last
            pe(trr, r=[f"h2f{s2}", "identf"], w=["ps3", "ps4"])
            act(lambda e: e.copy(h2T[:, 0:512], bank(3)), r=["ps3"], w=[f"h2Ta{s2}"])
            act(lambda e: e.copy(h2T[:, 512:1024], bank(4)), r=["ps4"], w=[f"h2Tb{s2}"])

        def stageC6(i):
            s2 = i % 2
            oh1, oh2, Mb = oh1s[s2], oh2s[s2], Mbs[s2]
            h2T3 = h2Ts[s2].rearrange("p (c t) -> p c t", t=128)

            def mmr(e):
                last = None
                for c in range(8):
                    last = e.matmul(bank(7)[:, 0:36], h2T3[:, c, :], wr3[:, c, :], start=(c == 0), stop=(c == 7))
                return last
            pe(mmr, r=[f"h2Ta{s2}", f"h2Tb{s2}", "wr"], w=["ps7"])
            dve(lambda e: e.tensor_tensor(Lg, bank(7)[:, 0:36], brt, ALU.add), r=["ps7", "brt"], w=["Lg"])
            dve(lambda e: e.reduce_max(sm[:, 0:1], Lg[:, 0:4], axis=AX.X), r=["Lg"], w=["gmax"])
            dve(lambda e: e.tensor_scalar(goh, Lg[:, 0:4], sm[:, 0:1], None, ALU.is_equal), r=["Lg", "gmax"], w=["goh"])
            dve(lambda e: e.tensor_scalar(sm[:, 1:2], sm[:, 0:1], -1.0, None, ALU.mult), r=["gmax"], w=["negg"])
            act(lambda e: e.activation(gex, Lg[:, 0:4], AF.Exp, bias=sm[:, 1:2], accum_out=sm[:, 2:3]), r=["Lg", "negg"], w=["gex", "gsum"])
            dve(lambda e: e.reciprocal(sm[:, 3:4], sm[:, 2:3]), r=["gsum"], w=["gw"])
            dve(lambda e: e.tensor_scalar(pen, goh, -1.0, 1e30, ALU.add, ALU.mult), r=["goh"], w=["pen"])
            dve(lambda e: e.tensor_tensor(elm.rearrange("p (g e) -> p g e", e=8), Lg[:, 4:36].rearrange("p (g e) -> p g e", e=8),
                                          pen.unsqueeze(2).broadcast_to([128, 4, 8]), ALU.add), r=["Lg", "pen"], w=["elm"])
            dve(lambda e: e.max(top8, elm), r=["elm"], w=["top8"])
            dve(lambda e: e.tensor_scalar(oh1, elm, top8[:, 0:1], None, ALU.is_equal), r=["elm", "top8"], w=[f"oh1_{s2}"])
            dve(lambda e: e.tensor_scalar(oh2, elm, top8[:, 1:2], None, ALU.is_equal), r=["elm", "top8"], w=[f"oh2_{s2}"])
            dve(lambda e: e.tensor_scalar(sm[:, 4:5], top8[:, 0:1], -1.0, None, ALU.mult), r=["top8"], w=["negv1"])
            act(lambda e: e.activation(sm[:, 5:6], top8[:, 1:2], AF.Exp, bias=sm[:, 4:5]), r=["top8", "negv1"], w=["e2"])
            dve(lambda e: e.tensor_scalar(sm[:, 6:7], sm[:, 5:6], 1.0, None, ALU.add), r=["e2"], w=["den"])
            dve(lambda e: e.reciprocal(sm[:, 7:8], sm[:, 6:7]), r=["den"], w=["p1"])
            dve(lambda e: e.tensor_tensor(wts3[:, i, 0:1], sm[:, 7:8], sm[:, 3:4], ALU.mult), r=["p1", "gw"], w=[f"w1_{i}"])
            dve(lambda e: e.tensor_tensor(wts3[:, i, 1:2], wts3[:, i, 0:1], sm[:, 5:6], ALU.mult), r=[f"w1_{i}", "e2"], w=[f"w2_{i}"])
            dve(lambda e: e.tensor_tensor(Mb, oh1, oh2, ALU.add), r=[f"oh1_{s2}", f"oh2_{s2}"], w=[f"Mb_{s2}"])

        def stageC7(i):
            s2 = i % 2
            s4 = i % 4
            oh1, oh2, Mb = oh1s[s2], oh2s[s2], Mbs[s2]
            pe(lambda e: e.matmul(bank(5)[:, 0:32], ustrict, Mb, start=True, stop=True), r=["ustrict", f"Mb_{s2}"], w=["ps5"])
            pe(lambda e: e.matmul(bank(6)[:, 0:32], onesm, Mb, start=True, stop=True), r=["onesm", f"Mb_{s2}"], w=["ps6"])
            dve(lambda e: e.tensor_tensor(posC, bank(5)[:, 0:32], base_cnt, ALU.add), r=["ps5", "base_cnt"], w=["posCr"])
            dve(lambda e: e.tensor_scalar(posC, posC, float(CAP - 1), None, ALU.min), r=["posCr"], w=["posCr"])
            dve(lambda e: e.tensor_tensor(posC, posC, eoff, ALU.add), r=["posCr", "eoff"], w=["posCr"])
            dve(lambda e: e.tensor_tensor(tmp32, posC, oh1, ALU.mult), r=["posCr", f"oh1_{s2}"], w=["tmp32"])
            dve(lambda e: e.reduce_sum(sm[:, 8:9], tmp32, axis=AX.X), r=["tmp32"], w=["s1f"])
            dve(lambda e: e.tensor_tensor(tmp32, posC, oh2, ALU.mult), r=["posCr", f"oh2_{s2}", "tmp32"], w=["tmp32"])
            dve(lambda e: e.reduce_sum(sm[:, 9:10], tmp32, axis=AX.X), r=["tmp32"], w=["s2f"])
            dve(lambda e: e.tensor_copy(slots3[:, i, 0:1], sm[:, 8:9]), r=["s1f"], w=[f"sl1_{i}"])
            dve(lambda e: e.tensor_copy(slots3[:, i, 1:2], sm[:, 9:10]), r=["s2f"], w=[f"sl2_{i}"])
            dve(lambda e: e.tensor_tensor(base_cnt, base_cnt, bank(6)[:, 0:32], ALU.add), r=["ps6", "base_cnt"], w=["base_cnt"])
            for k in range(2):
                S.add("pool", lambda e, k=k: e.indirect_dma_start(
                    out=xs_d, out_offset=bass.IndirectOffsetOnAxis(ap=slots3[:, i, k:k + 1], axis=0),
                    in_=h2b[s4], in_offset=None),
                    reads=[f"sl{k + 1}_{i}", f"h2b{s4}"] + [f"xs_zero_{z}" for z in range(4)], writes=[f"xs_d{k}"], dma=f"scat{k}_{s4}")
            if debug:
                dma("sp", rt_d[:, i, 0:2], wts3[:, i, :], r=[f"w1_{i}", f"w2_{i}"], key="dbgrt")

        stagesC = [stageC0, stageC1, stageC2, stageC3, stageC4o, stageC5, stageC6, stageC7]
        for s_ in range(nt_c + len(stagesC) - 1):
            for k_, fn in enumerate(stagesC):
                if 0 <= s_ - k_ < nt_c:
                    fn(s_ - k_)
        S.barrier()
        A.release(mark_phase)
        if stop_after == "C":
            return finish()

        w1s = A.f32(8 * 512)
        w3s = A.f32(8 * 512)
        w2s = A.f32(4 * 1024)
        w1b = [A.bf16(8 * 512) for _ in range(2)]
        w3b = [A.bf16(8 * 512) for _ in range(2)]
        w2b = [A.bf16(4 * 1024) for _ in range(2)]
        xg = [A.bf16(D) for _ in range(3)]
        XT = [A.bf16(8 * CAP) for _ in range(2)]
        AT = [A.bf16(4 * CAP) for _ in range(2)]
        thD = [A.f32(CAP // 2) for _ in range(2)]
        a1D = [A.f32(CAP // 2) for _ in range(2)]
        ysb = [A.bf16(D) for _ in range(3)]
        HC = CAP // 2

        def load_w(e_):
            for c in range(8):
                dma("sp", w1s[:, c * 512:(c + 1) * 512], w1_d[e_, c * 128:(c + 1) * 128, :], w=[f"w1s{c}"], key="w1s")
                dma("sp", w3s[:, c * 512:(c + 1) * 512], w3_d[e_, c * 128:(c + 1) * 128, :], w=[f"w3s{c}"], key="w3s")
                dma("sp", w2s[:, c * 512:(c + 1) * 512], w2_d[e_, (c // 2) * 128:(c // 2 + 1) * 128, (c % 2) * 512:(c % 2 + 1) * 512],
                    w=[f"w2s{c}"], key="w2s")

        def cast_w_chunk(e_, c):
            sl = e_ % 2
            act(lambda e: e.copy(w1b[sl][:, c * 512:(c + 1) * 512], w1s[:, c * 512:(c + 1) * 512]), r=[f"w1s{cc}" for cc in range(8)], w=[f"w1b{sl}_{c}"])
            dve(lambda e: e.tensor_copy(w3b[sl][:, c * 512:(c + 1) * 512], w3s[:, c * 512:(c + 1) * 512]), r=[f"w3s{cc}" for cc in range(8)], w=[f"w3b{sl}_{c}"])
            dve(lambda e: e.tensor_scalar(w2b[sl][:, c * 512:(c + 1) * 512], w2s[:, c * 512:(c + 1) * 512], 0.5, None, ALU.mult),
                r=[f"w2s{cc}" for cc in range(8)], w=[f"w2b{sl}_{c}"])

        def cast_w(e_):
            for c in range(8):
                cast_w_chunk(e_, c)

        nblk_ct = [0]

        def xblock(e_, b):
            sl = e_ % 2
            XT3 = XT[sl].rearrange("p (c t) -> p c t", t=CAP)
            g = nblk_ct[0] % 3
            nblk_ct[0] += 1
            blk = e_ * NBLK + b
            tb = 0 if b % 2 == 0 else 7
            dma("sp", xg[g], xs_d[blk * 128:(blk + 1) * 128, :], w=[f"xg{g}"], key=f"xg{g}")

            def trx(e):
                last = None
                for c in range(8):
                    last = e.transpose(bankbf(tb)[:, c * 128:(c + 1) * 128], xg[g][:, c * 128:(c + 1) * 128], ident)
                return last
            pe(trx, r=[f"xg{g}", "ident"], w=[f"ps{tb}"])
            if b % 2 == 0:
                act(lambda e: e.copy(XT3[:, :, b * 128:(b + 1) * 128], bankbf(tb).rearrange("p (c t) -> p c t", t=128)),
                    r=[f"ps{tb}"], w=[f"XT{sl}_{b}"])
            else:
                dve(lambda e: e.tensor_copy(XT3[:, :, b * 128:(b + 1) * 128], bankbf(tb).rearrange("p (c t) -> p c t", t=128)),
                    r=[f"ps{tb}"], w=[f"XT{sl}_{b}"])

        def gate_up_step(e_, k):
            sl = e_ % 2
            XT3 = XT[sl].rearrange("p (c t) -> p c t", t=CAP)
            AT3 = AT[sl].rearrange("p (c t) -> p c t", t=CAP)
            w1b3 = w1b[sl].rearrange("p (c n) -> p c n", n=512)
            w3b3 = w3b[sl].rearrange("p (c n) -> p c n", n=512)
            xtres = [f"XT{sl}_{b}" for b in range(NBLK)]
            dc, half = k // 2, k % 2
            bG = 1 + k % 2
            bU = 3 + k % 2
            t2 = k % 2

            def mmg(e):
                last = None
                for c in range(8):
                    last = e.matmul(bank(bG)[:, 0:HC], w1b3[:, c, dc * 128:(dc + 1) * 128], XT3[:, c, half * HC:(half + 1) * HC],
                                    start=(c == 0), stop=(c == 7))
                return last
            pe(mmg, r=xtres + [f"w1b{sl}_{c}" for c in range(8)], w=[f"ps{bG}"])

            def mmu(e):
                last = None
                for c in range(8):
                    last = e.matmul(bank(bU)[:, 0:HC], w3b3[:, c, dc * 128:(dc + 1) * 128], XT3[:, c, half * HC:(half + 1) * HC],
                                    start=(c == 0), stop=(c == 7))
                return last
            pe(mmu, r=xtres + [f"w3b{sl}_{c}" for c in range(8)], w=[f"ps{bU}"])
            act(lambda e: e.activation(thD[t2], bank(bG)[:, 0:HC], AF.Tanh, scale=0.5), r=[f"ps{bG}"], w=[f"thD{t2}"])
            dve(lambda e: e.scalar_tensor_tensor(a1D[t2], thD[t2], 1.0, bank(bG)[:, 0:HC], ALU.add, ALU.mult),
                r=[f"thD{t2}", f"ps{bG}"], w=[f"a1D{t2}"])
            dve(lambda e: e.tensor_tensor(AT3[:, dc, half * HC:(half + 1) * HC], a1D[t2], bank(bU)[:, 0:HC], ALU.mult),
                r=[f"a1D{t2}", f"ps{bU}"], w=[f"AT{sl}_{k}"])

        def down(e_):
            sl = e_ % 2
            AT3 = AT[sl].rearrange("p (c t) -> p c t", t=CAP)
            w2b3 = w2b[sl].rearrange("p (c n) -> p c n", n=1024)
            atres = [f"AT{sl}_{k}" for k in range(8)]
            for b in range(NBLK):
                blk = e_ * NBLK + b
                ysl = blk % 3
                for cg in range(2):
                    def mmy(e, b=b, cg=cg):
                        last = None
                        for dc in range(4):
                            last = e.matmul(bank(5 + cg), AT3[:, dc, b * 128:(b + 1) * 128], w2b3[:, dc, cg * 512:(cg + 1) * 512],
                                            start=(dc == 0), stop=(dc == 3))
                        return last
                    pe(mmy, r=atres + [f"w2b{sl}_{c}" for c in range(8)], w=[f"ps{5 + cg}"])
                    if cg == 0:
                        act(lambda e, ysl=ysl: e.copy(ysb[ysl][:, 0:512], bank(5)), r=["ps5"], w=[f"ysb{ysl}_0"])
                    else:
                        dve(lambda e, ysl=ysl: e.tensor_copy(ysb[ysl][:, 512:1024], bank(6)), r=["ps6"], w=[f"ysb{ysl}_1"])
                dma("sp", ys_d[blk * 128:(blk + 1) * 128, :], ysb[ysl], r=[f"ysb{ysl}_0", f"ysb{ysl}_1"], w=[f"ys_d_{S.uid()}"], key=f"ysb{ysl}")

        assert NBLK == 8
        if n_exp > 0:
            load_w(0)
            cast_w(0)
            if n_exp > 1:
                load_w(1)
            for b in range(NBLK):
                xblock(0, b)
        for e_ in range(n_exp):
            for k in range(8):
                gate_up_step(e_, k)
                if e_ + 1 < n_exp:
                    xblock(e_ + 1, k)
                    cast_w_chunk(e_ + 1, k)
            if e_ + 2 < n_exp:
                load_w(e_ + 2)
            down(e_)
        S.barrier()
        A.release(mark_phase)
        if stop_after == "D":
            return finish()

        fg = A.f32(D)
        dma("sp", fg, fg_d, w=["fg"], key="fg")
        x1L = [A.f32(D) for _ in range(3)]
        y1L = [A.bf16(D) for _ in range(3)]
        y2L = [A.bf16(D) for _ in range(3)]
        tE = [A.f32(D) for _ in range(2)]
        x2E = [A.f32(D) for _ in range(2)]
        junkE = A.bf16(D)
        ssE = [A.f32(1) for _ in range(2)]
        vE = [A.f32(1) for _ in range(2)]
        rsE = [A.f32(1) for _ in range(2)]
        oE = [A.f32(D) for _ in range(2)]

        def stageE1(i):
            s3 = i % 3
            dma("sp", x1L[s3], x1_d[i * 128:(i + 1) * 128, :], r=["x1_d"], w=[f"x1L{s3}"], key=f"x1L{s3}")
            for k, yL in ((0, y1L), (1, y2L)):
                S.add("pool", lambda e, k=k, yL=yL: e.indirect_dma_start(
                    out=yL[s3], out_offset=None, in_=ys_d,
                    in_offset=bass.IndirectOffsetOnAxis(ap=slots3[:, i, k:k + 1], axis=0)),
                    reads=["ys_d", "slots"], writes=[f"y{k}L{s3}"], dma=f"y{k}L{s3}")

        def stageE2(i):
            s3 = i % 3
            s2 = i % 2
            dve(lambda e: e.scalar_tensor_tensor(tE[s2], y1L[s3], wts3[:, i, 0:1], x1L[s3], ALU.mult, ALU.add),
                r=[f"y0L{s3}", f"x1L{s3}", "wts"], w=[f"tE{s2}"])
            dve(lambda e: e.scalar_tensor_tensor(x2E[s2], y2L[s3], wts3[:, i, 1:2], tE[s2], ALU.mult, ALU.add),
                r=[f"y1L{s3}", f"tE{s2}", "wts"], w=[f"x2E{s2}"])
            act(lambda e: e.activation(junkE, x2E[s2], AF.Square, accum_out=ssE[s2]), r=[f"x2E{s2}"], w=["junkE", f"ssE{s2}"])
            dve(lambda e: e.tensor_scalar(vE[s2], ssE[s2], 1.0 / D, EPS, ALU.mult, ALU.add), r=[f"ssE{s2}"], w=[f"vE{s2}"])
            pool(lambda e: e.tensor_tensor(rsE[s2], vE[s2], nhalf[:, 0:1], ALU.pow), r=[f"vE{s2}", "nhalf"], w=[f"rsE{s2}"])
            dve(lambda e: e.scalar_tensor_tensor(oE[s2], x2E[s2], rsE[s2], fg, ALU.mult, ALU.mult), r=[f"x2E{s2}", f"rsE{s2}", "fg"], w=[f"oE{s2}"])
            dma("sp", out_d[i * 128:(i + 1) * 128, :], oE[s2], r=[f"oE{s2}"], w=[f"out_d_{S.uid()}"], key=f"oE{s2}")

        for s_ in range(NT + 1):
            if s_ < NT:
                stageE1(s_)
            if s_ >= 1:
                stageE2(s_ - 1)
        return finish()


def _bc(v, n=128):
    v = np.asarray(v, dtype=np.float32).reshape(1, -1)
    return np.ascontiguousarray(np.broadcast_to(v, (n, v.shape[1])))


def core_inputs(I, b):
    f = np.float32
    invf = (np.float32(500000.0) ** (-np.arange(8, dtype=np.float32) / np.float32(8))).astype(f)
    return {
        "x": np.ascontiguousarray(I["x"][b]),
        "pos": np.ascontiguousarray(I["positions"][b].reshape(NT, 128).T.astype(np.int32)),
        "invf": _bc(invf),
        "g_mix": np.ascontiguousarray(I["norm_mix_g"][0].reshape(8, 128).T),
        "w_in": I["w_in"][0],
        "lng_bc": _bc(I["gm_ln_g"][0].reshape(-1)),
        "lnb_bc": _bc(I["gm_ln_b"][0].reshape(-1)),
        "wsT": np.ascontiguousarray(I["gm_w_s"][0].transpose(2, 0, 1)),
        "bs": np.ascontiguousarray(I["gm_b_s"][0].T),
        "lamv": np.ascontiguousarray(np.broadcast_to(
            np.stack([I["lam_q1"][0], I["lam_k1"][0], I["lam_q2"][0], I["lam_k2"][0]])[None], (128, 4, 64))).astype(f),
        "subg_col": np.ascontiguousarray(I["da_subln_g"][0].reshape(128, 1).astype(np.float32)),
        "w_br_a": I["w_br_a"][0],
        "w_br_b": I["w_br_b"][0],
        "w_out": I["w_out"][0],
        "g_ffn_bc": _bc(I["norm_ffn_g"][0]),
        "w_r": np.ascontiguousarray(np.concatenate([I["w_router_group"][0], I["w_router_expert"][0]], axis=1)),
        "b_r": _bc(np.concatenate([I["b_router_group"][0], I["b_router_expert"][0]])),
        "w1": I["w_exp_gate"][0],
        "w3": I["w_exp_up"][0],
        "w2": I["w_exp_down"][0],
        "fg_bc": _bc(I["final_norm_g"]),
    }


_CACHE = {}


def kernel(**inputs):
    I = {k: np.asarray(v) for k, v in inputs.items()}
    if "nc" not in _CACHE:
        _CACHE["nc"] = build_program()
    nc = _CACHE["nc"]
    in_maps = [core_inputs(I, b) for b in range(8)]
    res = run_bass_kernel_spmd(nc, in_maps, core_ids=list(range(8)))
    return np.stack([np.asarray(r["out"], dtype=np.float32) for r in res.results], axis=0)
```

```python
import math
from contextlib import ExitStack

import numpy as np
import concourse.bass as bass
import concourse.mybir as mybir
from concourse.bass_utils import run_bass_kernel_spmd

F32 = mybir.dt.float32
BF16 = mybir.dt.bfloat16
I32 = mybir.dt.int32
U32 = mybir.dt.uint32
ALU = mybir.AluOpType
AF = mybir.ActivationFunctionType
AX = mybir.AxisListType

ENGS = ("pe", "act", "dve", "pool", "sp")
import os as _os
NOSELF = tuple(x for x in _os.environ.get('KNOSELF', '').split(',') if x)


class _Op:
    __slots__ = ("eng", "fn", "deps", "semkey", "sigval", "needs_sig", "is_dma", "idx")

    def __init__(self, eng, fn, semkey, is_dma):
        self.eng = eng
        self.fn = fn
        self.deps = {}
        self.semkey = semkey
        self.sigval = None
        self.needs_sig = is_dma
        self.is_dma = is_dma


class Sched:
    def __init__(self, nc, stack):
        self.nc = nc
        self.stack = stack
        self.ops = {e: [] for e in ENGS}
        self.lastw = {}
        self.readers = {}
        self.sems = {}
        self.semcount = {}
        self.all_ops = []
        self.keep_prefix = None
        self._uid = 0

    def uid(self):
        self._uid += 1
        return self._uid

    def _sem(self, key):
        if key not in self.sems:
            name = "s_" + str(key).replace(" ", "").replace("(", "").replace(")", "").replace(",", "_").replace("'", "")
            self.sems[key] = self.stack.enter_context(self.nc.semaphore(name[:40]))
            self.semcount[key] = 0
        return self.sems[key]

    def begin_record(self):
        self._rec = []

    def end_record(self):
        r, self._rec = self._rec, None
        return r

    def add(self, eng, fn, reads=(), writes=(), dma=None):
        if getattr(self, "_rec", None) is not None:
            self._rec.append((eng, fn, tuple(reads), tuple(writes), dma))
            return None
        is_dma = dma is not None
        semkey = ("dma", dma) if is_dma else ("eng", eng)
        op = _Op(eng, fn, semkey, is_dma)
        self._sem(semkey)
        deps = {}

        def dep_on(o):
            if o is None or o is op:
                return
            if (not o.is_dma) and o.eng == "pe" and eng == "pe" and not is_dma:
                return
            if NOSELF and (not o.is_dma) and (not is_dma) and o.eng == eng and eng in NOSELF:
                return
            cur = deps.get(o.semkey)
            if cur is None or cur.idx < o.idx:
                deps[o.semkey] = o

        for r in reads:
            dep_on(self.lastw.get(r))
        for r in writes:
            dep_on(self.lastw.get(r))
            for o in self.readers.get(r, {}).values():
                dep_on(o)
        op.idx = len(self.all_ops)
        self.all_ops.append(op)
        for r in reads:
            self.readers.setdefault(r, {})[semkey] = op
        for r in writes:
            self.lastw[r] = op
            self.readers[r] = {}
        for o in deps.values():
            o.needs_sig = True
        op.deps = deps
        self.ops[eng].append(op)
        return op

    def barrier(self):
        tok = ("__barrier__",)
        last = []
        for e in ENGS:
            for o in reversed(self.ops[e]):
                if o.fn is not None:
                    last.append(o)
                    break
        lastdma = {}
        for o in self.all_ops:
            if o.is_dma:
                lastdma[o.semkey] = o
        keep_res = {r: o for r, o in self.lastw.items() if r.startswith(self.keep_prefix)} if self.keep_prefix else {}
        excl = {o.semkey for o in keep_res.values()}
        every = {o.semkey: o for o in last if not o.is_dma}
        every.update({k: o for k, o in lastdma.items() if k not in excl})
        for e in ENGS:
            op = _Op(e, None, ("eng", e), False)
            op.idx = len(self.all_ops)
            self.all_ops.append(op)
            op.deps = {k: o for k, o in every.items() if not (k == ("eng", "pe") and e == "pe")}
            for o in op.deps.values():
                o.needs_sig = True
            self.ops[e].append(op)
        self.lastw = dict(keep_res)
        self.readers = {}

    def finalize_and_emit(self, final_waits=()):
        nc = self.nc
        for o in self.all_ops:
            if o.fn is None:
                continue
            if o.needs_sig:
                inc = 16 if o.is_dma else 1
                self.semcount[o.semkey] += inc
                o.sigval = self.semcount[o.semkey]
        engmap = {"pe": "tensor", "act": "scalar", "dve": "vector", "pool": "gpsimd", "sp": "sync"}
        final = [(self.sems[o.semkey], o.sigval) for o in final_waits]

        def run(engname, eng):
            known = {}
            for o in self.ops[engname]:
                for k, d in o.deps.items():
                    v = d.sigval
                    assert v is not None, (k, d.eng)
                    if known.get(k, 0) >= v:
                        continue
                    known[k] = v
                    eng.wait_ge(self.sems[k], v)
                if o.fn is None:
                    continue
                inst = o.fn(eng)
                if o.needs_sig:
                    assert inst is not None
                    inst.then_inc(self.sems[o.semkey], 16 if o.is_dma else 1)
            if engname == "sp":
                for s, v in final:
                    eng.wait_ge(s, v)

        with nc.Block() as block:
            for engname in ENGS:
                getattr(block, engmap[engname])(lambda eng, _n=engname: run(_n, eng))


D = 1024
T = 8192
NT = T // 128
NC_IN = 2560
NEXP = 32
CAP = 1024
NBLK = CAP // 128
NSLOT = NEXP * CAP
EPS = 1e-6
LAM_INIT = 0.8 - 0.6 * math.exp(0.0)
TWO_PI = 2.0 * math.pi
C1 = 6.28125
C2 = TWO_PI - C1


INPUT_NAMES = []
import os
STAGES = os.environ.get('KSTAGES', '123')


class Arena:
    def __init__(self, nc, st, nbytes):
        self.t = st.enter_context(nc.sbuf_tensor("arena", [128, nbytes // 4], F32))
        self.off = 0
        self.cap = nbytes

    def _take(self, nbytes):
        nbytes = (nbytes + 31) // 32 * 32
        o = self.off
        self.off += nbytes
        assert self.off <= self.cap, ("SBUF arena overflow", self.off, self.cap)
        return o

    def f32(self, n):
        o = self._take(n * 4)
        return self.t[:, o // 4:o // 4 + n]

    def i32(self, n):
        return self.f32(n).bitcast(I32)

    def bf16(self, n):
        n2 = (n + 1) // 2 * 2
        o = self._take(n2 * 2)
        return self.t[:, o // 4:o // 4 + n2 // 2].bitcast(BF16)[:, 0:n]

    def mark(self):
        return self.off

    def release(self, m):
        self.off = m


def build_program(debug=False, stop_after=None, nt_a=NT, nt_b=NT, nt_c=NT, n_exp=NEXP):
    nc = bass.Bass("TRN2", target_bir_lowering=False)
    okind = "ExternalOutput" if debug else "Internal"

    early = stop_after in ("setup", "win", "A", "B", "C")
    INPUT_NAMES.clear()

    def din(name, shape, dt=F32):
        if early and name in ("w1", "w3", "w2"):
            return None
        INPUT_NAMES.append(name)
        return nc.dram_tensor(name, list(shape), dt, kind="ExternalInput").ap()

    def dscr(name, shape, dt):
        return nc.dram_tensor(name, list(shape), dt, kind=okind).ap()

    x_d = din("x", [T, D])
    pos_d = din("pos", [128, NT], I32)
    invf_d = din("invf", [128, 8])
    gmix_d = din("g_mix", [128, 8])
    win_d = din("w_in", [D, 4608])
    lng_d = din("lng_bc", [128, 512])
    lnb_d = din("lnb_bc", [128, 512])
    wsT_d = din("wsT", [128, 4, 128])
    bs_d = din("bs", [128, 4])
    lamv_d = din("lamv", [128, 4, 64])
    subgc_d = din("subg_col", [128, 1])
    wa_d = din("w_br_a", [512, D])
    wb_d = din("w_br_b", [512, D])
    wo_d = din("w_out", [D, D])
    gffn_d = din("g_ffn_bc", [128, D])
    wr_d = din("w_r", [D, 36])
    br_d = din("b_r", [128, 36])
    w1_d = din("w1", [NEXP, D, 512])
    w3_d = din("w3", [NEXP, D, 512])
    w2_d = din("w2", [NEXP, 512, D])
    fg_d = din("fg_bc", [128, D])
    out_d = nc.dram_tensor("out", [T, D], F32, kind="ExternalOutput").ap()

    qT_d = dscr("qT_s", [128, 4, T], BF16)
    kT_d = dscr("kT_s", [128, 4, T], BF16)
    v_d = dscr("v_s", [128, NT, 4, 130], BF16)
    yaT_d = dscr("yaT_s", [NT, 128, 512], BF16)
    ybT_d = dscr("ybT_s", [NT, 128, 512], BF16)
    x1_d = dscr("x1_s", [T, D], F32)
    xs_d = dscr("xs_s", [NSLOT, D], BF16)
    ys_d = dscr("ys_s", [NSLOT, D], BF16)
    rt_d = dscr("rt_s", [128, NT, 4], F32) if debug else None

    with ExitStack() as st:
        S = Sched(nc, st)
        A = Arena(nc, st, 192 * 1024)
        ps = st.enter_context(nc.psum_tensor("ps", [128, 8, 512], F32))

        def bank(b):
            return ps[:, b, :]

        def finish():
            lastd = {}
            for o in S.all_ops:
                if o.is_dma:
                    lastd[o.semkey] = o
            S.finalize_and_emit(final_waits=list(lastd.values()))
            return nc

        def bankbf(b):
            return ps[:, b, :].bitcast(BF16)

        dve = lambda fn, r=(), w=(): S.add("dve", fn, r, w)
        act = lambda fn, r=(), w=(): S.add("act", fn, r, w)
        pool = lambda fn, r=(), w=(): S.add("pool", fn, r, w)
        pe = lambda fn, r=(), w=(): S.add("pe", fn, r, w)

        def dma(q, out, in_, r=(), w=(), key=None):
            return S.add(q, lambda e: e.dma_start(out=out, in_=in_), r, w, dma=key)

        ident = A.bf16(128)
        identf = A.f32(128)
        ustrict = A.bf16(128)
        onesm = A.bf16(128)
        maskb = A.bf16(128)
        ctmp = A.f32(128)
        nhalf = A.f32(8)
        pool(lambda e: e.memset(nhalf, -0.5), w=["nhalf"])
        pool(lambda e: e.memset(identf, 0.0), w=["identf"])
        pool(lambda e: e.affine_select(identf, identf, [[-1, 128]], ALU.not_equal, 1.0, base=0,
                                       channel_multiplier=1), r=["identf"], w=["identf"])
        dve(lambda e: e.tensor_copy(ident, identf), r=["identf"], w=["ident"])
        pool(lambda e: e.memset(ctmp, 1.0), w=["ctmp"])
        pool(lambda e: e.affine_select(ctmp, ctmp, [[1, 128]], ALU.is_gt, 0.0, base=0,
                                       channel_multiplier=-1), r=["ctmp"], w=["ctmp"])
        dve(lambda e: e.tensor_copy(ustrict, ctmp), r=["ctmp"], w=["ustrict"])
        pool(lambda e: e.memset(ctmp, 0.0), r=["ctmp"], w=["ctmp"])
        pool(lambda e: e.affine_select(ctmp, ctmp, [[1, 128]], ALU.is_ge, -30000.0, base=0,
                                       channel_multiplier=-1), r=["ctmp"], w=["ctmp"])
        dve(lambda e: e.tensor_copy(maskb, ctmp), r=["ctmp"], w=["maskb"])
        dve(lambda e: e.memset(onesm, 1.0), w=["onesm"])

        slots_i = A.i32(NT * 2)
        wts = A.f32(NT * 2)
        slots3 = slots_i.rearrange("p (t k) -> p t k", k=2)
        wts3 = wts.rearrange("p (t k) -> p t k", k=2)
        tokid = A.i32(NT)
        pool(lambda e: e.iota(tokid, [[128, NT]], base=0, channel_multiplier=1), w=["tokid"])
        eoff_i = A.i32(NEXP)
        eoff = A.f32(NEXP)
        pool(lambda e: e.iota(eoff_i, [[CAP, NEXP]], base=0, channel_multiplier=0), w=["eoff_i"])
        dve(lambda e: e.tensor_copy(eoff, eoff_i), r=["eoff_i"], w=["eoff"])
        base_cnt = A.f32(NEXP)
        dve(lambda e: e.memset(base_cnt, 0.0), w=["base_cnt"])
        S.keep_prefix = "xs_zero_"
        ztile = A.bf16(4 * D)
        dve(lambda e: e.memset(ztile, 0.0), w=["ztile"])
        for z_ in range(NSLOT // 512):
            dma("act", xs_d[z_ * 512:(z_ + 1) * 512, :].rearrange("(n p) d -> p n d", p=128),
                ztile.rearrange("p (n d) -> p n d", d=D), r=["ztile"], w=["xs_zero_" + str(z_ % 4)], key=f"zfill{z_ % 4}")

        lamv = A.f32(256)
        lamp = A.f32(128)
        lsum = A.f32(2)
        lam_e = A.f32(2)
        neglam = A.f32(1)
        dma("sp", lamv, lamv_d.rearrange("p a b -> p (a b)"), w=["lamv"], key="lamv")
        lamv3 = lamv.rearrange("p (a b) -> p a b", b=64)
        dve(lambda e: e.tensor_tensor(lamp[:, 0:64], lamv3[:, 0, :], lamv3[:, 1, :], ALU.mult), r=["lamv"], w=["lamp"])
        dve(lambda e: e.tensor_tensor(lamp[:, 64:128], lamv3[:, 2, :], lamv3[:, 3, :], ALU.mult), r=["lamp", "lamv"], w=["lamp"])
        dve(lambda e: e.reduce_sum(lsum, lamp.rearrange("p (a b) -> p a b", b=64), axis=AX.X), r=["lamp"], w=["lsum"])
        act(lambda e: e.activation(lam_e, lsum, AF.Exp), r=["lsum"], w=["lam_e"])
        dve(lambda e: e.scalar_tensor_tensor(neglam, lam_e[:, 1:2], -LAM_INIT, lam_e[:, 0:1], ALU.add, ALU.subtract),
            r=["lam_e"], w=["neglam"])

        cos_t = A.f32(NT * 8)
        sin_t = A.f32(NT * 8)
        m0 = A.mark()
        pos_i = A.i32(NT)
        posf = A.f32(NT)
        invf = A.f32(8)
        ang = A.f32(NT * 8)
        kf = A.f32(NT * 8)
        ki = A.i32(NT * 8)
        rr = A.f32(NT * 8)
        r2 = A.f32(NT * 8)
        msk = A.f32(NT * 8)
        dma("sp", pos_i, pos_d, w=["pos_i"], key="pos_i")
        dma("sp", invf, invf_d, w=["invf"], key="invf")
        dve(lambda e: e.tensor_copy(posf, pos_i), r=["pos_i"], w=["posf"])
        ang3 = ang.rearrange("p (t j) -> p t j", j=8)
        dve(lambda e: e.tensor_tensor(ang3, posf.unsqueeze(2).broadcast_to([128, NT, 8]),
                                      invf.unsqueeze(1).broadcast_to([128, NT, 8]), ALU.mult),
            r=["posf", "invf"], w=["ang"])
        dve(lambda e: e.tensor_scalar(kf, ang, 1.0 / TWO_PI, None, ALU.mult), r=["ang"], w=["kf"])
        dve(lambda e: e.tensor_copy(ki, kf), r=["kf"], w=["ki"])
        dve(lambda e: e.tensor_copy(kf, ki), r=["ki"], w=["kf"])
        dve(lambda e: e.scalar_tensor_tensor(rr, kf, -C1, ang, ALU.mult, ALU.add), r=["kf", "ang"], w=["rr"])
        dve(lambda e: e.scalar_tensor_tensor(rr, kf, -C2, rr, ALU.mult, ALU.add), r=["kf", "rr"], w=["rr"])
        dve(lambda e: e.tensor_scalar(r2, rr, math.pi / 2, None, ALU.add), r=["rr"], w=["r2"])
        dve(lambda e: e.tensor_scalar(msk, r2, math.pi, None, ALU.is_gt), r=["r2"], w=["msk"])
        dve(lambda e: e.scalar_tensor_tensor(r2, msk, -TWO_PI, r2, ALU.mult, ALU.add), r=["msk", "r2"], w=["r2"])
        PI_SAFE = 3.1415925
        dve(lambda e: e.tensor_scalar(rr, rr, PI_SAFE, -PI_SAFE, ALU.min, ALU.max), r=["rr"], w=["rr"])
        dve(lambda e: e.tensor_scalar(r2, r2, PI_SAFE, -PI_SAFE, ALU.min, ALU.max), r=["r2"], w=["r2"])
        act(lambda e: e.activation(sin_t, rr, AF.Sin), r=["rr"], w=["sin_t"])
        act(lambda e: e.activation(cos_t, r2, AF.Sin), r=["r2"], w=["cos_t"])
        if debug:
            dbg_cs = nc.dram_tensor("dbg_cs", [128, 2, NT * 8], F32, kind="ExternalOutput").ap()
            dma("sp", dbg_cs[:, 0, :], cos_t, r=["cos_t"], key="dbgc")
            dma("sp", dbg_cs[:, 1, :], sin_t, r=["sin_t"], key="dbgs")
        S.barrier()
        A.release(m0)
        if stop_after == "setup":
            return finish()
        cos3 = cos_t.rearrange("p (t j) -> p t j", j=8)
        sin3 = sin_t.rearrange("p (t j) -> p t j", j=8)
        mark_phase = A.mark()

        def emit_rstd(ss, vtmp, rstd, n, scale, rname):
            dve(lambda e: e.tensor_scalar(vtmp, ss, scale, EPS, ALU.mult, ALU.add), r=[rname + "ss"], w=[rname + "v"])
            pool(lambda e: e.tensor_tensor(rstd, vtmp, nhalf[:, 0:n], ALU.pow), r=[rname + "v", "nhalf"], w=[rname + "rstd"])

        win = A.bf16(8 * NC_IN)
        win3 = win.rearrange("p (c n) -> p c n", n=NC_IN)
        gmix = A.f32(8)
        dma("sp", gmix, gmix_d, w=["gmix"], key="gmix")
        mA = A.mark()
        wst = [A.f32(NC_IN), A.f32(NC_IN)]
        for c in range(8):
            sl = c % 2
            dma("sp", wst[sl], win_d[c * 128:(c + 1) * 128, 0:NC_IN], w=[f"wst{sl}"], key=f"wst{sl}")
            if c % 2 == 0:
                dve(lambda e, c=c, sl=sl: e.tensor_scalar(win3[:, c, :], wst[sl], gmix[:, c:c + 1], None, ALU.mult),
                    r=[f"wst{sl}", "gmix"], w=[f"win{c}"])
            else:
                act(lambda e, c=c, sl=sl: e.activation(win3[:, c, :], wst[sl], AF.Copy, scale=gmix[:, c:c + 1]),
                    r=[f"wst{sl}", "gmix"], w=[f"win{c}"])
        S.barrier()
        A.release(mA)
        if stop_after == "win":
            dbg_w = nc.dram_tensor("dbg_w", [128, 8 * NC_IN], BF16, kind="ExternalOutput").ap()
            dma("sp", dbg_w, win, r=[f"win{c}" for c in range(8)], key="dbgw")
            return finish()
        winres = [f"win{c}" for c in range(8)]

        lng = A.f32(512)
        lnb = A.f32(512)
        wsTf = A.f32(512)
        wsT = A.bf16(512)
        bs = A.f32(4)
        dma("sp", lng, lng_d, w=["lng"], key="lng")
        dma("sp", lnb, lnb_d, w=["lnb"], key="lnb")
        dma("sp", wsTf, wsT_d.rearrange("p g t -> p (g t)"), w=["wsTf"], key="wsTf")
        dma("sp", bs, bs_d, w=["bs"], key="bs")
        pool(lambda e: e.affine_select(wsTf, wsTf, [[0, 4], [1, 128]], ALU.is_ge, 0.0, base=0,
                                       channel_multiplier=-1), r=["wsTf"], w=["wsTf"])
        dve(lambda e: e.tensor_copy(wsT, wsTf), r=["wsTf"], w=["wsT"])
        wsT3 = wsT.rearrange("p (g t) -> p g t", t=128)

        NXS = 3
        xt = [A.f32(D) for _ in range(NXS)]
        junk = A.bf16(D)
        ssA = [A.f32(1) for _ in range(2)]
        vA = [A.f32(1) for _ in range(2)]
        rsA = [A.f32(1) for _ in range(2)]
        hb = [A.bf16(D) for _ in range(2)]
        hT = [A.bf16(D) for _ in range(2)]
        x2b = [A.f32(512) for _ in range(2)]
        xhb = [A.f32(512) for _ in range(2)]
        gu = [A.f32(512) for _ in range(2)]
        gv = [A.f32(512) for _ in range(2)]
        sq = A.f32(512)
        lst = [A.f32(16) for _ in range(2)]
        vn = A.f32(512)
        vnb = [A.bf16(512) for _ in range(2)]
        qb = [A.bf16(512) for _ in range(3)]
        kb = [A.bf16(512) for _ in range(3)]
        rt = [A.f32(64 * 4) for _ in range(2)]
        vb = [A.bf16(4 * 130) for _ in range(2)]
        yab = [A.bf16(512) for _ in range(2)]
        yaT = [A.bf16(512) for _ in range(2)]
        qT = [A.bf16(512) for _ in range(2)]
        kT = [A.bf16(512) for _ in range(2)]
        for s_ in range(2):
            dve(lambda e, s_=s_: e.memset(vb[s_], 1.0), w=[f"vb{s_}"])

        def stageA0(i):
            xs = i % NXS
            s2 = i % 2
            KA1 = int(os.environ.get("KA1", "9"))
            dma("sp", xt[xs], x_d[i * 128:(i + 1) * 128, :], w=[f"xt{xs}"], key=f"xt{xs}")
            if KA1 < 2: return
            act(lambda e: e.activation(junk, xt[xs], AF.Square, accum_out=ssA[s2]), r=[f"xt{xs}"], w=["junk", f"Ass{s2}"])
            if KA1 < 3: return
            dve(lambda e: e.tensor_scalar(vA[s2], ssA[s2], 1.0 / D, EPS, ALU.mult, ALU.add), r=[f"Ass{s2}"], w=[f"Av{s2}"])
            if KA1 < 4: return
            pool(lambda e: e.tensor_tensor(rsA[s2], vA[s2], nhalf[:, 0:1], ALU.pow), r=[f"Av{s2}", "nhalf"], w=[f"Ars{s2}"])
            if KA1 < 5: return
            dve(lambda e: e.tensor_scalar(hb[s2], xt[xs], rsA[s2], None, ALU.mult), r=[f"xt{xs}", f"Ars{s2}"], w=[f"hb{s2}"])
            if KA1 < 6: return

        def stageA1(i):
            s2 = i % 2
            KA1 = 9

            def tr(e):
                last = None
                for c in range(8):
                    last = e.transpose(bankbf(0)[:, c * 128:(c + 1) * 128], hb[s2][:, c * 128:(c + 1) * 128], ident)
                return last
            pe(tr, r=[f"hb{s2}", "ident"], w=["ps0"])
            if KA1 < 7: return
            act(lambda e: e.copy(hT[s2], bankbf(0)), r=["ps0"], w=[f"hT{s2}"])

        def zmm(i, cg, bk):
            s2 = i % 2
            hT3 = hT[s2].rearrange("p (c t) -> p c t", t=128)

            def mm(e):
                last = None
                for c in range(8):
                    last = e.matmul(bank(bk), hT3[:, c, :], win3[:, c, cg * 512:(cg + 1) * 512],
                                    start=(c == 0), stop=(c == 7))
                return last
            pe(mm, r=[f"hT{s2}"] + winres, w=[f"ps{bk}"])

        def gelu_chain(i, which, bk, outbuf, oname):
            x2 = x2b[which]
            xh = xhb[which]
            n2, nh = f"x2_{which}", f"xh_{which}"
            act(lambda e: e.activation(x2, bank(bk), AF.Square), r=[f"ps{bk}"], w=[n2])
            act(lambda e: e.activation(xh, bank(bk), AF.Copy, scale=0.5), r=[f"ps{bk}"], w=[nh])
            dve(lambda e: e.tensor_scalar(x2, x2, 0.044715, 1.0, ALU.mult, ALU.add), r=[n2], w=[n2])
            dve(lambda e: e.tensor_tensor(x2, x2, xh, ALU.mult), r=[n2, nh], w=[n2])
            act(lambda e: e.activation(x2, x2, AF.Tanh, scale=2.0 * 0.7978845608028654), r=[n2], w=[n2])
            dve(lambda e: e.scalar_tensor_tensor(outbuf, x2, 1.0, xh, ALU.add, ALU.mult), r=[n2, nh], w=[oname])

        def rope(i, bk, dst, dname, tmp):
            z3 = bank(bk).rearrange("p (s d) -> p s d", d=64)
            d3 = dst.rearrange("p (s d) -> p s d", d=64)
            cb = cos3[:, i, :].unsqueeze(1).broadcast_to([128, 8, 8])
            sb = sin3[:, i, :].unsqueeze(1).broadcast_to([128, 8, 8])
            t4 = tmp.rearrange("p (a s j) -> p a s j", a=4, j=8)
            tn = dname + "_rt"
            act(lambda e: e.copy(dst, bank(bk)), r=[f"ps{bk}"], w=[dname])
            dve(lambda e: e.tensor_tensor(t4[:, 0], z3[:, :, 0:8], cb, ALU.mult), r=[f"ps{bk}", "cos_t", dname], w=[tn + "0"])
            dve(lambda e: e.tensor_tensor(t4[:, 1], z3[:, :, 8:16], sb, ALU.mult), r=[f"ps{bk}", "sin_t"], w=[tn + "1"])
            dve(lambda e: e.tensor_tensor(t4[:, 2], z3[:, :, 8:16], cb, ALU.mult), r=[f"ps{bk}", "cos_t"], w=[tn + "2"])
            dve(lambda e: e.tensor_tensor(t4[:, 3], z3[:, :, 0:8], sb, ALU.mult), r=[f"ps{bk}", "sin_t"], w=[tn + "3"])
            dve(lambda e: e.tensor_tensor(d3[:, :, 0:8], t4[:, 0], t4[:, 1], ALU.subtract), r=[tn + "0", tn + "1"], w=[dname])
            dve(lambda e: e.tensor_tensor(d3[:, :, 8:16], t4[:, 2], t4[:, 3], ALU.add), r=[tn + "2", tn + "3"], w=[dname])

        def stageA2(i):
            s2 = i % 2
            zmm(i, 0, 1)
            gelu_chain(i, 0, 1, gu[s2], f"gu{s2}")
            zmm(i, 1, 2)
            gelu_chain(i, 1, 2, gv[s2], f"gv{s2}")
            s3 = i % 3
            zmm(i, 2, 3)
            rope(i, 3, qb[s3], f"qb{s3}", rt[0])
            zmm(i, 3, 1)
            rope(i, 1, kb[s3], f"kb{s3}", rt[1])
            zmm(i, 4, 2)
            vb3 = vb[s2].rearrange("p (h d) -> p h d", d=130)
            act(lambda e: e.copy(vb3[:, :, 0:128], bank(2).rearrange("p (h d) -> p h d", d=128)),
                r=["ps2"], w=[f"vb{s2}"])
            dma("sp", v_d[:, i, :, :], vb3, r=[f"vb{s2}"], w=[f"v_d_{S.uid()}"], key=f"vb{s2}")
            g_ = gv[s2]
            g3 = g_.rearrange("p (g d) -> p g d", d=128)
            L = lst[s2]
            ln = f"lst{s2}"
            dve(lambda e: e.reduce_sum(L[:, 0:4], g3, axis=AX.X), r=[f"gv{s2}"], w=[ln + "s"])
            dve(lambda e: e.tensor_tensor(sq, g_, g_, ALU.mult), r=[f"gv{s2}"], w=["sq"])
            dve(lambda e: e.reduce_sum(L[:, 4:8], sq.rearrange("p (g d) -> p g d", d=128), axis=AX.X), r=["sq"], w=[ln + "q"])
            dve(lambda e: e.tensor_scalar(L[:, 8:12], L[:, 0:4], 1.0 / 128, None, ALU.mult), r=[ln + "s"], w=[ln + "m"])
            dve(lambda e: e.tensor_tensor(L[:, 12:16], L[:, 8:12], L[:, 8:12], ALU.mult), r=[ln + "m"], w=[ln + "v"])
            dve(lambda e: e.scalar_tensor_tensor(L[:, 12:16], L[:, 4:8], 1.0 / 128, L[:, 12:16], ALU.mult, ALU.subtract),
                r=[ln + "q", ln + "v"], w=[ln + "v"])
            dve(lambda e: e.tensor_scalar(L[:, 12:16], L[:, 12:16], EPS, None, ALU.add), r=[ln + "v"], w=[ln + "v"])
            pool(lambda e: e.tensor_tensor(L[:, 4:8], L[:, 12:16], nhalf[:, 0:4], ALU.pow), r=[ln + "v", "nhalf", ln + "q"], w=[ln + "r"])
            for g in range(4):
                dve(lambda e, g=g: e.tensor_scalar(vn[:, g * 128:(g + 1) * 128], g_[:, g * 128:(g + 1) * 128],
                                                   L[:, 8 + g:9 + g], L[:, 4 + g:5 + g], ALU.subtract, ALU.mult),
                    r=[f"gv{s2}", ln + "m", ln + "r"], w=[f"vn{g}"])
            vnr = [f"vn{g}" for g in range(4)]
            dve(lambda e: e.tensor_tensor(vn, vn, lng, ALU.mult), r=vnr + ["lng"], w=vnr)
            dve(lambda e: e.tensor_tensor(vnb[s2], vn, lnb, ALU.add), r=vnr + ["lnb"], w=[f"vnb{s2}"])

        def stageA3(i):
            s2 = i % 2

            def sp_mm(e):
                last = None
                for g in range(4):
                    last = e.matmul(bank(4)[:, g * 128:(g + 1) * 128], wsT3[:, g, :], vnb[s2][:, g * 128:(g + 1) * 128],
                                    start=True, stop=True)
                return last
            KA3 = int(os.environ.get("KA3", "9"))
            pe(sp_mm, r=[f"vnb{s2}", "wsT"], w=["ps4"])
            if KA3 < 2: return
            for g in range(4):
                dve(lambda e, g=g: e.scalar_tensor_tensor(yab[s2][:, g * 128:(g + 1) * 128], bank(4)[:, g * 128:(g + 1) * 128],
                                                          bs[:, g:g + 1], gu[s2][:, g * 128:(g + 1) * 128], ALU.add, ALU.mult),
                    r=["ps4", "bs", f"gu{s2}"], w=[f"yab{s2}_{g}"])

        def stageA4(i):
            s2 = i % 2
            s3 = i % 3
            KA3 = 9
            yres = [f"yab{s2}_{g}" for g in range(4)]

            def tr(src, bk):
                def f(e):
                    last = None
                    for c in range(4):
                        last = e.transpose(bankbf(bk)[:, c * 128:(c + 1) * 128], src[:, c * 128:(c + 1) * 128], ident)
                    return last
                return f
            pe(tr(yab[s2], 5), r=yres + ["ident"], w=["ps5"])
            pe(tr(qb[s3], 6), r=[f"qb{s3}", "ident"], w=["ps6"])
            pe(tr(kb[s3], 7), r=[f"kb{s3}", "ident"], w=["ps7"])
            act(lambda e: e.copy(yaT[s2], bankbf(5)[:, 0:512]), r=["ps5"], w=[f"yaT{s2}"])
            dve(lambda e: e.tensor_copy(qT[s2], bankbf(6)[:, 0:512]), r=["ps6"], w=[f"qT{s2}"])
            act(lambda e: e.copy(kT[s2], bankbf(7)[:, 0:512]), r=["ps7"], w=[f"kT{s2}"])
            if KA3 < 5: return
            dma("sp", yaT_d[i], yaT[s2], r=[f"yaT{s2}"], w=[f"yaT_d_{S.uid()}"], key=f"yaT{s2}")
            dma("sp", qT_d[:, :, i * 128:(i + 1) * 128], qT[s2].rearrange("p (h t) -> p h t", t=128),
                r=[f"qT{s2}"], w=[f"qT_d_{S.uid()}"], key=f"qT{s2}")
            dma("sp", kT_d[:, :, i * 128:(i + 1) * 128], kT[s2].rearrange("p (h t) -> p h t", t=128),
                r=[f"kT{s2}"], w=[f"kT_d_{S.uid()}"], key=f"kT{s2}")

        stagesA = [stageA0, stageA1, stageA2, stageA3, stageA4]
        for s_ in range(nt_a + len(stagesA) - 1):
            lists = []
            for k_, fn in enumerate(stagesA):
                if 0 <= s_ - k_ < nt_a:
                    S.begin_record()
                    fn(s_ - k_)
                    lists.append(S.end_record())
            pos_ = [0] * len(lists)
            while True:
                best, bf = None, 2.0
                for li, L_ in enumerate(lists):
                    if pos_[li] < len(L_):
                        f_ = pos_[li] / len(L_)
                        if f_ < bf:
                            best, bf = li, f_
                if best is None:
                    break
                S.add(*lists[best][pos_[best]])
                pos_[best] += 1
        S.barrier()
        A.release(mark_phase)

        if stop_after == "A":
            return finish()

        KT_sb = A.bf16(4 * T)
        KT3 = KT_sb.rearrange("p (h t) -> p h t", t=T)
        V_sb = A.bf16(NT * 4 * 130)
        V4 = V_sb.rearrange("p (i h d) -> p i h d", h=4, d=130)
        for h in range(4):
            dma("sp", KT3[:, h, :], kT_d[:, h, :], r=["kT_d"], w=[f"KT{h}"], key=f"KTl{h}")
        for c in range(4):
            dma("sp", V4[:, c * 16:(c + 1) * 16], v_d[:, c * 16:(c + 1) * 16], r=["v_d"], w=[f"V{c}"], key=f"Vl{c}")
        subgc = A.f32(1)
        dma("sp", subgc, subgc_d, w=["subgc"], key="subgc")
        dve(lambda e: e.tensor_scalar(subgc, subgc, 1.0 - LAM_INIT, None, ALU.mult), r=["subgc"], w=["subgc"])
        onesf = A.f32(128)
        dve(lambda e: e.memset(onesf, 1.0), w=["onesf"])
        QTs = [A.bf16(4 * 512) for _ in range(2)]
        NPT = 4
        pTall = A.bf16(2 * NPT * 512)
        pT4 = pTall.rearrange("p (m s q) -> p m s q", m=2, s=NPT)
        pT = [[pT4[:, m_, s_, :] for s_ in range(NPT)] for m_ in range(2)]
        racc = [A.f32(512) for _ in range(2)]
        a0B = A.f32(512)
        a1B = A.f32(512)
        l1B = A.f32(512)
        rlb = [A.f32(512) for _ in range(2)]
        t1B = A.f32(512)
        t2B = A.f32(512)
        oB = A.f32(512)
        sqB = A.f32(512)
        v4B = A.f32(4)
        rs4B = A.f32(4)
        RmB = A.f32(512)
        ybT = [A.bf16(512) for _ in range(2)]
        n_st = nt_b // 4
        blocks = [(I_, h, j) for I_ in range(n_st) for h in range(4) for j in range(4 * I_ + 4)]

        def load_q(I_):
            sl = I_ % 2
            dma("sp", QTs[sl].rearrange("p (h t) -> p h t", t=512), qT_d[:, :, I_ * 512:(I_ + 1) * 512],
                r=["qT_d"], w=[f"QT{sl}"], key=f"QT{sl}")

        def emit_qk(n):
            I_, h, j = blocks[n]
            par = n % 2
            qlo = max(0, j - 4 * I_)
            ncol = 512 - qlo * 128
            Q3 = QTs[I_ % 2].rearrange("p (h t) -> p h t", t=512)
            diag = j >= 4 * I_

            def f(e):
                last = None
                for m in range(2):
                    last = e.matmul(bank(2 * m + par)[:, 0:ncol], KT3[m * 64:(m + 1) * 64, h, j * 128:(j + 1) * 128],
                                    Q3[m * 64:(m + 1) * 64, h, qlo * 128:512], start=True, stop=not diag)
                if diag:
                    for m in range(2):
                        last = e.matmul(bank(2 * m + par)[:, 0:128], ident, maskb, start=False, stop=True)
                return last
            pe(f, r=[f"KT{h}", f"QT{I_ % 2}", "ident", "maskb"], w=[f"ps{par}", f"ps{2 + par}"])
            sl_ = n % NPT
            act(lambda e: e.activation(pT4[:, :, sl_, 0:ncol], ps[:, par:par + 3:2, 0:ncol], AF.Exp, scale=0.125),
                r=[f"ps{par}", f"ps{2 + par}"], w=[f"pT0{sl_}", f"pT1{sl_}"])

        def emit_pv(n):
            I_, h, j = blocks[n]
            par = n % NPT
            qlo = max(0, j - 4 * I_)
            ncol = 512 - qlo * 128
            jlast = 4 * I_ + 3

            def f(e):
                last = None
                for m in range(2):
                    last = e.matmul(bank(4 + m)[:, qlo * 128:512], V4[:, j, h, 0:128], pT[m][par][:, 0:ncol],
                                    start=(j == 0), stop=(j == jlast))
                last = e.matmul(bank(6)[:, qlo * 128:512], onesm, pT[1][par][:, 0:ncol], start=(j == 0), stop=(j == jlast))
                return last
            pe(f, r=[f"pT0{par}", f"pT1{par}", f"V{j // 16}", "onesm"], w=["ps4", "ps5", "ps6"])
            rc = racc[(I_ * 4 + h) % 2]
            rn = f"racc{(I_ * 4 + h) % 2}"
            if j == 0:
                dve(lambda e: e.tensor_copy(rc, pT[0][par]), r=[f"pT0{par}"], w=[rn])
            else:
                dve(lambda e: e.tensor_tensor(rc[:, qlo * 128:512], rc[:, qlo * 128:512], pT[0][par][:, 0:ncol], ALU.add),
                    r=[f"pT0{par}", rn], w=[rn])
            if j == jlast:
                offs = [0, 1, 2, 4, 6, 9, 10, 12, 13] if I_ >= 3 else ([0, 1, 2, 3, 4, 5, 6, 7, 8] if I_ == 2 else [0] * 9)
                for k_, fn in enumerate(head_steps(I_, h)):
                    pending.append((n + offs[k_], fn))

        def head_steps(I_, h):
            sl = (I_ * 4 + h) % 2
            rc = racc[(I_ * 4 + h) % 2]
            rn = f"racc{(I_ * 4 + h) % 2}"

            def s0():
                dve(lambda e: e.tensor_copy(a1B, bank(5)), r=["ps5"], w=["a1B"])
                dve(lambda e: e.tensor_copy(a0B, bank(4)), r=["ps4"], w=["a0B"])
                dve(lambda e: e.tensor_copy(l1B, bank(6)), r=["ps6"], w=["l1B"])

            def s1():
                pe(lambda e: e.matmul(bank(7), onesf, rc, start=True, stop=True), r=["onesf", rn], w=["ps7"])

            def s2a():
                dve(lambda e: e.reciprocal(rlb[1], l1B), r=["l1B"], w=["rlb1"])

            def s2b():
                dve(lambda e: e.reciprocal(rlb[0], bank(7)), r=["ps7"], w=["rlb0"])

            def s2():
                dve(lambda e: e.tensor_tensor(t2B, a1B, rlb[1], ALU.mult), r=["a1B", "rlb1"], w=["t2B"])
                dve(lambda e: e.tensor_tensor(t1B, a0B, rlb[0], ALU.mult), r=["a0B", "rlb0"], w=["t1B"])
                dve(lambda e: e.scalar_tensor_tensor(oB, t2B, neglam, t1B, ALU.mult, ALU.add), r=["t1B", "t2B", "neglam"], w=["oB"])
                dve(lambda e: e.tensor_tensor(sqB, oB, oB, ALU.mult), r=["oB"], w=["sqB"])

            def s3():
                def ssq_mm(e):
                    last = None
                    for r_ in range(4):
                        last = e.matmul(bank(7)[:, r_:r_ + 1], sqB[:, r_ * 128:(r_ + 1) * 128], onesf[:, 0:1], start=True, stop=True)
                    return last
                pe(ssq_mm, r=["sqB", "onesf"], w=["ps7"])

            def s4():
                dve(lambda e: e.tensor_scalar(v4B, bank(7)[:, 0:4], 1.0 / 128, EPS, ALU.mult, ALU.add), r=["ps7"], w=["v4B"])
                pool(lambda e: e.tensor_tensor(rs4B, v4B, nhalf[:, 0:4], ALU.pow), r=["v4B", "nhalf"], w=["rs4B"])
                for r_ in range(4):
                    dve(lambda e, r_=r_: e.tensor_scalar(RmB[:, r_ * 128:(r_ + 1) * 128], identf, rs4B[:, r_:r_ + 1], None, ALU.mult),
                        r=["rs4B", "identf"], w=[f"RmB{r_}"])

            def s5():
                def bc_mm(e):
                    last = None
                    for r_ in range(4):
                        last = e.matmul(bank(7)[:, r_ * 128:(r_ + 1) * 128], onesf, RmB[:, r_ * 128:(r_ + 1) * 128], start=True, stop=True)
                    return last
                pe(bc_mm, r=[f"RmB{r_}" for r_ in range(4)] + ["onesf"], w=["ps7"])

            def s6():
                dve(lambda e: e.scalar_tensor_tensor(ybT[sl], oB, subgc, bank(7), ALU.mult, ALU.mult), r=["oB", "subgc", "ps7"], w=[f"ybT{sl}"])
                dma("sp", ybT_d[4 * I_:4 * I_ + 4, :, h * 128:(h + 1) * 128].rearrange("r p t -> p r t"),
                    ybT[sl].rearrange("p (r t) -> p r t", t=128), r=[f"ybT{sl}"], w=[f"ybT_d_{S.uid()}"], key=f"ybT{sl}")
            return [s0, s1, s2a, s2b, s2, s3, s4, s5, s6]

        def warmup(nmm, bk):
            def f(e):
                last = None
                for _ in range(nmm):
                    last = e.matmul(bank(bk), ident, KT3[:, 0, 0:512], start=True, stop=True)
                return last
            pe(f, r=["ident", "KT0"], w=[f"ps{bk}"])

        pending = []
        if n_st > 0:
            load_q(0)
        for n in range(len(blocks) + 16):
            if n < len(blocks):
                I_, h, j = blocks[n]
                if h == 0 and j == 0:
                    if I_ + 1 < n_st:
                        load_q(I_ + 1)
                    warmup(20, n % 2)
                emit_qk(n)
            if 1 <= n <= len(blocks):
                emit_pv(n - 1)
            due = [p for p in pending if p[0] <= n - 1]
            pending[:] = [p for p in pending if p[0] > n - 1]
            for _, fn in due:
                fn()
        assert not pending
        S.barrier()
        A.release(mark_phase)
        if stop_after == "B":
            return finish()

        wg = A.bf16(8 * 2048)
        wg3 = wg.rearrange("p (c n) -> p c n", n=2048)
        wa = A.bf16(4 * 1024)
        wa3 = wa.rearrange("p (c n) -> p c n", n=1024)
        wb = A.bf16(4 * 1024)
        wb3 = wb.rearrange("p (c n) -> p c n", n=1024)
        wo = A.bf16(8 * 1024)
        wo3 = wo.rearrange("p (c n) -> p c n", n=1024)
        wr = A.f32(8 * 36)
        wr3 = wr.rearrange("p (c n) -> p c n", n=36)
        gffn = A.f32(D)
        brt = A.f32(36)
        gmixC = A.f32(8)
        dma("sp", gmixC, gmix_d, w=["gmixC"], key="gmixC")
        dma("sp", gffn, gffn_d, w=["gffn"], key="gffn")
        dma("sp", brt, br_d, w=["brt"], key="brt")
        dma("sp", wr3, wr_d.rearrange("(c p) n -> p c n", p=128), w=["wr"], key="wr")
        mC = A.mark()
        stg = [A.f32(2048), A.f32(2048)]
        nld = [0]

        def wload(src, ncol, dst, scale_ap=None, scale_f=None, dname=None):
            sl = nld[0] % 2
            nld[0] += 1
            dma("sp", stg[sl][:, 0:ncol], src, w=[f"stg{sl}"], key=f"stg{sl}")
            if sl == 0:
                if scale_ap is not None:
                    dve(lambda e: e.tensor_scalar(dst, stg[sl][:, 0:ncol], scale_ap, None, ALU.mult), r=[f"stg{sl}", "gmixC"], w=[dname])
                elif scale_f is not None:
                    dve(lambda e: e.tensor_scalar(dst, stg[sl][:, 0:ncol], scale_f, None, ALU.mult), r=[f"stg{sl}"], w=[dname])
                else:
                    dve(lambda e: e.tensor_copy(dst, stg[sl][:, 0:ncol]), r=[f"stg{sl}"], w=[dname])
            else:
                sc = scale_ap if scale_ap is not None else (scale_f if scale_f is not None else 1.0)
                act(lambda e: e.activation(dst, stg[sl][:, 0:ncol], AF.Copy, scale=sc), r=[f"stg{sl}", "gmixC"], w=[dname])
        for c in range(8):
            wload(win_d[c * 128:(c + 1) * 128, NC_IN:4608], 2048, wg3[:, c, :], scale_ap=gmixC[:, c:c + 1], dname=f"wg{c}")
        for c in range(4):
            wload(wa_d[c * 128:(c + 1) * 128, :], 1024, wa3[:, c, :], dname=f"wa{c}")
            wload(wb_d[c * 128:(c + 1) * 128, :], 1024, wb3[:, c, :], dname=f"wb{c}")
        for c in range(8):
            wload(wo_d[c * 128:(c + 1) * 128, :], 1024, wo3[:, c, :], scale_f=0.5, dname=f"wo{c}")
        S.barrier()
        A.release(mC)
        wgres = [f"wg{c}" for c in range(8)]

        xtC = [A.f32(D) for _ in range(5)]
        junkC = A.bf16(D)
        ssC = [A.f32(1) for _ in range(2)]
        vC = [A.f32(1) for _ in range(2)]
        rsC = [A.f32(1) for _ in range(2)]
        hbC = [A.bf16(D) for _ in range(2)]
        hTC = [A.bf16(D) for _ in range(2)]
        yaL = [A.bf16(512) for _ in range(3)]
        ybL = [A.bf16(512) for _ in range(3)]
        th = A.f32(2048)
        m1 = A.f32(D)
        m2 = A.f32(D)
        mbs = [A.bf16(D) for _ in range(2)]
        mTs = [A.bf16(D) for _ in range(2)]
        x1t = [A.f32(D) for _ in range(2)]
        ss2 = A.f32(1)
        v2 = A.f32(1)
        rs2 = A.f32(1)
        h2fs = [A.f32(D) for _ in range(2)]
        h2b = [A.bf16(D) for _ in range(4)]
        h2Ts = [A.f32(D) for _ in range(2)]
        Lg = A.f32(36)
        sm = A.f32(16)
        goh = A.f32(4)
        gex = A.f32(4)
        pen = A.f32(4)
        elm = A.f32(32)
        top8 = A.f32(8)
        oh1s = [A.f32(32) for _ in range(2)]
        oh2s = [A.f32(32) for _ in range(2)]
        Mbs = [A.bf16(32) for _ in range(2)]
        posC = A.f32(32)
        tmp32 = A.f32(32)

        def stageC0(i):
            xs = i % 5
            s2 = i % 2
            s3 = i % 3
            dma("sp", xtC[xs], x_d[i * 128:(i + 1) * 128, :], w=[f"xtC{xs}"], key=f"xtC{xs}")
            dma("sp", yaL[s3], yaT_d[i], r=["yaT_d"], w=[f"yaL{s3}"], key=f"yaL{s3}")
            dma("sp", ybL[s3], ybT_d[i], r=["ybT_d"], w=[f"ybL{s3}"], key=f"ybL{s3}")
            act(lambda e: e.activation(junkC, xtC[xs], AF.Square, accum_out=ssC[s2]), r=[f"xtC{xs}"], w=["junkC", f"Css{s2}"])
            dve(lambda e: e.tensor_scalar(vC[s2], ssC[s2], 1.0 / D, EPS, ALU.mult, ALU.add), r=[f"Css{s2}"], w=[f"Cv{s2}"])
            pool(lambda e: e.tensor_tensor(rsC[s2], vC[s2], nhalf[:, 0:1], ALU.pow), r=[f"Cv{s2}", "nhalf"], w=[f"Crs{s2}"])
            dve(lambda e: e.tensor_scalar(hbC[s2], xtC[xs], rsC[s2], None, ALU.mult), r=[f"xtC{xs}", f"Crs{s2}"], w=[f"hbC{s2}"])

        def stageC1(i):
            s2 = i % 2

            def tr(e):
                last = None
                for c in range(8):
                    last = e.transpose(bankbf(0)[:, c * 128:(c + 1) * 128], hbC[s2][:, c * 128:(c + 1) * 128], ident)
                return last
            pe(tr, r=[f"hbC{s2}", "ident"], w=["ps0"])
            act(lambda e: e.copy(hTC[s2], bankbf(0)), r=["ps0"], w=[f"hTC{s2}"])

        def stageC2(i):
            s2 = i % 2
            s3 = i % 3
            hT3 = hTC[s2].rearrange("p (c t) -> p c t", t=128)
            for cg in range(4):
                bk = 1 + cg % 2

                def mm(e, cg=cg, bk=bk):
                    last = None
                    for c in range(8):
                        last = e.matmul(bank(bk), hT3[:, c, :], wg3[:, c, cg * 512:(cg + 1) * 512], start=(c == 0), stop=(c == 7))
                    return last
                pe(mm, r=[f"hTC{s2}"] + wgres, w=[f"ps{bk}"])
                act(lambda e, cg=cg, bk=bk: e.activation(th[:, cg * 512:(cg + 1) * 512], bank(bk), AF.Tanh, scale=0.5),
                    r=[f"ps{bk}"], w=[f"th{cg}"])
            yl3 = yaL[s3].rearrange("p (c t) -> p c t", t=128)
            bl3 = ybL[s3].rearrange("p (c t) -> p c t", t=128)
            for half in range(2):
                def mma(e, half=half):
                    last = None
                    for c in range(4):
                        last = e.matmul(bank(3 + half), yl3[:, c, :], wa3[:, c, half * 512:(half + 1) * 512], start=(c == 0), stop=(c == 3))
                    return last
                pe(mma, r=[f"yaL{s3}"] + [f"wa{c}" for c in range(4)], w=[f"ps{3 + half}"])

                def mmb(e, half=half):
                    last = None
                    for c in range(4):
                        last = e.matmul(bank(5 + half), bl3[:, c, :], wb3[:, c, half * 512:(half + 1) * 512], start=(c == 0), stop=(c == 3))
                    return last
                pe(mmb, r=[f"ybL{s3}"] + [f"wb{c}" for c in range(4)], w=[f"ps{5 + half}"])
            for half in range(2):
                dve(lambda e, half=half: e.scalar_tensor_tensor(m1[:, half * 512:(half + 1) * 512], th[:, half * 512:(half + 1) * 512], 1.0,
                                                                bank(3 + half), ALU.add, ALU.mult),
                    r=[f"th{half}", f"ps{3 + half}"], w=[f"m1{half}"])
                dve(lambda e, half=half: e.scalar_tensor_tensor(m2[:, half * 512:(half + 1) * 512], th[:, 1024 + half * 512:1024 + (half + 1) * 512], 1.0,
                                                                bank(5 + half), ALU.add, ALU.mult),
                    r=[f"th{2 + half}", f"ps{5 + half}"], w=[f"m2{half}"])
            dve(lambda e: e.tensor_tensor(mbs[s2], m1, m2, ALU.add), r=["m10", "m11", "m20", "m21"], w=[f"mb{s2}"])

        def stageC3(i):
            s2 = i % 2

            def trm(e):
                last = None
                for c in range(8):
                    last = e.transpose(bankbf(0)[:, c * 128:(c + 1) * 128], mbs[s2][:, c * 128:(c + 1) * 128], ident)
                return last
            pe(trm, r=[f"mb{s2}", "ident"], w=["ps0"])
            act(lambda e: e.copy(mTs[s2], bankbf(0)), r=["ps0"], w=[f"mT{s2}"])

        def stageC4o(i):
            xs = i % 5
            s2 = i % 2
            s4 = i % 4
            mT3 = mTs[s2].rearrange("p (c t) -> p c t", t=128)
            for half in range(2):
                def mmo(e, half=half):
                    last = None
                    for c in range(8):
                        last = e.matmul(bank(1 + half), mT3[:, c, :], wo3[:, c, half * 512:(half + 1) * 512], start=(c == 0), stop=(c == 7))
                    return last
                pe(mmo, r=[f"mT{s2}"] + [f"wo{c}" for c in range(8)], w=[f"ps{1 + half}"])
                dve(lambda e, half=half: e.tensor_tensor(x1t[s2][:, half * 512:(half + 1) * 512], xtC[xs][:, half * 512:(half + 1) * 512],
                                                         bank(1 + half), ALU.add),
                    r=[f"xtC{xs}", f"ps{1 + half}"], w=[f"x1t{s2}_{half}"])
            x1res = [f"x1t{s2}_0", f"x1t{s2}_1"]
            dma("sp", x1_d[i * 128:(i + 1) * 128, :], x1t[s2], r=x1res, w=[f"x1_d_{S.uid()}"], key=f"x1t{s2}")
            act(lambda e: e.activation(junkC, x1t[s2], AF.Square, accum_out=ss2), r=x1res, w=["junkC", "ss2"])
            dve(lambda e: e.tensor_scalar(v2, ss2, 1.0 / D, EPS, ALU.mult, ALU.add), r=["ss2"], w=["v2"])
            pool(lambda e: e.tensor_tensor(rs2, v2, nhalf[:, 0:1], ALU.pow), r=["v2", "nhalf"], w=["rs2"])
            dve(lambda e: e.scalar_tensor_tensor(h2fs[s2], x1t[s2], rs2, gffn, ALU.mult, ALU.mult), r=x1res + ["rs2", "gffn"], w=[f"h2f{s2}"])
            act(lambda e: e.copy(h2b[s4], h2fs[s2]), r=[f"h2f{s2}"], w=[f"h2b{s4}"])

        def stageC5(i):
            s2 = i % 2
            h2f = h2fs[s2]
            h2T = h2Ts[s2]

            def trr(e):
                last = None
                for c in range(8):
                    last = e.transpose(ps[:, 3 + c // 4, (c % 4) * 128:(c % 4 + 1) * 128], h2f[:, c * 128:(c + 1) * 128], identf)
                return last
            pe(trr, r=[f"h2f{s2}", "identf"], w=["ps3", "ps4"])
            act(lambda e: e.copy(h2T[:, 0:512], bank(3)), r=["ps3"], w=[f"h2Ta{s2}"])
            act(lambda e: e.copy(h2T[:, 512:1024], bank(4)), r=["ps4"], w=[f"h2Tb{s2}"])

        def stageC6(i):
            s2 = i % 2
            oh1, oh2, Mb = oh1s[s2], oh2s[s2], Mbs[s2]
            h2T3 = h2Ts[s2].rearrange("p (c t) -> p c t", t=128)

            def mmr(e):
                last = None
                for c in range(8):
                    last = e.matmul(bank(7)[:, 0:36], h2T3[:, c, :], wr3[:, c, :], start=(c == 0), stop=(c == 7))
                return last
            pe(mmr, r=[f"h2Ta{s2}", f"h2Tb{s2}", "wr"], w=["ps7"])
            dve(lambda e: e.tensor_tensor(Lg, bank(7)[:, 0:36], brt, ALU.add), r=["ps7", "brt"], w=["Lg"])
            dve(lambda e: e.reduce_max(sm[:, 0:1], Lg[:, 0:4], axis=AX.X), r=["Lg"], w=["gmax"])
            dve(lambda e: e.tensor_scalar(goh, Lg[:, 0:4], sm[:, 0:1], None, ALU.is_equal), r=["Lg", "gmax"], w=["goh"])
            dve(lambda e: e.tensor_scalar(sm[:, 1:2], sm[:, 0:1], -1.0, None, ALU.mult), r=["gmax"], w=["negg"])
            act(lambda e: e.activation(gex, Lg[:, 0:4], AF.Exp, bias=sm[:, 1:2], accum_out=sm[:, 2:3]), r=["Lg", "negg"], w=["gex", "gsum"])
            dve(lambda e: e.reciprocal(sm[:, 3:4], sm[:, 2:3]), r=["gsum"], w=["gw"])
            dve(lambda e: e.tensor_scalar(pen, goh, -1.0, 1e30, ALU.add, ALU.mult), r=["goh"], w=["pen"])
            dve(lambda e: e.tensor_tensor(elm.rearrange("p (g e) -> p g e", e=8), Lg[:, 4:36].rearrange("p (g e) -> p g e", e=8),
                                          pen.unsqueeze(2).broadcast_to([128, 4, 8]), ALU.add), r=["Lg", "pen"], w=["elm"])
            dve(lambda e: e.max(top8, elm), r=["elm"], w=["top8"])
            dve(lambda e: e.tensor_scalar(oh1, elm, top8[:, 0:1], None, ALU.is_equal), r=["elm", "top8"], w=[f"oh1_{s2}"])
            dve(lambda e: e.tensor_scalar(oh2, elm, top8[:, 1:2], None, ALU.is_equal), r=["elm", "top8"], w=[f"oh2_{s2}"])
            dve(lambda e: e.tensor_scalar(sm[:, 4:5], top8[:, 0:1], -1.0, None, ALU.mult), r=["top8"], w=["negv1"])
            act(lambda e: e.activation(sm[:, 5:6], top8[:, 1:2], AF.Exp, bias=sm[:, 4:5]), r=["top8", "negv1"], w=["e2"])
            dve(lambda e: e.tensor_scalar(sm[:, 6:7], sm[:, 5:6], 1.0, None, ALU.add), r=["e2"], w=["den"])
            dve(lambda e: e.reciprocal(sm[:, 7:8], sm[:, 6:7]), r=["den"], w=["p1"])
            dve(lambda e: e.tensor_tensor(wts3[:, i, 0:1], sm[:, 7:8], sm[:, 3:4], ALU.mult), r=["p1", "gw"], w=[f"w1_{i}"])
            dve(lambda e: e.tensor_tensor(wts3[:, i, 1:2], wts3[:, i, 0:1], sm[:, 5:6], ALU.mult), r=[f"w1_{i}", "e2"], w=[f"w2_{i}"])
            dve(lambda e: e.tensor_tensor(Mb, oh1, oh2, ALU.add), r=[f"oh1_{s2}", f"oh2_{s2}"], w=[f"Mb_{s2}"])

        def stageC7(i):
            s2 = i % 2
            s4 = i % 4
            oh1, oh2, Mb = oh1s[s2], oh2s[s2], Mbs[s2]
            pe(lambda e: e.matmul(bank(5)[:, 0:32], ustrict, Mb, start=True, stop=True), r=["ustrict", f"Mb_{s2}"], w=["ps5"])
            pe(lambda e: e.matmul(bank(6)[:, 0:32], onesm, Mb, start=True, stop=True), r=["onesm", f"Mb_{s2}"], w=["ps6"])
            dve(lambda e: e.tensor_tensor(posC, bank(5)[:, 0:32], base_cnt, ALU.add), r=["ps5", "base_cnt"], w=["posCr"])
            dve(lambda e: e.tensor_scalar(posC, posC, float(CAP - 1), None, ALU.min), r=["posCr"], w=["posCr"])
            dve(lambda e: e.tensor_tensor(posC, posC, eoff, ALU.add), r=["posCr", "eoff"], w=["posCr"])
            dve(lambda e: e.tensor_tensor(tmp32, posC, oh1, ALU.mult), r=["posCr", f"oh1_{s2}"], w=["tmp32"])
            dve(lambda e: e.reduce_sum(sm[:, 8:9], tmp32, axis=AX.X), r=["tmp32"], w=["s1f"])
            dve(lambda e: e.tensor_tensor(tmp32, posC, oh2, ALU.mult), r=["posCr", f"oh2_{s2}", "tmp32"], w=["tmp32"])
            dve(lambda e: e.reduce_sum(sm[:, 9:10], tmp32, axis=AX.X), r=["tmp32"], w=["s2f"])
            dve(lambda e: e.tensor_copy(slots3[:, i, 0:1], sm[:, 8:9]), r=["s1f"], w=[f"sl1_{i}"])
            dve(lambda e: e.tensor_copy(slots3[:, i, 1:2], sm[:, 9:10]), r=["s2f"], w=[f"sl2_{i}"])
            dve(lambda e: e.tensor_tensor(base_cnt, base_cnt, bank(6)[:, 0:32], ALU.add), r=["ps6", "base_cnt"], w=["base_cnt"])
            for k in range(2):
                S.add("pool", lambda e, k=k: e.indirect_dma_start(
                    out=xs_d, out_offset=bass.IndirectOffsetOnAxis(ap=slots3[:, i, k:k + 1], axis=0),
                    in_=h2b[s4], in_offset=None),
                    reads=[f"sl{k + 1}_{i}", f"h2b{s4}"] + [f"xs_zero_{z}" for z in range(4)], writes=[f"xs_d{k}"], dma=f"scat{k}_{s4}")
            if debug:
                dma("sp", rt_d[:, i, 0:2], wts3[:, i, :], r=[f"w1_{i}", f"w2_{i}"], key="dbgrt")

        stagesC = [stageC0, stageC1, stageC2, stageC3, stageC4o, stageC5, stageC6, stageC7]
        for s_ in range(nt_c + len(stagesC) - 1):
            for k_, fn in enumerate(stagesC):
                if 0 <= s_ - k_ < nt_c:
                    fn(s_ - k_)
        S.barrier()
        A.release(mark_phase)
        if stop_after == "C":
            return finish()

        w1s = A.f32(8 * 512)
        w3s = A.f32(8 * 512)
        w2s = A.f32(4 * 1024)
        w1b = [A.bf16(8 * 512) for _ in range(2)]
        w3b = [A.bf16(8 * 512) for _ in range(2)]
        w2b = [A.bf16(4 * 1024) for _ in range(2)]
        xg = [A.bf16(D) for _ in range(3)]
        XT = [A.bf16(8 * CAP) for _ in range(2)]
        AT = [A.bf16(4 * CAP) for _ in range(2)]
        thD = [A.f32(CAP // 2) for _ in range(2)]
        a1D = [A.f32(CAP // 2) for _ in range(2)]
        ysb = [A.bf16(D) for _ in range(3)]
        HC = CAP // 2

        def load_w(e_):
            for c in range(8):
                dma("sp", w1s[:, c * 512:(c + 1) * 512], w1_d[e_, c * 128:(c + 1) * 128, :], w=[f"w1s{c}"], key="w1s")
                dma("sp", w3s[:, c * 512:(c + 1) * 512], w3_d[e_, c * 128:(c + 1) * 128, :], w=[f"w3s{c}"], key="w3s")
                dma("sp", w2s[:, c * 512:(c + 1) * 512], w2_d[e_, (c // 2) * 128:(c // 2 + 1) * 128, (c % 2) * 512:(c % 2 + 1) * 512],
                    w=[f"w2s{c}"], key="w2s")

        def cast_w_chunk(e_, c):
            sl = e_ % 2
            act(lambda e: e.copy(w1b[sl][:, c * 512:(c + 1) * 512], w1s[:, c * 512:(c + 1) * 512]), r=[f"w1s{cc}" for cc in range(8)], w=[f"w1b{sl}_{c}"])
            dve(lambda e: e.tensor_copy(w3b[sl][:, c * 512:(c + 1) * 512], w3s[:, c * 512:(c + 1) * 512]), r=[f"w3s{cc}" for cc in range(8)], w=[f"w3b{sl}_{c}"])
            dve(lambda e: e.tensor_scalar(w2b[sl][:, c * 512:(c + 1) * 512], w2s[:, c * 512:(c + 1) * 512], 0.5, None, ALU.mult),
                r=[f"w2s{cc}" for cc in range(8)], w=[f"w2b{sl}_{c}"])

        def cast_w(e_):
            for c in range(8):
                cast_w_chunk(e_, c)

        nblk_ct = [0]

        def xblock(e_, b):
            sl = e_ % 2
            XT3 = XT[sl].rearrange("p (c t) -> p c t", t=CAP)
            g = nblk_ct[0] % 3
            nblk_ct[0] += 1
            blk = e_ * NBLK + b
            tb = 0 if b % 2 == 0 else 7
            dma("sp", xg[g], xs_d[blk * 128:(blk + 1) * 128, :], w=[f"xg{g}"], key=f"xg{g}")

            def trx(e):
                last = None
                for c in range(8):
                    last = e.transpose(bankbf(tb)[:, c * 128:(c + 1) * 128], xg[g][:, c * 128:(c + 1) * 128], ident)
                return last
            pe(trx, r=[f"xg{g}", "ident"], w=[f"ps{tb}"])
            if b % 2 == 0:
                act(lambda e: e.copy(XT3[:, :, b * 128:(b + 1) * 128], bankbf(tb).rearrange("p (c t) -> p c t", t=128)),
                    r=[f"ps{tb}"], w=[f"XT{sl}_{b}"])
            else:
                dve(lambda e: e.tensor_copy(XT3[:, :, b * 128:(b + 1) * 128], bankbf(tb).rearrange("p (c t) -> p c t", t=128)),
                    r=[f"ps{tb}"], w=[f"XT{sl}_{b}"])

        def gate_up_step(e_, k):
            sl = e_ % 2
            XT3 = XT[sl].rearrange("p (c t) -> p c t", t=CAP)
            AT3 = AT[sl].rearrange("p (c t) -> p c t", t=CAP)
            w1b3 = w1b[sl].rearrange("p (c n) -> p c n", n=512)
            w3b3 = w3b[sl].rearrange("p (c n) -> p c n", n=512)
            xtres = [f"XT{sl}_{b}" for b in range(NBLK)]
            dc, half = k // 2, k % 2
            bG = 1 + k % 2
            bU = 3 + k % 2
            t2 = k % 2

            def mmg(e):
                last = None
                for c in range(8):
                    last = e.matmul(bank(bG)[:, 0:HC], w1b3[:, c, dc * 128:(dc + 1) * 128], XT3[:, c, half * HC:(half + 1) * HC],
                                    start=(c == 0), stop=(c == 7))
                return last
            pe(mmg, r=xtres + [f"w1b{sl}_{c}" for c in range(8)], w=[f"ps{bG}"])

            def mmu(e):
                last = None
                for c in range(8):
                    last = e.matmul(bank(bU)[:, 0:HC], w3b3[:, c, dc * 128:(dc + 1) * 128], XT3[:, c, half * HC:(half + 1) * HC],
                                    start=(c == 0), stop=(c == 7))
                return last
            pe(mmu, r=xtres + [f"w3b{sl}_{c}" for c in range(8)], w=[f"ps{bU}"])
            act(lambda e: e.activation(thD[t2], bank(bG)[:, 0:HC], AF.Tanh, scale=0.5), r=[f"ps{bG}"], w=[f"thD{t2}"])
            dve(lambda e: e.scalar_tensor_tensor(a1D[t2], thD[t2], 1.0, bank(bG)[:, 0:HC], ALU.add, ALU.mult),
                r=[f"thD{t2}", f"ps{bG}"], w=[f"a1D{t2}"])
            dve(lambda e: e.tensor_tensor(AT3[:, dc, half * HC:(half + 1) * HC], a1D[t2], bank(bU)[:, 0:HC], ALU.mult),
                r=[f"a1D{t2}", f"ps{bU}"], w=[f"AT{sl}_{k}"])

        def down(e_):
            sl = e_ % 2
            AT3 = AT[sl].rearrange("p (c t) -> p c t", t=CAP)
            w2b3 = w2b[sl].rearrange("p (c n) -> p c n", n=1024)
            atres = [f"AT{sl}_{k}" for k in range(8)]
            for b in range(NBLK):
                blk = e_ * NBLK + b
                ysl = blk % 3
                for cg in range(2):
                    def mmy(e, b=b, cg=cg):
                        last = None
                        for dc in range(4):
                            last = e.matmul(bank(5 + cg), AT3[:, dc, b * 128:(b + 1) * 128], w2b3[:, dc, cg * 512:(cg + 1) * 512],
                                            start=(dc == 0), stop=(dc == 3))
                        return last
                    pe(mmy, r=atres + [f"w2b{sl}_{c}" for c in range(8)], w=[f"ps{5 + cg}"])
                    if cg == 0:
                        act(lambda e, ysl=ysl: e.copy(ysb[ysl][:, 0:512], bank(5)), r=["ps5"], w=[f"ysb{ysl}_0"])
                    else:
                        dve(lambda e, ysl=ysl: e.tensor_copy(ysb[ysl][:, 512:1024], bank(6)), r=["ps6"], w=[f"ysb{ysl}_1"])
                dma("sp", ys_d[blk * 128:(blk + 1) * 128, :], ysb[ysl], r=[f"ysb{ysl}_0", f"ysb{ysl}_1"], w=[f"ys_d_{S.uid()}"], key=f"ysb{ysl}")

        assert NBLK == 8
        if n_exp > 0:
            load_w(0)
            cast_w(0)
            if n_exp > 1:
                load_w(1)
            for b in range(NBLK):
                xblock(0, b)
        for e_ in range(n_exp):
            for k in range(8):
                gate_up_step(e_, k)
                if e_ + 1 < n_exp:
                    xblock(e_ + 1, k)
                    cast_w_chunk(e_ + 1, k)
            if e_ + 2 < n_exp:
                load_w(e_ + 2)
            down(e_)
        S.barrier()
        A.release(mark_phase)
        if stop_after == "D":
            return finish()

        fg = A.f32(D)
        dma("sp", fg, fg_d, w=["fg"], key="fg")
        x1L = [A.f32(D) for _ in range(3)]
        y1L = [A.bf16(D) for _ in range(3)]
        y2L = [A.bf16(D) for _ in range(3)]
        tE = [A.f32(D) for _ in range(2)]
        x2E = [A.f32(D) for _ in range(2)]
        junkE = A.bf16(D)
        ssE = [A.f32(1) for _ in range(2)]
        vE = [A.f32(1) for _ in range(2)]
        rsE = [A.f32(1) for _ in range(2)]
        oE = [A.f32(D) for _ in range(2)]

        def stageE1(i):
            s3 = i % 3
            dma("sp", x1L[s3], x1_d[i * 128:(i + 1) * 128, :], r=["x1_d"], w=[f"x1L{s3}"], key=f"x1L{s3}")
            for k, yL in ((0, y1L), (1, y2L)):
                S.add("pool", lambda e, k=k, yL=yL: e.indirect_dma_start(
                    out=yL[s3], out_offset=None, in_=ys_d,
                    in_offset=bass.IndirectOffsetOnAxis(ap=slots3[:, i, k:k + 1], axis=0)),
                    reads=["ys_d", "slots"], writes=[f"y{k}L{s3}"], dma=f"y{k}L{s3}")

        def stageE2(i):
            s3 = i % 3
            s2 = i % 2
            dve(lambda e: e.scalar_tensor_tensor(tE[s2], y1L[s3], wts3[:, i, 0:1], x1L[s3], ALU.mult, ALU.add),
                r=[f"y0L{s3}", f"x1L{s3}", "wts"], w=[f"tE{s2}"])
            dve(lambda e: e.scalar_tensor_tensor(x2E[s2], y2L[s3], wts3[:, i, 1:2], tE[s2], ALU.mult, ALU.add),
                r=[f"y1L{s3}", f"tE{s2}", "wts"], w=[f"x2E{s2}"])
            act(lambda e: e.activation(junkE, x2E[s2], AF.Square, accum_out=ssE[s2]), r=[f"x2E{s2}"], w=["junkE", f"ssE{s2}"])
            dve(lambda e: e.tensor_scalar(vE[s2], ssE[s2], 1.0 / D, EPS, ALU.mult, ALU.add), r=[f"ssE{s2}"], w=[f"vE{s2}"])
            pool(lambda e: e.tensor_tensor(rsE[s2], vE[s2], nhalf[:, 0:1], ALU.pow), r=[f"vE{s2}", "nhalf"], w=[f"rsE{s2}"])
            dve(lambda e: e.scalar_tensor_tensor(oE[s2], x2E[s2], rsE[s2], fg, ALU.mult, ALU.mult), r=[f"x2E{s2}", f"rsE{s2}", "fg"], w=[f"oE{s2}"])
            dma("sp", out_d[i * 128:(i + 1) * 128, :], oE[s2], r=[f"oE{s2}"], w=[f"out_d_{S.uid()}"], key=f"oE{s2}")

        for s_ in range(NT + 1):
            if s_ < NT:
                stageE1(s_)
            if s_ >= 1:
                stageE2(s_ - 1)
        return finish()


def _bc(v, n=128):
    v = np.asarray(v, dtype=np.float32).reshape(1, -1)
    return np.ascontiguousarray(np.broadcast_to(v, (n, v.shape[1])))


def core_inputs(I, b):
    f = np.float32
    invf = (np.float32(500000.0) ** (-np.arange(8, dtype=np.float32) / np.float32(8))).astype(f)
    return {
        "x": np.ascontiguousarray(I["x"][b]),
        "pos": np.ascontiguousarray(I["positions"][b].reshape(NT, 128).T.astype(np.int32)),
        "invf": _bc(invf),
        "g_mix": np.ascontiguousarray(I["norm_mix_g"][0].reshape(8, 128).T),
        "w_in": I["w_in"][0],
        "lng_bc": _bc(I["gm_ln_g"][0].reshape(-1)),
        "lnb_bc": _bc(I["gm_ln_b"][0].reshape(-1)),
        "wsT": np.ascontiguousarray(I["gm_w_s"][0].transpose(2, 0, 1)),
        "bs": np.ascontiguousarray(I["gm_b_s"][0].T),
        "lamv": np.ascontiguousarray(np.broadcast_to(
            np.stack([I["lam_q1"][0], I["lam_k1"][0], I["lam_q2"][0], I["lam_k2"][0]])[None], (128, 4, 64))).astype(f),
        "subg_col": np.ascontiguousarray(I["da_subln_g"][0].reshape(128, 1).astype(np.float32)),
        "w_br_a": I["w_br_a"][0],
        "w_br_b": I["w_br_b"][0],
        "w_out": I["w_out"][0],
        "g_ffn_bc": _bc(I["norm_ffn_g"][0]),
        "w_r": np.ascontiguousarray(np.concatenate([I["w_router_group"][0], I["w_router_expert"][0]], axis=1)),
        "b_r": _bc(np.concatenate([I["b_router_group"][0], I["b_router_expert"][0]])),
        "w1": I["w_exp_gate"][0],
        "w3": I["w_exp_up"][0],
        "w2": I["w_exp_down"][0],
        "fg_bc": _bc(I["final_norm_g"]),
    }


_CACHE = {}


def kernel(**inputs):
    I = {k: np.asarray(v) for k, v in inputs.items()}
    if "nc" not in _CACHE:
        _CACHE["nc"] = build_program()
    nc = _CACHE["nc"]
    in_maps = [core_inputs(I, b) for b in range(8)]
    res = run_bass_kernel_spmd(nc, in_maps, core_ids=list(range(8)))
    return np.stack([np.asarray(r["out"], dtype=np.float32) for r in res.results], axis=0)
```

```python
import math
from contextlib import ExitStack

import numpy as np
import concourse.bass as bass
import concourse.mybir as mybir
from concourse.bass_utils import run_bass_kernel_spmd

F32 = mybir.dt.float32
BF16 = mybir.dt.bfloat16
I32 = mybir.dt.int32
U32 = mybir.dt.uint32
ALU = mybir.AluOpType
AF = mybir.ActivationFunctionType
AX = mybir.AxisListType

ENGS = ("pe", "act", "dve", "pool", "sp")
import os as _os
NOSELF = tuple(x for x in _os.environ.get('KNOSELF', '').split(',') if x)


class _Op:
    __slots__ = ("eng", "fn", "deps", "semkey", "sigval", "needs_sig", "is_dma", "idx")

    def __init__(self, eng, fn, semkey, is_dma):
        self.eng = eng
        self.fn = fn
        self.deps = {}
        self.semkey = semkey
        self.sigval = None
        self.needs_sig = is_dma
        self.is_dma = is_dma


class Sched:
    def __init__(self, nc, stack):
        self.nc = nc
        self.stack = stack
        self.ops = {e: [] for e in ENGS}
        self.lastw = {}
        self.readers = {}
        self.sems = {}
        self.semcount = {}
        self.all_ops = []
        self.keep_prefix = None
        self._uid = 0

    def uid(self):
        self._uid += 1
        return self._uid

    def _sem(self, key):
        if key not in self.sems:
            name = "s_" + str(key).replace(" ", "").replace("(", "").replace(")", "").replace(",", "_").replace("'", "")
            self.sems[key] = self.stack.enter_context(self.nc.semaphore(name[:40]))
            self.semcount[key] = 0
        return self.sems[key]

    def begin_record(self):
        self._rec = []

    def end_record(self):
        r, self._rec = self._rec, None
        return r

    def add(self, eng, fn, reads=(), writes=(), dma=None):
        if getattr(self, "_rec", None) is not None:
            self._rec.append((eng, fn, tuple(reads), tuple(writes), dma))
            return None
        is_dma = dma is not None
        semkey = ("dma", dma) if is_dma else ("eng", eng)
        op = _Op(eng, fn, semkey, is_dma)
        self._sem(semkey)
        deps = {}

        def dep_on(o):
            if o is None or o is op:
                return
            if (not o.is_dma) and o.eng == "pe" and eng == "pe" and not is_dma:
                return
            if NOSELF and (not o.is_dma) and (not is_dma) and o.eng == eng and eng in NOSELF:
                return
            cur = deps.get(o.semkey)
            if cur is None or cur.idx < o.idx:
                deps[o.semkey] = o

        for r in reads:
            dep_on(self.lastw.get(r))
            if r.startswith("ps"):
                for o in self.readers.get(r, {}).values():
                    if o.eng != eng:
                        dep_on(o)
        for r in writes:
            dep_on(self.lastw.get(r))
            for o in self.readers.get(r, {}).values():
                dep_on(o)
        op.idx = len(self.all_ops)
        self.all_ops.append(op)
        for r in reads:
            self.readers.setdefault(r, {})[semkey] = op
        for r in writes:
            self.lastw[r] = op
            self.readers[r] = {}
        for o in deps.values():
            o.needs_sig = True
        op.deps = deps
        self.ops[eng].append(op)
        return op

    def barrier(self):
        tok = ("__barrier__",)
        last = []
        for e in ENGS:
            for o in reversed(self.ops[e]):
                if o.fn is not None:
                    last.append(o)
                    break
        lastdma = {}
        for o in self.all_ops:
            if o.is_dma:
                lastdma[o.semkey] = o
        keep_res = {r: o for r, o in self.lastw.items() if r.startswith(self.keep_prefix)} if self.keep_prefix else {}
        excl = {o.semkey for o in keep_res.values()}
        every = {o.semkey: o for o in last if not o.is_dma}
        every.update({k: o for k, o in lastdma.items() if k not in excl})
        for e in ENGS:
            op = _Op(e, None, ("eng", e), False)
            op.idx = len(self.all_ops)
            self.all_ops.append(op)
            op.deps = {k: o for k, o in every.items() if not (k == ("eng", "pe") and e == "pe")}
            for o in op.deps.values():
                o.needs_sig = True
            self.ops[e].append(op)
        self.lastw = dict(keep_res)
        self.readers = {}

    def finalize_and_emit(self, final_waits=()):
        nc = self.nc
        for o in self.all_ops:
            if o.fn is None:
                continue
            if o.needs_sig:
                inc = 16 if o.is_dma else 1
                self.semcount[o.semkey] += inc
                o.sigval = self.semcount[o.semkey]
        engmap = {"pe": "tensor", "act": "scalar", "dve": "vector", "pool": "gpsimd", "sp": "sync"}
        final = [(self.sems[o.semkey], o.sigval) for o in final_waits]

        def run(engname, eng):
            known = {}
            for o in self.ops[engname]:
                for k, d in o.deps.items():
                    v = d.sigval
                    assert v is not None, (k, d.eng)
                    if known.get(k, 0) >= v:
                        continue
                    known[k] = v
                    eng.wait_ge(self.sems[k], v)
                if o.fn is None:
                    continue
                inst = o.fn(eng)
                if o.needs_sig:
                    assert inst is not None
                    inst.then_inc(self.sems[o.semkey], 16 if o.is_dma else 1)
            if engname == "sp":
                for s, v in final:
                    eng.wait_ge(s, v)

        with nc.Block() as block:
            for engname in ENGS:
                getattr(block, engmap[engname])(lambda eng, _n=engname: run(_n, eng))


D = 1024
T = 8192
NT = T // 128
NC_IN = 2560
NEXP = 32
CAP = 1024
NBLK = CAP // 128
NSLOT = NEXP * CAP
EPS = 1e-6
LAM_INIT = 0.8 - 0.6 * math.exp(0.0)
TWO_PI = 2.0 * math.pi
C1 = 6.28125
C2 = TWO_PI - C1


INPUT_NAMES = []
import os
STAGES = os.environ.get('KSTAGES', '123')


class Arena:
    def __init__(self, nc, st, nbytes):
        self.t = st.enter_context(nc.sbuf_tensor("arena", [128, nbytes // 4], F32))
        self.off = 0
        self.cap = nbytes

    def _take(self, nbytes):
        nbytes = (nbytes + 31) // 32 * 32
        o = self.off
        self.off += nbytes
        assert self.off <= self.cap, ("SBUF arena overflow", self.off, self.cap)
        return o

    def f32(self, n):
        o = self._take(n * 4)
        return self.t[:, o // 4:o // 4 + n]

    def i32(self, n):
        return self.f32(n).bitcast(I32)

    def bf16(self, n):
        n2 = (n + 1) // 2 * 2
        o = self._take(n2 * 2)
        return self.t[:, o // 4:o // 4 + n2 // 2].bitcast(BF16)[:, 0:n]

    def mark(self):
        return self.off

    def release(self, m):
        self.off = m


def build_program(debug=False, stop_after=None, nt_a=NT, nt_b=NT, nt_c=NT, n_exp=NEXP):
    nc = bass.Bass("TRN2", target_bir_lowering=False)
    okind = "ExternalOutput" if debug else "Internal"

    early = stop_after in ("setup", "win", "A", "B", "C")
    INPUT_NAMES.clear()

    def din(name, shape, dt=F32):
        if early and name in ("w1", "w3", "w2"):
            return None
        INPUT_NAMES.append(name)
        return nc.dram_tensor(name, list(shape), dt, kind="ExternalInput").ap()

    def dscr(name, shape, dt):
        return nc.dram_tensor(name, list(shape), dt, kind=okind).ap()

    x_d = din("x", [T, D])
    pos_d = din("pos", [128, NT], I32)
    invf_d = din("invf", [128, 8])
    gmix_d = din("g_mix", [128, 8])
    win_d = din("w_in", [D, 4608])
    lng_d = din("lng_bc", [128, 512])
    lnb_d = din("lnb_bc", [128, 512])
    wsT_d = din("wsT", [128, 4, 128])
    bs_d = din("bs", [128, 4])
    lamv_d = din("lamv", [128, 4, 64])
    subgc_d = din("subg_col", [128, 1])
    wa_d = din("w_br_a", [512, D])
    wb_d = din("w_br_b", [512, D])
    wo_d = din("w_out", [D, D])
    gffn_d = din("g_ffn_bc", [128, D])
    wr_d = din("w_r", [D, 36])
    br_d = din("b_r", [128, 36])
    w1_d = din("w1", [NEXP, D, 512])
    w3_d = din("w3", [NEXP, D, 512])
    w2_d = din("w2", [NEXP, 512, D])
    fg_d = din("fg_bc", [128, D])
    out_d = nc.dram_tensor("out", [T, D], F32, kind="ExternalOutput").ap()

    qT_d = dscr("qT_s", [128, 4, T], BF16)
    kT_d = dscr("kT_s", [128, 4, T], BF16)
    v_d = dscr("v_s", [128, NT, 4, 130], BF16)
    yaT_d = dscr("yaT_s", [NT, 128, 512], BF16)
    ybT_d = dscr("ybT_s", [NT, 128, 512], BF16)
    x1_d = dscr("x1_s", [T, D], F32)
    xs_d = dscr("xs_s", [NSLOT, D], BF16)
    ys_d = dscr("ys_s", [NSLOT, D], BF16)
    rt_d = dscr("rt_s", [128, NT, 4], F32) if debug else None

    with ExitStack() as st:
        S = Sched(nc, st)
        A = Arena(nc, st, 192 * 1024)
        ps = st.enter_context(nc.psum_tensor("ps", [128, 8, 512], F32))

        def bank(b):
            return ps[:, b, :]

        def finish():
            lastd = {}
            for o in S.all_ops:
                if o.is_dma:
                    lastd[o.semkey] = o
            S.finalize_and_emit(final_waits=list(lastd.values()))
            return nc

        def bankbf(b):
            return ps[:, b, :].bitcast(BF16)

        dve = lambda fn, r=(), w=(): S.add("dve", fn, r, w)
        act = lambda fn, r=(), w=(): S.add("act", fn, r, w)
        pool = lambda fn, r=(), w=(): S.add("pool", fn, r, w)
        pe = lambda fn, r=(), w=(): S.add("pe", fn, r, w)

        def dma(q, out, in_, r=(), w=(), key=None):
            return S.add(q, lambda e: e.dma_start(out=out, in_=in_), r, w, dma=key)

        ident = A.bf16(128)
        identf = A.f32(128)
        ustrict = A.bf16(128)
        onesm = A.bf16(128)
        maskb = A.bf16(128)
        ctmp = A.f32(128)
        nhalf = A.f32(8)
        pool(lambda e: e.memset(nhalf, -0.5), w=["nhalf"])
        pool(lambda e: e.memset(identf, 0.0), w=["identf"])
        pool(lambda e: e.affine_select(identf, identf, [[-1, 128]], ALU.not_equal, 1.0, base=0,
                                       channel_multiplier=1), r=["identf"], w=["identf"])
        dve(lambda e: e.tensor_copy(ident, identf), r=["identf"], w=["ident"])
        pool(lambda e: e.memset(ctmp, 1.0), w=["ctmp"])
        pool(lambda e: e.affine_select(ctmp, ctmp, [[1, 128]], ALU.is_gt, 0.0, base=0,
                                       channel_multiplier=-1), r=["ctmp"], w=["ctmp"])
        dve(lambda e: e.tensor_copy(ustrict, ctmp), r=["ctmp"], w=["ustrict"])
        pool(lambda e: e.memset(ctmp, 0.0), r=["ctmp"], w=["ctmp"])
        pool(lambda e: e.affine_select(ctmp, ctmp, [[1, 128]], ALU.is_ge, -30000.0, base=0,
                                       channel_multiplier=-1), r=["ctmp"], w=["ctmp"])
        dve(lambda e: e.tensor_copy(maskb, ctmp), r=["ctmp"], w=["maskb"])
        dve(lambda e: e.memset(onesm, 1.0), w=["onesm"])

        slots_i = A.i32(NT * 2)
        wts = A.f32(NT * 2)
        slots3 = slots_i.rearrange("p (t k) -> p t k", k=2)
        wts3 = wts.rearrange("p (t k) -> p t k", k=2)
        tokid = A.i32(NT)
        pool(lambda e: e.iota(tokid, [[128, NT]], base=0, channel_multiplier=1), w=["tokid"])
        eoff_i = A.i32(NEXP)
        eoff = A.f32(NEXP)
        pool(lambda e: e.iota(eoff_i, [[CAP, NEXP]], base=0, channel_multiplier=0), w=["eoff_i"])
        dve(lambda e: e.tensor_copy(eoff, eoff_i), r=["eoff_i"], w=["eoff"])
        base_cnt = A.f32(NEXP)
        dve(lambda e: e.memset(base_cnt, 0.0), w=["base_cnt"])
        S.keep_prefix = "xs_zero_"
        ztile = A.bf16(4 * D)
        dve(lambda e: e.memset(ztile, 0.0), w=["ztile"])
        for z_ in range(NSLOT // 512):
            dma("act", xs_d[z_ * 512:(z_ + 1) * 512, :].rearrange("(n p) d -> p n d", p=128),
                ztile.rearrange("p (n d) -> p n d", d=D), r=["ztile"], w=["xs_zero_" + str(z_ % 4)], key=f"zfill{z_ % 4}")

        lamv = A.f32(256)
        lamp = A.f32(128)
        lsum = A.f32(2)
        lam_e = A.f32(2)
        neglam = A.f32(1)
        dma("sp", lamv, lamv_d.rearrange("p a b -> p (a b)"), w=["lamv"], key="lamv")
        lamv3 = lamv.rearrange("p (a b) -> p a b", b=64)
        dve(lambda e: e.tensor_tensor(lamp[:, 0:64], lamv3[:, 0, :], lamv3[:, 1, :], ALU.mult), r=["lamv"], w=["lamp"])
        dve(lambda e: e.tensor_tensor(lamp[:, 64:128], lamv3[:, 2, :], lamv3[:, 3, :], ALU.mult), r=["lamp", "lamv"], w=["lamp"])
        dve(lambda e: e.reduce_sum(lsum, lamp.rearrange("p (a b) -> p a b", b=64), axis=AX.X), r=["lamp"], w=["lsum"])
        act(lambda e: e.activation(lam_e, lsum, AF.Exp), r=["lsum"], w=["lam_e"])
        dve(lambda e: e.scalar_tensor_tensor(neglam, lam_e[:, 1:2], -LAM_INIT, lam_e[:, 0:1], ALU.add, ALU.subtract),
            r=["lam_e"], w=["neglam"])

        cos_t = A.f32(NT * 8)
        sin_t = A.f32(NT * 8)
        m0 = A.mark()
        pos_i = A.i32(NT)
        posf = A.f32(NT)
        invf = A.f32(8)
        ang = A.f32(NT * 8)
        kf = A.f32(NT * 8)
        ki = A.i32(NT * 8)
        rr = A.f32(NT * 8)
        r2 = A.f32(NT * 8)
        msk = A.f32(NT * 8)
        dma("sp", pos_i, pos_d, w=["pos_i"], key="pos_i")
        dma("sp", invf, invf_d, w=["invf"], key="invf")
        dve(lambda e: e.tensor_copy(posf, pos_i), r=["pos_i"], w=["posf"])
        ang3 = ang.rearrange("p (t j) -> p t j", j=8)
        dve(lambda e: e.tensor_tensor(ang3, posf.unsqueeze(2).broadcast_to([128, NT, 8]),
                                      invf.unsqueeze(1).broadcast_to([128, NT, 8]), ALU.mult),
            r=["posf", "invf"], w=["ang"])
        dve(lambda e: e.tensor_scalar(kf, ang, 1.0 / TWO_PI, None, ALU.mult), r=["ang"], w=["kf"])
        dve(lambda e: e.tensor_copy(ki, kf), r=["kf"], w=["ki"])
        dve(lambda e: e.tensor_copy(kf, ki), r=["ki"], w=["kf"])
        dve(lambda e: e.scalar_tensor_tensor(rr, kf, -C1, ang, ALU.mult, ALU.add), r=["kf", "ang"], w=["rr"])
        dve(lambda e: e.scalar_tensor_tensor(rr, kf, -C2, rr, ALU.mult, ALU.add), r=["kf", "rr"], w=["rr"])
        dve(lambda e: e.tensor_scalar(r2, rr, math.pi / 2, None, ALU.add), r=["rr"], w=["r2"])
        dve(lambda e: e.tensor_scalar(msk, r2, math.pi, None, ALU.is_gt), r=["r2"], w=["msk"])
        dve(lambda e: e.scalar_tensor_tensor(r2, msk, -TWO_PI, r2, ALU.mult, ALU.add), r=["msk", "r2"], w=["r2"])
        PI_SAFE = 3.1415925
        dve(lambda e: e.tensor_scalar(rr, rr, PI_SAFE, -PI_SAFE, ALU.min, ALU.max), r=["rr"], w=["rr"])
        dve(lambda e: e.tensor_scalar(r2, r2, PI_SAFE, -PI_SAFE, ALU.min, ALU.max), r=["r2"], w=["r2"])
        act(lambda e: e.activation(sin_t, rr, AF.Sin), r=["rr"], w=["sin_t"])
        act(lambda e: e.activation(cos_t, r2, AF.Sin), r=["r2"], w=["cos_t"])
        if debug:
            dbg_cs = nc.dram_tensor("dbg_cs", [128, 2, NT * 8], F32, kind="ExternalOutput").ap()
            dma("sp", dbg_cs[:, 0, :], cos_t, r=["cos_t"], key="dbgc")
            dma("sp", dbg_cs[:, 1, :], sin_t, r=["sin_t"], key="dbgs")
        S.barrier()
        A.release(m0)
        if stop_after == "setup":
            return finish()
        cos3 = cos_t.rearrange("p (t j) -> p t j", j=8)
        sin3 = sin_t.rearrange("p (t j) -> p t j", j=8)
        mark_phase = A.mark()

        def emit_rstd(ss, vtmp, rstd, n, scale, rname):
            dve(lambda e: e.tensor_scalar(vtmp, ss, scale, EPS, ALU.mult, ALU.add), r=[rname + "ss"], w=[rname + "v"])
            pool(lambda e: e.tensor_tensor(rstd, vtmp, nhalf[:, 0:n], ALU.pow), r=[rname + "v", "nhalf"], w=[rname + "rstd"])

        win = A.bf16(8 * NC_IN)
        win3 = win.rearrange("p (c n) -> p c n", n=NC_IN)
        gmix = A.f32(8)
        dma("sp", gmix, gmix_d, w=["gmix"], key="gmix")
        mA = A.mark()
        wst = [A.f32(NC_IN), A.f32(NC_IN)]
        for c in range(8):
            sl = c % 2
            dma("sp", wst[sl], win_d[c * 128:(c + 1) * 128, 0:NC_IN], w=[f"wst{sl}"], key=f"wst{sl}")
            if c % 2 == 0:
                dve(lambda e, c=c, sl=sl: e.tensor_scalar(win3[:, c, :], wst[sl], gmix[:, c:c + 1], None, ALU.mult),
                    r=[f"wst{sl}", "gmix"], w=[f"win{c}"])
            else:
                act(lambda e, c=c, sl=sl: e.activation(win3[:, c, :], wst[sl], AF.Copy, scale=gmix[:, c:c + 1]),
                    r=[f"wst{sl}", "gmix"], w=[f"win{c}"])
        S.barrier()
        A.release(mA)
        if stop_after == "win":
            dbg_w = nc.dram_tensor("dbg_w", [128, 8 * NC_IN], BF16, kind="ExternalOutput").ap()
            dma("sp", dbg_w, win, r=[f"win{c}" for c in range(8)], key="dbgw")
            return finish()
        winres = [f"win{c}" for c in range(8)]

        lng = A.f32(512)
        lnb = A.f32(512)
        wsTf = A.f32(512)
        wsT = A.bf16(512)
        bs = A.f32(4)
        dma("sp", lng, lng_d, w=["lng"], key="lng")
        dma("sp", lnb, lnb_d, w=["lnb"], key="lnb")
        dma("sp", wsTf, wsT_d.rearrange("p g t -> p (g t)"), w=["wsTf"], key="wsTf")
        dma("sp", bs, bs_d, w=["bs"], key="bs")
        pool(lambda e: e.affine_select(wsTf, wsTf, [[0, 4], [1, 128]], ALU.is_ge, 0.0, base=0,
                                       channel_multiplier=-1), r=["wsTf"], w=["wsTf"])
        dve(lambda e: e.tensor_copy(wsT, wsTf), r=["wsTf"], w=["wsT"])
        wsT3 = wsT.rearrange("p (g t) -> p g t", t=128)

        NXS = 3
        xt = [A.f32(D) for _ in range(NXS)]
        junk = A.bf16(D)
        ssA = [A.f32(1) for _ in range(2)]
        vA = [A.f32(1) for _ in range(2)]
        rsA = [A.f32(1) for _ in range(2)]
        hb = [A.bf16(D) for _ in range(2)]
        hT = [A.bf16(D) for _ in range(2)]
        x2b = [A.f32(512) for _ in range(2)]
        xhb = [A.f32(512) for _ in range(2)]
        gu = [A.f32(512) for _ in range(2)]
        gv = [A.f32(512) for _ in range(2)]
        sq = A.f32(512)
        lst = [A.f32(16) for _ in range(2)]
        vn = A.f32(512)
        vnb = [A.bf16(512) for _ in range(2)]
        qb = [A.bf16(512) for _ in range(3)]
        kb = [A.bf16(512) for _ in range(3)]
        rt = [A.f32(64 * 4) for _ in range(2)]
        vb = [A.bf16(4 * 130) for _ in range(2)]
        yab = [A.bf16(512) for _ in range(2)]
        yaT = [A.bf16(512) for _ in range(2)]
        qT = [A.bf16(512) for _ in range(2)]
        kT = [A.bf16(512) for _ in range(2)]
        for s_ in range(2):
            dve(lambda e, s_=s_: e.memset(vb[s_], 1.0), w=[f"vb{s_}"])

        def stageA0(i):
            xs = i % NXS
            s2 = i % 2
            KA1 = int(os.environ.get("KA1", "9"))
            dma("sp", xt[xs], x_d[i * 128:(i + 1) * 128, :], w=[f"xt{xs}"], key=f"xt{xs}")
            if KA1 < 2: return
            act(lambda e: e.activation(junk, xt[xs], AF.Square, accum_out=ssA[s2]), r=[f"xt{xs}"], w=["junk", f"Ass{s2}"])
            if KA1 < 3: return
            dve(lambda e: e.tensor_scalar(vA[s2], ssA[s2], 1.0 / D, EPS, ALU.mult, ALU.add), r=[f"Ass{s2}"], w=[f"Av{s2}"])
            if KA1 < 4: return
            pool(lambda e: e.tensor_tensor(rsA[s2], vA[s2], nhalf[:, 0:1], ALU.pow), r=[f"Av{s2}", "nhalf"], w=[f"Ars{s2}"])
            if KA1 < 5: return
            dve(lambda e: e.tensor_scalar(hb[s2], xt[xs], rsA[s2], None, ALU.mult), r=[f"xt{xs}", f"Ars{s2}"], w=[f"hb{s2}"])
            if KA1 < 6: return

        def stageA1(i):
            s2 = i % 2
            KA1 = 9

            def tr(e):
                last = None
                for c in range(8):
                    last = e.transpose(bankbf(0)[:, c * 128:(c + 1) * 128], hb[s2][:, c * 128:(c + 1) * 128], ident)
                return last
            pe(tr, r=[f"hb{s2}", "ident"], w=["ps0"])
            if KA1 < 7: return
            act(lambda e: e.copy(hT[s2], bankbf(0)), r=["ps0"], w=[f"hT{s2}"])

        def zmm(i, cg, bk):
            s2 = i % 2
            hT3 = hT[s2].rearrange("p (c t) -> p c t", t=128)

            def mm(e):
                last = None
                for c in range(8):
                    last = e.matmul(bank(bk), hT3[:, c, :], win3[:, c, cg * 512:(cg + 1) * 512],
                                    start=(c == 0), stop=(c == 7))
                return last
            pe(mm, r=[f"hT{s2}"] + winres, w=[f"ps{bk}"])

        def gelu_chain(i, which, bk, outbuf, oname):
            x2 = x2b[which]
            xh = xhb[which]
            n2, nh = f"x2_{which}", f"xh_{which}"
            act(lambda e: e.activation(x2, bank(bk), AF.Square), r=[f"ps{bk}"], w=[n2])
            act(lambda e: e.activation(xh, bank(bk), AF.Copy, scale=0.5), r=[f"ps{bk}"], w=[nh])
            dve(lambda e: e.tensor_scalar(x2, x2, 0.044715, 1.0, ALU.mult, ALU.add), r=[n2], w=[n2])
            dve(lambda e: e.tensor_tensor(x2, x2, xh, ALU.mult), r=[n2, nh], w=[n2])
            act(lambda e: e.activation(x2, x2, AF.Tanh, scale=2.0 * 0.7978845608028654), r=[n2], w=[n2])
            dve(lambda e: e.scalar_tensor_tensor(outbuf, x2, 1.0, xh, ALU.add, ALU.mult), r=[n2, nh], w=[oname])

        def rope(i, bk, dst, dname, tmp):
            z3 = bank(bk).rearrange("p (s d) -> p s d", d=64)
            d3 = dst.rearrange("p (s d) -> p s d", d=64)
            cb = cos3[:, i, :].unsqueeze(1).broadcast_to([128, 8, 8])
            sb = sin3[:, i, :].unsqueeze(1).broadcast_to([128, 8, 8])
            t4 = tmp.rearrange("p (a s j) -> p a s j", a=4, j=8)
            tn = dname + "_rt"
            act(lambda e: e.copy(dst, bank(bk)), r=[f"ps{bk}"], w=[dname])
            dve(lambda e: e.tensor_tensor(t4[:, 0], z3[:, :, 0:8], cb, ALU.mult), r=[f"ps{bk}", "cos_t", dname], w=[tn + "0"])
            dve(lambda e: e.tensor_tensor(t4[:, 1], z3[:, :, 8:16], sb, ALU.mult), r=[f"ps{bk}", "sin_t"], w=[tn + "1"])
            dve(lambda e: e.tensor_tensor(t4[:, 2], z3[:, :, 8:16], cb, ALU.mult), r=[f"ps{bk}", "cos_t"], w=[tn + "2"])
            dve(lambda e: e.tensor_tensor(t4[:, 3], z3[:, :, 0:8], sb, ALU.mult), r=[f"ps{bk}", "sin_t"], w=[tn + "3"])
            dve(lambda e: e.tensor_tensor(d3[:, :, 0:8], t4[:, 0], t4[:, 1], ALU.subtract), r=[tn + "0", tn + "1"], w=[dname])
            dve(lambda e: e.tensor_tensor(d3[:, :, 8:16], t4[:, 2], t4[:, 3], ALU.add), r=[tn + "2", tn + "3"], w=[dname])

        def stageA2(i):
            s2 = i % 2
            zmm(i, 0, 1)
            gelu_chain(i, 0, 1, gu[s2], f"gu{s2}")
            zmm(i, 1, 2)
            gelu_chain(i, 1, 2, gv[s2], f"gv{s2}")
            s3 = i % 3
            zmm(i, 2, 3)
            rope(i, 3, qb[s3], f"qb{s3}", rt[0])
            zmm(i, 3, 1)
            rope(i, 1, kb[s3], f"kb{s3}", rt[1])
            zmm(i, 4, 2)
            vb3 = vb[s2].rearrange("p (h d) -> p h d", d=130)
            act(lambda e: e.copy(vb3[:, :, 0:128], bank(2).rearrange("p (h d) -> p h d", d=128)),
                r=["ps2"], w=[f"vb{s2}"])
            dma("sp", v_d[:, i, :, :], vb3, r=[f"vb{s2}"], w=[f"v_d_{S.uid()}"], key=f"vb{s2}")
            g_ = gv[s2]
            g3 = g_.rearrange("p (g d) -> p g d", d=128)
            L = lst[s2]
            ln = f"lst{s2}"
            dve(lambda e: e.reduce_sum(L[:, 0:4], g3, axis=AX.X), r=[f"gv{s2}"], w=[ln + "s"])
            dve(lambda e: e.tensor_tensor(sq, g_, g_, ALU.mult), r=[f"gv{s2}"], w=["sq"])
            dve(lambda e: e.reduce_sum(L[:, 4:8], sq.rearrange("p (g d) -> p g d", d=128), axis=AX.X), r=["sq"], w=[ln + "q"])
            dve(lambda e: e.tensor_scalar(L[:, 8:12], L[:, 0:4], 1.0 / 128, None, ALU.mult), r=[ln + "s"], w=[ln + "m"])
            dve(lambda e: e.tensor_tensor(L[:, 12:16], L[:, 8:12], L[:, 8:12], ALU.mult), r=[ln + "m"], w=[ln + "v"])
            dve(lambda e: e.scalar_tensor_tensor(L[:, 12:16], L[:, 4:8], 1.0 / 128, L[:, 12:16], ALU.mult, ALU.subtract),
                r=[ln + "q", ln + "v"], w=[ln + "v"])
            dve(lambda e: e.tensor_scalar(L[:, 12:16], L[:, 12:16], EPS, None, ALU.add), r=[ln + "v"], w=[ln + "v"])
            pool(lambda e: e.tensor_tensor(L[:, 4:8], L[:, 12:16], nhalf[:, 0:4], ALU.pow), r=[ln + "v", "nhalf", ln + "q"], w=[ln + "r"])
            for g in range(4):
                dve(lambda e, g=g: e.tensor_scalar(vn[:, g * 128:(g + 1) * 128], g_[:, g * 128:(g + 1) * 128],
                                                   L[:, 8 + g:9 + g], L[:, 4 + g:5 + g], ALU.subtract, ALU.mult),
                    r=[f"gv{s2}", ln + "m", ln + "r"], w=[f"vn{g}"])
            vnr = [f"vn{g}" for g in range(4)]
            dve(lambda e: e.tensor_tensor(vn, vn, lng, ALU.mult), r=vnr + ["lng"], w=vnr)
            dve(lambda e: e.tensor_tensor(vnb[s2], vn, lnb, ALU.add), r=vnr + ["lnb"], w=[f"vnb{s2}"])

        def stageA3(i):
            s2 = i % 2

            def sp_mm(e):
                last = None
                for g in range(4):
                    last = e.matmul(bank(4)[:, g * 128:(g + 1) * 128], wsT3[:, g, :], vnb[s2][:, g * 128:(g + 1) * 128],
                                    start=True, stop=True)
                return last
            KA3 = int(os.environ.get("KA3", "9"))
            pe(sp_mm, r=[f"vnb{s2}", "wsT"], w=["ps4"])
            if KA3 < 2: return
            for g in range(4):
                dve(lambda e, g=g: e.scalar_tensor_tensor(yab[s2][:, g * 128:(g + 1) * 128], bank(4)[:, g * 128:(g + 1) * 128],
                                                          bs[:, g:g + 1], gu[s2][:, g * 128:(g + 1) * 128], ALU.add, ALU.mult),
                    r=["ps4", "bs", f"gu{s2}"], w=[f"yab{s2}_{g}"])

        def stageA4(i):
            s2 = i % 2
            s3 = i % 3
            KA3 = 9
            yres = [f"yab{s2}_{g}" for g in range(4)]

            def tr(src, bk):
                def f(e):
                    last = None
                    for c in range(4):
                        last = e.transpose(bankbf(bk)[:, c * 128:(c + 1) * 128], src[:, c * 128:(c + 1) * 128], ident)
                    return last
                return f
            pe(tr(yab[s2], 5), r=yres + ["ident"], w=["ps5"])
            pe(tr(qb[s3], 6), r=[f"qb{s3}", "ident"], w=["ps6"])
            pe(tr(kb[s3], 7), r=[f"kb{s3}", "ident"], w=["ps7"])
            act(lambda e: e.copy(yaT[s2], bankbf(5)[:, 0:512]), r=["ps5"], w=[f"yaT{s2}"])
            dve(lambda e: e.tensor_copy(qT[s2], bankbf(6)[:, 0:512]), r=["ps6"], w=[f"qT{s2}"])
            act(lambda e: e.copy(kT[s2], bankbf(7)[:, 0:512]), r=["ps7"], w=[f"kT{s2}"])
            if KA3 < 5: return
            dma("sp", yaT_d[i], yaT[s2], r=[f"yaT{s2}"], w=[f"yaT_d_{S.uid()}"], key=f"yaT{s2}")
            dma("sp", qT_d[:, :, i * 128:(i + 1) * 128], qT[s2].rearrange("p (h t) -> p h t", t=128),
                r=[f"qT{s2}"], w=[f"qT_d_{S.uid()}"], key=f"qT{s2}")
            dma("sp", kT_d[:, :, i * 128:(i + 1) * 128], kT[s2].rearrange("p (h t) -> p h t", t=128),
                r=[f"kT{s2}"], w=[f"kT_d_{S.uid()}"], key=f"kT{s2}")

        stagesA = [stageA0, stageA1, stageA2, stageA3, stageA4]
        for s_ in range(nt_a + len(stagesA) - 1):
            lists = []
            for k_, fn in enumerate(stagesA):
                if 0 <= s_ - k_ < nt_a:
                    S.begin_record()
                    fn(s_ - k_)
                    lists.append(S.end_record())
            pos_ = [0] * len(lists)
            while True:
                best, bf = None, 2.0
                for li, L_ in enumerate(lists):
                    if pos_[li] < len(L_):
                        f_ = pos_[li] / len(L_)
                        if f_ < bf:
                            best, bf = li, f_
                if best is None:
                    break
                S.add(*lists[best][pos_[best]])
                pos_[best] += 1
        S.barrier()
        A.release(mark_phase)

        if stop_after == "A":
            return finish()

        KT_sb = A.bf16(4 * T)
        KT3 = KT_sb.rearrange("p (h t) -> p h t", t=T)
        V_sb = A.bf16(NT * 4 * 130)
        V4 = V_sb.rearrange("p (i h d) -> p i h d", h=4, d=130)
        for h in range(4):
            dma("sp", KT3[:, h, :], kT_d[:, h, :], r=["kT_d"], w=[f"KT{h}"], key=f"KTl{h}")
        for c in range(4):
            dma("sp", V4[:, c * 16:(c + 1) * 16], v_d[:, c * 16:(c + 1) * 16], r=["v_d"], w=[f"V{c}"], key=f"Vl{c}")
        subgc = A.f32(1)
        dma("sp", subgc, subgc_d, w=["subgc"], key="subgc")
        dve(lambda e: e.tensor_scalar(subgc, subgc, 1.0 - LAM_INIT, None, ALU.mult), r=["subgc"], w=["subgc"])
        onesf = A.f32(128)
        dve(lambda e: e.memset(onesf, 1.0), w=["onesf"])
        QTs = [A.bf16(4 * 512) for _ in range(2)]
        NPT = 4
        pTall = A.bf16(2 * NPT * 512)
        pT4 = pTall.rearrange("p (m s q) -> p m s q", m=2, s=NPT)
        pT = [[pT4[:, m_, s_, :] for s_ in range(NPT)] for m_ in range(2)]
        racc = [A.f32(512) for _ in range(2)]
        a0B = A.f32(512)
        a1B = A.f32(512)
        l1B = A.f32(512)
        rlb = [A.f32(512) for _ in range(2)]
        t1B = A.f32(512)
        t2B = A.f32(512)
        oB = A.f32(512)
        sqB = A.f32(512)
        v4B = A.f32(4)
        rs4B = A.f32(4)
        RmB = A.f32(512)
        ybT = [A.bf16(512) for _ in range(2)]
        n_st = nt_b // 4
        blocks = [(I_, h, j) for I_ in range(n_st) for h in range(4) for j in range(4 * I_ + 4)]

        def load_q(I_):
            sl = I_ % 2
            dma("sp", QTs[sl].rearrange("p (h t) -> p h t", t=512), qT_d[:, :, I_ * 512:(I_ + 1) * 512],
                r=["qT_d"], w=[f"QT{sl}"], key=f"QT{sl}")

        def emit_qk(n):
            I_, h, j = blocks[n]
            par = n % 2
            qlo = max(0, j - 4 * I_)
            ncol = 512 - qlo * 128
            Q3 = QTs[I_ % 2].rearrange("p (h t) -> p h t", t=512)
            diag = j >= 4 * I_

            def f(e):
                last = None
                for m in range(2):
                    last = e.matmul(bank(2 * m + par)[:, 0:ncol], KT3[m * 64:(m + 1) * 64, h, j * 128:(j + 1) * 128],
                                    Q3[m * 64:(m + 1) * 64, h, qlo * 128:512], start=True, stop=not diag)
                if diag:
                    for m in range(2):
                        last = e.matmul(bank(2 * m + par)[:, 0:128], ident, maskb, start=False, stop=True)
                return last
            pe(f, r=[f"KT{h}", f"QT{I_ % 2}", "ident", "maskb"], w=[f"ps{par}", f"ps{2 + par}"])
            sl_ = n % NPT
            act(lambda e: e.activation(pT4[:, :, sl_, 0:ncol], ps[:, par:par + 3:2, 0:ncol], AF.Exp, scale=0.125),
                r=[f"ps{par}", f"ps{2 + par}"], w=[f"pT0{sl_}", f"pT1{sl_}"])

        def emit_pv(n):
            I_, h, j = blocks[n]
            par = n % NPT
            qlo = max(0, j - 4 * I_)
            ncol = 512 - qlo * 128
            jlast = 4 * I_ + 3

            def f(e):
                last = None
                for m in range(2):
                    last = e.matmul(bank(4 + m)[:, qlo * 128:512], V4[:, j, h, 0:128], pT[m][par][:, 0:ncol],
                                    start=(j == 0), stop=(j == jlast))
                last = e.matmul(bank(6)[:, qlo * 128:512], onesm, pT[1][par][:, 0:ncol], start=(j == 0), stop=(j == jlast))
                return last
            pe(f, r=[f"pT0{par}", f"pT1{par}", f"V{j // 16}", "onesm"], w=["ps4", "ps5", "ps6"])
            rc = racc[(I_ * 4 + h) % 2]
            rn = f"racc{(I_ * 4 + h) % 2}"
            if j == 0:
                dve(lambda e: e.tensor_copy(rc, pT[0][par]), r=[f"pT0{par}"], w=[rn])
            else:
                dve(lambda e: e.tensor_tensor(rc[:, qlo * 128:512], rc[:, qlo * 128:512], pT[0][par][:, 0:ncol], ALU.add),
                    r=[f"pT0{par}", rn], w=[rn])
            if j == jlast:
                offs = [0, 1, 2, 4, 6, 9, 10, 12, 13] if I_ >= 3 else ([0, 1, 2, 3, 4, 5, 6, 7, 8] if I_ == 2 else [0] * 9)
                for k_, fn in enumerate(head_steps(I_, h)):
                    pending.append((n + offs[k_], fn))

        def head_steps(I_, h):
            sl = (I_ * 4 + h) % 2
            rc = racc[(I_ * 4 + h) % 2]
            rn = f"racc{(I_ * 4 + h) % 2}"

            def s0():
                dve(lambda e: e.tensor_copy(a1B, bank(5)), r=["ps5"], w=["a1B"])
                dve(lambda e: e.tensor_copy(a0B, bank(4)), r=["ps4"], w=["a0B"])
                dve(lambda e: e.tensor_copy(l1B, bank(6)), r=["ps6"], w=["l1B"])

            def s1():
                pe(lambda e: e.matmul(bank(7), onesf, rc, start=True, stop=True), r=["onesf", rn], w=["ps7"])

            def s2a():
                dve(lambda e: e.reciprocal(rlb[1], l1B), r=["l1B"], w=["rlb1"])

            def s2b():
                dve(lambda e: e.reciprocal(rlb[0], bank(7)), r=["ps7"], w=["rlb0"])

            def s2():
                dve(lambda e: e.tensor_tensor(t2B, a1B, rlb[1], ALU.mult), r=["a1B", "rlb1"], w=["t2B"])
                dve(lambda e: e.tensor_tensor(t1B, a0B, rlb[0], ALU.mult), r=["a0B", "rlb0"], w=["t1B"])
                dve(lambda e: e.scalar_tensor_tensor(oB, t2B, neglam, t1B, ALU.mult, ALU.add), r=["t1B", "t2B", "neglam"], w=["oB"])
                dve(lambda e: e.tensor_tensor(sqB, oB, oB, ALU.mult), r=["oB"], w=["sqB"])

            def s3():
                def ssq_mm(e):
                    last = None
                    for r_ in range(4):
                        last = e.matmul(bank(7)[:, r_:r_ + 1], sqB[:, r_ * 128:(r_ + 1) * 128], onesf[:, 0:1], start=True, stop=True)
                    return last
                pe(ssq_mm, r=["sqB", "onesf"], w=["ps7"])

            def s4():
                dve(lambda e: e.tensor_scalar(v4B, bank(7)[:, 0:4], 1.0 / 128, EPS, ALU.mult, ALU.add), r=["ps7"], w=["v4B"])
                pool(lambda e: e.tensor_tensor(rs4B, v4B, nhalf[:, 0:4], ALU.pow), r=["v4B", "nhalf"], w=["rs4B"])
                for r_ in range(4):
                    dve(lambda e, r_=r_: e.tensor_scalar(RmB[:, r_ * 128:(r_ + 1) * 128], identf, rs4B[:, r_:r_ + 1], None, ALU.mult),
                        r=["rs4B", "identf"], w=[f"RmB{r_}"])

            def s5():
                def bc_mm(e):
                    last = None
                    for r_ in range(4):
                        last = e.matmul(bank(7)[:, r_ * 128:(r_ + 1) * 128], onesf, RmB[:, r_ * 128:(r_ + 1) * 128], start=True, stop=True)
                    return last
                pe(bc_mm, r=[f"RmB{r_}" for r_ in range(4)] + ["onesf"], w=["ps7"])

            def s6():
                dve(lambda e: e.scalar_tensor_tensor(ybT[sl], oB, subgc, bank(7), ALU.mult, ALU.mult), r=["oB", "subgc", "ps7"], w=[f"ybT{sl}"])
                dma("sp", ybT_d[4 * I_:4 * I_ + 4, :, h * 128:(h + 1) * 128].rearrange("r p t -> p r t"),
                    ybT[sl].rearrange("p (r t) -> p r t", t=128), r=[f"ybT{sl}"], w=[f"ybT_d_{S.uid()}"], key=f"ybT{sl}")
            return [s0, s1, s2a, s2b, s2, s3, s4, s5, s6]

        def warmup(nmm, bk):
            def f(e):
                last = None
                for _ in range(nmm):
                    last = e.matmul(bank(bk), ident, KT3[:, 0, 0:512], start=True, stop=True)
                return last
            pe(f, r=["ident", "KT0"], w=[f"ps{bk}"])

        pending = []
        if n_st > 0:
            load_q(0)
        for n in range(len(blocks) + 16):
            if n < len(blocks):
                I_, h, j = blocks[n]
                if h == 0 and j == 0:
                    if I_ + 1 < n_st:
                        load_q(I_ + 1)
                    warmup(20, n % 2)
                emit_qk(n)
            if 1 <= n <= len(blocks):
                emit_pv(n - 1)
            due = [p for p in pending if p[0] <= n - 1]
            pending[:] = [p for p in pending if p[0] > n - 1]
            for _, fn in due:
                fn()
        assert not pending
        S.barrier()
        A.release(mark_phase)
        if stop_after == "B":
            return finish()

        wg = A.bf16(8 * 2048)
        wg3 = wg.rearrange("p (c n) -> p c n", n=2048)
        wa = A.bf16(4 * 1024)
        wa3 = wa.rearrange("p (c n) -> p c n", n=1024)
        wb = A.bf16(4 * 1024)
        wb3 = wb.rearrange("p (c n) -> p c n", n=1024)
        wo = A.bf16(8 * 1024)
        wo3 = wo.rearrange("p (c n) -> p c n", n=1024)
        wr = A.f32(8 * 36)
        wr3 = wr.rearrange("p (c n) -> p c n", n=36)
        gffn = A.f32(D)
        brt = A.f32(36)
        gmixC = A.f32(8)
        dma("sp", gmixC, gmix_d, w=["gmixC"], key="gmixC")
        dma("sp", gffn, gffn_d, w=["gffn"], key="gffn")
        dma("sp", brt, br_d, w=["brt"], key="brt")
        dma("sp", wr3, wr_d.rearrange("(c p) n -> p c n", p=128), w=["wr"], key="wr")
        mC = A.mark()
        stg = [A.f32(2048), A.f32(2048)]
        nld = [0]

        def wload(src, ncol, dst, scale_ap=None, scale_f=None, dname=None):
            sl = nld[0] % 2
            nld[0] += 1
            dma("sp", stg[sl][:, 0:ncol], src, w=[f"stg{sl}"], key=f"stg{sl}")
            if sl == 0:
                if scale_ap is not None:
                    dve(lambda e: e.tensor_scalar(dst, stg[sl][:, 0:ncol], scale_ap, None, ALU.mult), r=[f"stg{sl}", "gmixC"], w=[dname])
                elif scale_f is not None:
                    dve(lambda e: e.tensor_scalar(dst, stg[sl][:, 0:ncol], scale_f, None, ALU.mult), r=[f"stg{sl}"], w=[dname])
                else:
                    dve(lambda e: e.tensor_copy(dst, stg[sl][:, 0:ncol]), r=[f"stg{sl}"], w=[dname])
            else:
                sc = scale_ap if scale_ap is not None else (scale_f if scale_f is not None else 1.0)
                act(lambda e: e.activation(dst, stg[sl][:, 0:ncol], AF.Copy, scale=sc), r=[f"stg{sl}", "gmixC"], w=[dname])
        for c in range(8):
            wload(win_d[c * 128:(c + 1) * 128, NC_IN:4608], 2048, wg3[:, c, :], scale_ap=gmixC[:, c:c + 1], dname=f"wg{c}")
        for c in range(4):
            wload(wa_d[c * 128:(c + 1) * 128, :], 1024, wa3[:, c, :], dname=f"wa{c}")
            wload(wb_d[c * 128:(c + 1) * 128, :], 1024, wb3[:, c, :], dname=f"wb{c}")
        for c in range(8):
            wload(wo_d[c * 128:(c + 1) * 128, :], 1024, wo3[:, c, :], scale_f=0.5, dname=f"wo{c}")
        S.barrier()
        A.release(mC)
        wgres = [f"wg{c}" for c in range(8)]

        xtC = [A.f32(D) for _ in range(5)]
        junkC = A.bf16(D)
        ssC = [A.f32(1) for _ in range(2)]
        vC = [A.f32(1) for _ in range(2)]
        rsC = [A.f32(1) for _ in range(2)]
        hbC = [A.bf16(D) for _ in range(2)]
        hTC = [A.bf16(D) for _ in range(2)]
        yaL = [A.bf16(512) for _ in range(3)]
        ybL = [A.bf16(512) for _ in range(3)]
        th = A.f32(2048)
        m1 = A.f32(D)
        m2 = A.f32(D)
        mbs = [A.bf16(D) for _ in range(2)]
        mTs = [A.bf16(D) for _ in range(2)]
        x1t = [A.f32(D) for _ in range(2)]
        ss2 = A.f32(1)
        v2 = A.f32(1)
        rs2 = A.f32(1)
        h2fs = [A.f32(D) for _ in range(2)]
        h2b = [A.bf16(D) for _ in range(4)]
        h2Ts = [A.f32(D) for _ in range(2)]
        Lg = A.f32(36)
        sm = A.f32(16)
        goh = A.f32(4)
        gex = A.f32(4)
        pen = A.f32(4)
        elm = A.f32(32)
        top8 = A.f32(8)
        oh1s = [A.f32(32) for _ in range(2)]
        oh2s = [A.f32(32) for _ in range(2)]
        Mbs = [A.bf16(32) for _ in range(2)]
        posC = A.f32(32)
        tmp32 = A.f32(32)

        def stageC0(i):
            xs = i % 5
            s2 = i % 2
            s3 = i % 3
            dma("sp", xtC[xs], x_d[i * 128:(i + 1) * 128, :], w=[f"xtC{xs}"], key=f"xtC{xs}")
            dma("sp", yaL[s3], yaT_d[i], r=["yaT_d"], w=[f"yaL{s3}"], key=f"yaL{s3}")
            dma("sp", ybL[s3], ybT_d[i], r=["ybT_d"], w=[f"ybL{s3}"], key=f"ybL{s3}")
            act(lambda e: e.activation(junkC, xtC[xs], AF.Square, accum_out=ssC[s2]), r=[f"xtC{xs}"], w=["junkC", f"Css{s2}"])
            dve(lambda e: e.tensor_scalar(vC[s2], ssC[s2], 1.0 / D, EPS, ALU.mult, ALU.add), r=[f"Css{s2}"], w=[f"Cv{s2}"])
            pool(lambda e: e.tensor_tensor(rsC[s2], vC[s2], nhalf[:, 0:1], ALU.pow), r=[f"Cv{s2}", "nhalf"], w=[f"Crs{s2}"])
            dve(lambda e: e.tensor_scalar(hbC[s2], xtC[xs], rsC[s2], None, ALU.mult), r=[f"xtC{xs}", f"Crs{s2}"], w=[f"hbC{s2}"])

        def stageC1(i):
            s2 = i % 2

            def tr(e):
                last = None
                for c in range(8):
                    last = e.transpose(bankbf(0)[:, c * 128:(c + 1) * 128], hbC[s2][:, c * 128:(c + 1) * 128], ident)
                return last
            pe(tr, r=[f"hbC{s2}", "ident"], w=["ps0"])
            act(lambda e: e.copy(hTC[s2], bankbf(0)), r=["ps0"], w=[f"hTC{s2}"])

        def stageC2(i):
            s2 = i % 2
            s3 = i % 3
            hT3 = hTC[s2].rearrange("p (c t) -> p c t", t=128)
            for cg in range(4):
                bk = 1 + cg % 2

                def mm(e, cg=cg, bk=bk):
                    last = None
                    for c in range(8):
                        last = e.matmul(bank(bk), hT3[:, c, :], wg3[:, c, cg * 512:(cg + 1) * 512], start=(c == 0), stop=(c == 7))
                    return last
                pe(mm, r=[f"hTC{s2}"] + wgres, w=[f"ps{bk}"])
                act(lambda e, cg=cg, bk=bk: e.activation(th[:, cg * 512:(cg + 1) * 512], bank(bk), AF.Tanh, scale=0.5),
                    r=[f"ps{bk}"], w=[f"th{cg}"])
            yl3 = yaL[s3].rearrange("p (c t) -> p c t", t=128)
            bl3 = ybL[s3].rearrange("p (c t) -> p c t", t=128)
            for half in range(2):
                def mma(e, half=half):
                    last = None
                    for c in range(4):
                        last = e.matmul(bank(3 + half), yl3[:, c, :], wa3[:, c, half * 512:(half + 1) * 512], start=(c == 0), stop=(c == 3))
                    return last
                pe(mma, r=[f"yaL{s3}"] + [f"wa{c}" for c in range(4)], w=[f"ps{3 + half}"])

                def mmb(e, half=half):
                    last = None
                    for c in range(4):
                        last = e.matmul(bank(5 + half), bl3[:, c, :], wb3[:, c, half * 512:(half + 1) * 512], start=(c == 0), stop=(c == 3))
                    return last
                pe(mmb, r=[f"ybL{s3}"] + [f"wb{c}" for c in range(4)], w=[f"ps{5 + half}"])
            for half in range(2):
                dve(lambda e, half=half: e.scalar_tensor_tensor(m1[:, half * 512:(half + 1) * 512], th[:, half * 512:(half + 1) * 512], 1.0,
                                                                bank(3 + half), ALU.add, ALU.mult),
                    r=[f"th{half}", f"ps{3 + half}"], w=[f"m1{half}"])
                dve(lambda e, half=half: e.scalar_tensor_tensor(m2[:, half * 512:(half + 1) * 512], th[:, 1024 + half * 512:1024 + (half + 1) * 512], 1.0,
                                                                bank(5 + half), ALU.add, ALU.mult),
                    r=[f"th{2 + half}", f"ps{5 + half}"], w=[f"m2{half}"])
            dve(lambda e: e.tensor_tensor(mbs[s2], m1, m2, ALU.add), r=["m10", "m11", "m20", "m21"], w=[f"mb{s2}"])

        def stageC3(i):
            s2 = i % 2

            def trm(e):
                last = None
                for c in range(8):
                    last = e.transpose(bankbf(0)[:, c * 128:(c + 1) * 128], mbs[s2][:, c * 128:(c + 1) * 128], ident)
                return last
            pe(trm, r=[f"mb{s2}", "ident"], w=["ps0"])
            act(lambda e: e.copy(mTs[s2], bankbf(0)), r=["ps0"], w=[f"mT{s2}"])

        def stageC4o(i):
            xs = i % 5
            s2 = i % 2
            s4 = i % 4
            mT3 = mTs[s2].rearrange("p (c t) -> p c t", t=128)
            for half in range(2):
                def mmo(e, half=half):
                    last = None
                    for c in range(8):
                        last = e.matmul(bank(1 + half), mT3[:, c, :], wo3[:, c, half * 512:(half + 1) * 512], start=(c == 0), stop=(c == 7))
                    return last
                pe(mmo, r=[f"mT{s2}"] + [f"wo{c}" for c in range(8)], w=[f"ps{1 + half}"])
                dve(lambda e, half=half: e.tensor_tensor(x1t[s2][:, half * 512:(half + 1) * 512], xtC[xs][:, half * 512:(half + 1) * 512],
                                                         bank(1 + half), ALU.add),
                    r=[f"xtC{xs}", f"ps{1 + half}"], w=[f"x1t{s2}_{half}"])
            x1res = [f"x1t{s2}_0", f"x1t{s2}_1"]
            dma("sp", x1_d[i * 128:(i + 1) * 128, :], x1t[s2], r=x1res, w=[f"x1_d_{S.uid()}"], key=f"x1t{s2}")
            act(lambda e: e.activation(junkC, x1t[s2], AF.Square, accum_out=ss2), r=x1res, w=["junkC", "ss2"])
            dve(lambda e: e.tensor_scalar(v2, ss2, 1.0 / D, EPS, ALU.mult, ALU.add), r=["ss2"], w=["v2"])
            pool(lambda e: e.tensor_tensor(rs2, v2, nhalf[:, 0:1], ALU.pow), r=["v2", "nhalf"], w=["rs2"])
            dve(lambda e: e.scalar_tensor_tensor(h2fs[s2], x1t[s2], rs2, gffn, ALU.mult, ALU.mult), r=x1res + ["rs2", "gffn"], w=[f"h2f{s2}"])
            act(lambda e: e.copy(h2b[s4], h2fs[s2]), r=[f"h2f{s2}"], w=[f"h2b{s4}"])

        def stageC5(i):
            s2 = i % 2
            h2f = h2fs[s2]
            h2T = h2Ts[s2]

            def trr(e):
                last = None
                for c in range(8):
                    last = e.transpose(ps[:, 3 + c // 4, (c % 4) * 128:(c % 4 + 1) * 128], h2f[:, c * 128:(c + 1) * 128], identf)
                return last
            pe(trr, r=[f"h2f{s2}", "identf"], w=["ps3", "ps4"])
            act(lambda e: e.copy(h2T[:, 0:512], bank(3)), r=["ps3"], w=[f"h2Ta{s2}"])
            act(lambda e: e.copy(h2T[:, 512:1024], bank(4)), r=["ps4"], w=[f"h2Tb{s2}"])

        def stageC6(i):
            s2 = i % 2
            oh1, oh2, Mb = oh1s[s2], oh2s[s2], Mbs[s2]
            h2T3 = h2Ts[s2].rearrange("p (c t) -> p c t", t=128)

            def mmr(e):
                last = None
                for c in range(8):
                    last = e.matmul(bank(7)[:, 0:36], h2T3[:, c, :], wr3[:, c, :], start=(c == 0), stop=(c == 7))
                return last
            pe(mmr, r=[f"h2Ta{s2}", f"h2Tb{s2}", "wr"], w=["ps7"])
            dve(lambda e: e.tensor_tensor(Lg, bank(7)[:, 0:36], brt, ALU.add), r=["ps7", "brt"], w=["Lg"])
            dve(lambda e: e.reduce_max(sm[:, 0:1], Lg[:, 0:4], axis=AX.X), r=["Lg"], w=["gmax"])
            dve(lambda e: e.tensor_scalar(goh, Lg[:, 0:4], sm[:, 0:1], None, ALU.is_equal), r=["Lg", "gmax"], w=["goh"])
            dve(lambda e: e.tensor_scalar(sm[:, 1:2], sm[:, 0:1], -1.0, None, ALU.mult), r=["gmax"], w=["negg"])
            act(lambda e: e.activation(gex, Lg[:, 0:4], AF.Exp, bias=sm[:, 1:2], accum_out=sm[:, 2:3]), r=["Lg", "negg"], w=["gex", "gsum"])
            dve(lambda e: e.reciprocal(sm[:, 3:4], sm[:, 2:3]), r=["gsum"], w=["gw"])
            dve(lambda e: e.tensor_scalar(pen, goh, -1.0, 1e30, ALU.add, ALU.mult), r=["goh"], w=["pen"])
            dve(lambda e: e.tensor_tensor(elm.rearrange("p (g e) -> p g e", e=8), Lg[:, 4:36].rearrange("p (g e) -> p g e", e=8),
                                          pen.unsqueeze(2).broadcast_to([128, 4, 8]), ALU.add), r=["Lg", "pen"], w=["elm"])
            dve(lambda e: e.max(top8, elm), r=["elm"], w=["top8"])
            dve(lambda e: e.tensor_scalar(oh1, elm, top8[:, 0:1], None, ALU.is_equal), r=["elm", "top8"], w=[f"oh1_{s2}"])
            dve(lambda e: e.tensor_scalar(oh2, elm, top8[:, 1:2], None, ALU.is_equal), r=["elm", "top8"], w=[f"oh2_{s2}"])
            dve(lambda e: e.tensor_scalar(sm[:, 4:5], top8[:, 0:1], -1.0, None, ALU.mult), r=["top8"], w=["negv1"])
            act(lambda e: e.activation(sm[:, 5:6], top8[:, 1:2], AF.Exp, bias=sm[:, 4:5]), r=["top8", "negv1"], w=["e2"])
            dve(lambda e: e.tensor_scalar(sm[:, 6:7], sm[:, 5:6], 1.0, None, ALU.add), r=["e2"], w=["den"])
            dve(lambda e: e.reciprocal(sm[:, 7:8], sm[:, 6:7]), r=["den"], w=["p1"])
            dve(lambda e: e.tensor_tensor(wts3[:, i, 0:1], sm[:, 7:8], sm[:, 3:4], ALU.mult), r=["p1", "gw"], w=[f"w1_{i}"])
            dve(lambda e: e.tensor_tensor(wts3[:, i, 1:2], wts3[:, i, 0:1], sm[:, 5:6], ALU.mult), r=[f"w1_{i}", "e2"], w=[f"w2_{i}"])
            dve(lambda e: e.tensor_tensor(Mb, oh1, oh2, ALU.add), r=[f"oh1_{s2}", f"oh2_{s2}"], w=[f"Mb_{s2}"])

        def stageC7(i):
            s2 = i % 2
            s4 = i % 4
            oh1, oh2, Mb = oh1s[s2], oh2s[s2], Mbs[s2]
            pe(lambda e: e.matmul(bank(5)[:, 0:32], ustrict, Mb, start=True, stop=True), r=["ustrict", f"Mb_{s2}"], w=["ps5"])
            pe(lambda e: e.matmul(bank(6)[:, 0:32], onesm, Mb, start=True, stop=True), r=["onesm", f"Mb_{s2}"], w=["ps6"])
            dve(lambda e: e.tensor_tensor(posC, bank(5)[:, 0:32], base_cnt, ALU.add), r=["ps5", "base_cnt"], w=["posCr"])
            dve(lambda e: e.tensor_scalar(posC, posC, float(CAP - 1), None, ALU.min), r=["posCr"], w=["posCr"])
            dve(lambda e: e.tensor_tensor(posC, posC, eoff, ALU.add), r=["posCr", "eoff"], w=["posCr"])
            dve(lambda e: e.tensor_tensor(tmp32, posC, oh1, ALU.mult), r=["posCr", f"oh1_{s2}"], w=["tmp32"])
            dve(lambda e: e.reduce_sum(sm[:, 8:9], tmp32, axis=AX.X), r=["tmp32"], w=["s1f"])
            dve(lambda e: e.tensor_tensor(tmp32, posC, oh2, ALU.mult), r=["posCr", f"oh2_{s2}", "tmp32"], w=["tmp32"])
            dve(lambda e: e.reduce_sum(sm[:, 9:10], tmp32, axis=AX.X), r=["tmp32"], w=["s2f"])
            dve(lambda e: e.tensor_copy(slots3[:, i, 0:1], sm[:, 8:9]), r=["s1f"], w=[f"sl1_{i}"])
            dve(lambda e: e.tensor_copy(slots3[:, i, 1:2], sm[:, 9:10]), r=["s2f"], w=[f"sl2_{i}"])
            dve(lambda e: e.tensor_tensor(base_cnt, base_cnt, bank(6)[:, 0:32], ALU.add), r=["ps6", "base_cnt"], w=["base_cnt"])
            for k in range(2):
                S.add("pool", lambda e, k=k: e.indirect_dma_start(
                    out=xs_d, out_offset=bass.IndirectOffsetOnAxis(ap=slots3[:, i, k:k + 1], axis=0),
                    in_=h2b[s4], in_offset=None),
                    reads=[f"sl{k + 1}_{i}", f"h2b{s4}"] + [f"xs_zero_{z}" for z in range(4)], writes=[f"xs_d{k}"], dma=f"scat{k}_{s4}")
            if debug:
                dma("sp", rt_d[:, i, 0:2], wts3[:, i, :], r=[f"w1_{i}", f"w2_{i}"], key="dbgrt")

        stagesC = [stageC0, stageC1, stageC2, stageC3, stageC4o, stageC5, stageC6, stageC7]
        groupsC = [[0, 1, 2, 6], [3, 4, 5, 7]]

        def merge_emit(lists):
            pos_ = [0] * len(lists)
            while True:
                best, bf = None, 2.0
                for li, L_ in enumerate(lists):
                    if pos_[li] < len(L_):
                        f_ = pos_[li] / len(L_)
                        if f_ < bf:
                            best, bf = li, f_
                if best is None:
                    break
                S.add(*lists[best][pos_[best]])
                pos_[best] += 1

        for s_ in range(nt_c + len(stagesC) - 1):
            for grp in groupsC:
                lists = []
                for k_ in grp:
                    if 0 <= s_ - k_ < nt_c:
                        S.begin_record()
                        stagesC[k_](s_ - k_)
                        lists.append(S.end_record())
                merge_emit(lists)
        S.barrier()
        A.release(mark_phase)
        if stop_after == "C":
            return finish()

        w1s = A.f32(8 * 512)
        w3s = A.f32(8 * 512)
        w2s = A.f32(4 * 1024)
        w1b = [A.bf16(8 * 512) for _ in range(2)]
        w3b = [A.bf16(8 * 512) for _ in range(2)]
        w2b = [A.bf16(4 * 1024) for _ in range(2)]
        xg = [A.bf16(D) for _ in range(3)]
        XT = [A.bf16(8 * CAP) for _ in range(2)]
        AT = [A.bf16(4 * CAP) for _ in range(2)]
        thD = [A.f32(CAP // 2) for _ in range(2)]
        a1D = [A.f32(CAP // 2) for _ in range(2)]
        ysb = [A.bf16(D) for _ in range(3)]
        HC = CAP // 2

        def load_w(e_):
            for c in range(8):
                dma("sp", w1s[:, c * 512:(c + 1) * 512], w1_d[e_, c * 128:(c + 1) * 128, :], w=[f"w1s{c}"], key="w1s")
                dma("sp", w3s[:, c * 512:(c + 1) * 512], w3_d[e_, c * 128:(c + 1) * 128, :], w=[f"w3s{c}"], key="w3s")
                dma("sp", w2s[:, c * 512:(c + 1) * 512], w2_d[e_, (c // 2) * 128:(c // 2 + 1) * 128, (c % 2) * 512:(c % 2 + 1) * 512],
                    w=[f"w2s{c}"], key="w2s")

        def cast_w_chunk(e_, c):
            sl = e_ % 2
            act(lambda e: e.copy(w1b[sl][:, c * 512:(c + 1) * 512], w1s[:, c * 512:(c + 1) * 512]), r=[f"w1s{cc}" for cc in range(8)], w=[f"w1b{sl}_{c}"])
            dve(lambda e: e.tensor_copy(w3b[sl][:, c * 512:(c + 1) * 512], w3s[:, c * 512:(c + 1) * 512]), r=[f"w3s{cc}" for cc in range(8)], w=[f"w3b{sl}_{c}"])
            dve(lambda e: e.tensor_scalar(w2b[sl][:, c * 512:(c + 1) * 512], w2s[:, c * 512:(c + 1) * 512], 0.5, None, ALU.mult),
                r=[f"w2s{cc}" for cc in range(8)], w=[f"w2b{sl}_{c}"])

        def cast_w(e_):
            for c in range(8):
                cast_w_chunk(e_, c)

        nblk_ct = [0]

        def xblock(e_, b):
            sl = e_ % 2
            XT3 = XT[sl].rearrange("p (c t) -> p c t", t=CAP)
            g = nblk_ct[0] % 3
            nblk_ct[0] += 1
            blk = e_ * NBLK + b
            tb = 0 if b % 2 == 0 else 7
            dma("sp", xg[g], xs_d[blk * 128:(blk + 1) * 128, :], w=[f"xg{g}"], key=f"xg{g}")

            def trx(e):
                last = None
                for c in range(8):
                    last = e.transpose(bankbf(tb)[:, c * 128:(c + 1) * 128], xg[g][:, c * 128:(c + 1) * 128], ident)
                return last
            pe(trx, r=[f"xg{g}", "ident"], w=[f"ps{tb}"])
            if b % 2 == 0:
                act(lambda e: e.copy(XT3[:, :, b * 128:(b + 1) * 128], bankbf(tb).rearrange("p (c t) -> p c t", t=128)),
                    r=[f"ps{tb}"], w=[f"XT{sl}_{b}"])
            else:
                dve(lambda e: e.tensor_copy(XT3[:, :, b * 128:(b + 1) * 128], bankbf(tb).rearrange("p (c t) -> p c t", t=128)),
                    r=[f"ps{tb}"], w=[f"XT{sl}_{b}"])

        def gate_up_step(e_, k):
            sl = e_ % 2
            XT3 = XT[sl].rearrange("p (c t) -> p c t", t=CAP)
            AT3 = AT[sl].rearrange("p (c t) -> p c t", t=CAP)
            w1b3 = w1b[sl].rearrange("p (c n) -> p c n", n=512)
            w3b3 = w3b[sl].rearrange("p (c n) -> p c n", n=512)
            xtres = [f"XT{sl}_{b}" for b in range(NBLK)]
            dc, half = k // 2, k % 2
            bG = 1 + k % 2
            bU = 3 + k % 2
            t2 = k % 2

            def mmg(e):
                last = None
                for c in range(8):
                    last = e.matmul(bank(bG)[:, 0:HC], w1b3[:, c, dc * 128:(dc + 1) * 128], XT3[:, c, half * HC:(half + 1) * HC],
                                    start=(c == 0), stop=(c == 7))
                return last
            pe(mmg, r=xtres + [f"w1b{sl}_{c}" for c in range(8)], w=[f"ps{bG}"])

            def mmu(e):
                last = None
                for c in range(8):
                    last = e.matmul(bank(bU)[:, 0:HC], w3b3[:, c, dc * 128:(dc + 1) * 128], XT3[:, c, half * HC:(half + 1) * HC],
                                    start=(c == 0), stop=(c == 7))
                return last
            pe(mmu, r=xtres + [f"w3b{sl}_{c}" for c in range(8)], w=[f"ps{bU}"])
            act(lambda e: e.activation(thD[t2], bank(bG)[:, 0:HC], AF.Tanh, scale=0.5), r=[f"ps{bG}"], w=[f"thD{t2}"])
            dve(lambda e: e.scalar_tensor_tensor(a1D[t2], thD[t2], 1.0, bank(bG)[:, 0:HC], ALU.add, ALU.mult),
                r=[f"thD{t2}", f"ps{bG}"], w=[f"a1D{t2}"])
            dve(lambda e: e.tensor_tensor(AT3[:, dc, half * HC:(half + 1) * HC], a1D[t2], bank(bU)[:, 0:HC], ALU.mult),
                r=[f"a1D{t2}", f"ps{bU}"], w=[f"AT{sl}_{k}"])

        def down(e_):
            sl = e_ % 2
            AT3 = AT[sl].rearrange("p (c t) -> p c t", t=CAP)
            w2b3 = w2b[sl].rearrange("p (c n) -> p c n", n=1024)
            atres = [f"AT{sl}_{k}" for k in range(8)]
            for b in range(NBLK):
                blk = e_ * NBLK + b
                ysl = blk % 3
                for cg in range(2):
                    def mmy(e, b=b, cg=cg):
                        last = None
                        for dc in range(4):
                            last = e.matmul(bank(5 + cg), AT3[:, dc, b * 128:(b + 1) * 128], w2b3[:, dc, cg * 512:(cg + 1) * 512],
                                            start=(dc == 0), stop=(dc == 3))
                        return last
                    pe(mmy, r=atres + [f"w2b{sl}_{c}" for c in range(8)], w=[f"ps{5 + cg}"])
                    if cg == 0:
                        act(lambda e, ysl=ysl: e.copy(ysb[ysl][:, 0:512], bank(5)), r=["ps5"], w=[f"ysb{ysl}_0"])
                    else:
                        dve(lambda e, ysl=ysl: e.tensor_copy(ysb[ysl][:, 512:1024], bank(6)), r=["ps6"], w=[f"ysb{ysl}_1"])
                dma("sp", ys_d[blk * 128:(blk + 1) * 128, :], ysb[ysl], r=[f"ysb{ysl}_0", f"ysb{ysl}_1"], w=[f"ys_d_{S.uid()}"], key=f"ysb{ysl}")

        assert NBLK == 8
        if n_exp > 0:
            load_w(0)
            cast_w(0)
            if n_exp > 1:
                load_w(1)
            for b in range(NBLK):
                xblock(0, b)
        for e_ in range(n_exp):
            for k in range(8):
                gate_up_step(e_, k)
                if e_ + 1 < n_exp:
                    xblock(e_ + 1, k)
                    cast_w_chunk(e_ + 1, k)
            if e_ + 2 < n_exp:
                load_w(e_ + 2)
            down(e_)
        S.barrier()
        A.release(mark_phase)
        if stop_after == "D":
            return finish()

        fg = A.f32(D)
        dma("sp", fg, fg_d, w=["fg"], key="fg")
        x1L = [A.f32(D) for _ in range(3)]
        y1L = [A.bf16(D) for _ in range(3)]
        y2L = [A.bf16(D) for _ in range(3)]
        tE = [A.f32(D) for _ in range(2)]
        x2E = [A.f32(D) for _ in range(2)]
        junkE = A.bf16(D)
        ssE = [A.f32(1) for _ in range(2)]
        vE = [A.f32(1) for _ in range(2)]
        rsE = [A.f32(1) for _ in range(2)]
        oE = [A.f32(D) for _ in range(2)]

        def stageE1(i):
            s3 = i % 3
            dma("sp", x1L[s3], x1_d[i * 128:(i + 1) * 128, :], r=["x1_d"], w=[f"x1L{s3}"], key=f"x1L{s3}")
            for k, yL in ((0, y1L), (1, y2L)):
                S.add("pool", lambda e, k=k, yL=yL: e.indirect_dma_start(
                    out=yL[s3], out_offset=None, in_=ys_d,
                    in_offset=bass.IndirectOffsetOnAxis(ap=slots3[:, i, k:k + 1], axis=0)),
                    reads=["ys_d", "slots"], writes=[f"y{k}L{s3}"], dma=f"y{k}L{s3}")

        def stageE2(i):
            s3 = i % 3
            s2 = i % 2
            dve(lambda e: e.scalar_tensor_tensor(tE[s2], y1L[s3], wts3[:, i, 0:1], x1L[s3], ALU.mult, ALU.add),
                r=[f"y0L{s3}", f"x1L{s3}", "wts"], w=[f"tE{s2}"])
            dve(lambda e: e.scalar_tensor_tensor(x2E[s2], y2L[s3], wts3[:, i, 1:2], tE[s2], ALU.mult, ALU.add),
                r=[f"y1L{s3}", f"tE{s2}", "wts"], w=[f"x2E{s2}"])
            act(lambda e: e.activation(junkE, x2E[s2], AF.Square, accum_out=ssE[s2]), r=[f"x2E{s2}"], w=["junkE", f"ssE{s2}"])
            dve(lambda e: e.tensor_scalar(vE[s2], ssE[s2], 1.0 / D, EPS, ALU.mult, ALU.add), r=[f"ssE{s2}"], w=[f"vE{s2}"])
            pool(lambda e: e.tensor_tensor(rsE[s2], vE[s2], nhalf[:, 0:1], ALU.pow), r=[f"vE{s2}", "nhalf"], w=[f"rsE{s2}"])
            dve(lambda e: e.scalar_tensor_tensor(oE[s2], x2E[s2], rsE[s2], fg, ALU.mult, ALU.mult), r=[f"x2E{s2}", f"rsE{s2}", "fg"], w=[f"oE{s2}"])
            dma("sp", out_d[i * 128:(i + 1) * 128, :], oE[s2], r=[f"oE{s2}"], w=[f"out_d_{S.uid()}"], key=f"oE{s2}")

        for s_ in range(NT + 1):
            if s_ < NT:
                stageE1(s_)
            if s_ >= 1:
                stageE2(s_ - 1)
        return finish()


def _bc(v, n=128):
    v = np.asarray(v, dtype=np.float32).reshape(1, -1)
    return np.ascontiguousarray(np.broadcast_to(v, (n, v.shape[1])))


def core_inputs(I, b):
    f = np.float32
    invf = (np.float32(500000.0) ** (-np.arange(8, dtype=np.float32) / np.float32(8))).astype(f)
    return {
        "x": np.ascontiguousarray(I["x"][b]),
        "pos": np.ascontiguousarray(I["positions"][b].reshape(NT, 128).T.astype(np.int32)),
        "invf": _bc(invf),
        "g_mix": np.ascontiguousarray(I["norm_mix_g"][0].reshape(8, 128).T),
        "w_in": I["w_in"][0],
        "lng_bc": _bc(I["gm_ln_g"][0].reshape(-1)),
        "lnb_bc": _bc(I["gm_ln_b"][0].reshape(-1)),
        "wsT": np.ascontiguousarray(I["gm_w_s"][0].transpose(2, 0, 1)),
        "bs": np.ascontiguousarray(I["gm_b_s"][0].T),
        "lamv": np.ascontiguousarray(np.broadcast_to(
            np.stack([I["lam_q1"][0], I["lam_k1"][0], I["lam_q2"][0], I["lam_k2"][0]])[None], (128, 4, 64))).astype(f),
        "subg_col": np.ascontiguousarray(I["da_subln_g"][0].reshape(128, 1).astype(np.float32)),
        "w_br_a": I["w_br_a"][0],
        "w_br_b": I["w_br_b"][0],
        "w_out": I["w_out"][0],
        "g_ffn_bc": _bc(I["norm_ffn_g"][0]),
        "w_r": np.ascontiguousarray(np.concatenate([I["w_router_group"][0], I["w_router_expert"][0]], axis=1)),
        "b_r": _bc(np.concatenate([I["b_router_group"][0], I["b_router_expert"][0]])),
        "w1": I["w_exp_gate"][0],
        "w3": I["w_exp_up"][0],
        "w2": I["w_exp_down"][0],
        "fg_bc": _bc(I["final_norm_g"]),
    }


_CACHE = {}


def kernel(**inputs):
    I = {k: np.asarray(v) for k, v in inputs.items()}
    if "nc" not in _CACHE:
        _CACHE["nc"] = build_program()
    nc = _CACHE["nc"]
    in_maps = [core_inputs(I, b) for b in range(8)]
    res = run_bass_kernel_spmd(nc, in_maps, core_ids=list(range(8)))
    return np.stack([np.asarray(r["out"], dtype=np.float32) for r in res.results], axis=0)
```

```python
import math
from contextlib import ExitStack

import numpy as np
import concourse.bass as bass
import concourse.mybir as mybir
from concourse.bass_utils import run_bass_kernel_spmd

F32 = mybir.dt.float32
BF16 = mybir.dt.bfloat16
I32 = mybir.dt.int32
U32 = mybir.dt.uint32
ALU = mybir.AluOpType
AF = mybir.ActivationFunctionType
AX = mybir.AxisListType

ENGS = ("pe", "act", "dve", "pool", "sp")
import os as _os
NOSELF = tuple(x for x in _os.environ.get('KNOSELF', '').split(',') if x)


class _Op:
    __slots__ = ("eng", "fn", "deps", "semkey", "sigval", "needs_sig", "is_dma", "idx")

    def __init__(self, eng, fn, semkey, is_dma):
        self.eng = eng
        self.fn = fn
        self.deps = {}
        self.semkey = semkey
        self.sigval = None
        self.needs_sig = is_dma
        self.is_dma = is_dma


class Sched:
    def __init__(self, nc, stack):
        self.nc = nc
        self.stack = stack
        self.ops = {e: [] for e in ENGS}
        self.lastw = {}
        self.readers = {}
        self.sems = {}
        self.semcount = {}
        self.all_ops = []
        self.keep_prefix = None
        self._uid = 0

    def uid(self):
        self._uid += 1
        return self._uid

    def _sem(self, key):
        if key not in self.sems:
            name = "s_" + str(key).replace(" ", "").replace("(", "").replace(")", "").replace(",", "_").replace("'", "")
            self.sems[key] = self.stack.enter_context(self.nc.semaphore(name[:40]))
            self.semcount[key] = 0
        return self.sems[key]

    def begin_record(self):
        self._rec = []

    def end_record(self):
        r, self._rec = self._rec, None
        return r

    def add(self, eng, fn, reads=(), writes=(), dma=None):
        if getattr(self, "_rec", None) is not None:
            self._rec.append((eng, fn, tuple(reads), tuple(writes), dma))
            return None
        is_dma = dma is not None
        semkey = ("dma", dma) if is_dma else ("eng", eng)
        op = _Op(eng, fn, semkey, is_dma)
        self._sem(semkey)
        deps = {}

        def dep_on(o):
            if o is None or o is op:
                return
            if (not o.is_dma) and o.eng == "pe" and eng == "pe" and not is_dma:
                return
            if NOSELF and (not o.is_dma) and (not is_dma) and o.eng == eng and eng in NOSELF:
                return
            cur = deps.get(o.semkey)
            if cur is None or cur.idx < o.idx:
                deps[o.semkey] = o

        for r in reads:
            dep_on(self.lastw.get(r))
            if r.startswith("ps"):
                for o in self.readers.get(r, {}).values():
                    if o.eng != eng:
                        dep_on(o)
        for r in writes:
            dep_on(self.lastw.get(r))
            for o in self.readers.get(r, {}).values():
                dep_on(o)
        op.idx = len(self.all_ops)
        self.all_ops.append(op)
        for r in reads:
            self.readers.setdefault(r, {})[semkey] = op
        for r in writes:
            self.lastw[r] = op
            self.readers[r] = {}
        for o in deps.values():
            o.needs_sig = True
        op.deps = deps
        self.ops[eng].append(op)
        return op

    def barrier(self):
        tok = ("__barrier__",)
        last = []
        for e in ENGS:
            for o in reversed(self.ops[e]):
                if o.fn is not None:
                    last.append(o)
                    break
        lastdma = {}
        for o in self.all_ops:
            if o.is_dma:
                lastdma[o.semkey] = o
        keep_res = {r: o for r, o in self.lastw.items() if r.startswith(self.keep_prefix)} if self.keep_prefix else {}
        excl = {o.semkey for o in keep_res.values()}
        every = {o.semkey: o for o in last if not o.is_dma}
        every.update({k: o for k, o in lastdma.items() if k not in excl})
        for e in ENGS:
            op = _Op(e, None, ("eng", e), False)
            op.idx = len(self.all_ops)
            self.all_ops.append(op)
            op.deps = {k: o for k, o in every.items() if not (k == ("eng", "pe") and e == "pe")}
            for o in op.deps.values():
                o.needs_sig = True
            self.ops[e].append(op)
        self.lastw = dict(keep_res)
        self.readers = {}

    def finalize_and_emit(self, final_waits=()):
        nc = self.nc
        for o in self.all_ops:
            if o.fn is None:
                continue
            if o.needs_sig:
                inc = 16 if o.is_dma else 1
                self.semcount[o.semkey] += inc
                o.sigval = self.semcount[o.semkey]
        engmap = {"pe": "tensor", "act": "scalar", "dve": "vector", "pool": "gpsimd", "sp": "sync"}
        final = [(self.sems[o.semkey], o.sigval) for o in final_waits]

        def run(engname, eng):
            known = {}
            for o in self.ops[engname]:
                for k, d in o.deps.items():
                    v = d.sigval
                    assert v is not None, (k, d.eng)
                    if known.get(k, 0) >= v:
                        continue
                    known[k] = v
                    eng.wait_ge(self.sems[k], v)
                if o.fn is None:
                    continue
                inst = o.fn(eng)
                if o.needs_sig:
                    assert inst is not None
                    inst.then_inc(self.sems[o.semkey], 16 if o.is_dma else 1)
            if engname == "sp":
                for s, v in final:
                    eng.wait_ge(s, v)

        with nc.Block() as block:
            for engname in ENGS:
                getattr(block, engmap[engname])(lambda eng, _n=engname: run(_n, eng))


D = 1024
T = 8192
NT = T // 128
NC_IN = 2560
NEXP = 32
CAP = 1024
NBLK = CAP // 128
NSLOT = NEXP * CAP
EPS = 1e-6
LAM_INIT = 0.8 - 0.6 * math.exp(0.0)
TWO_PI = 2.0 * math.pi
C1 = 6.28125
C2 = TWO_PI - C1


INPUT_NAMES = []
import os
STAGES = os.environ.get('KSTAGES', '123')


class Arena:
    def __init__(self, nc, st, nbytes):
        self.t = st.enter_context(nc.sbuf_tensor("arena", [128, nbytes // 4], F32))
        self.off = 0
        self.cap = nbytes

    def _take(self, nbytes):
        nbytes = (nbytes + 31) // 32 * 32
        o = self.off
        self.off += nbytes
        assert self.off <= self.cap, ("SBUF arena overflow", self.off, self.cap)
        return o

    def f32(self, n):
        o = self._take(n * 4)
        return self.t[:, o // 4:o // 4 + n]

    def i32(self, n):
        return self.f32(n).bitcast(I32)

    def bf16(self, n):
        n2 = (n + 1) // 2 * 2
        o = self._take(n2 * 2)
        return self.t[:, o // 4:o // 4 + n2 // 2].bitcast(BF16)[:, 0:n]

    def mark(self):
        return self.off

    def release(self, m):
        self.off = m


def build_program(debug=False, stop_after=None, nt_a=NT, nt_b=NT, nt_c=NT, n_exp=NEXP):
    nc = bass.Bass("TRN2", target_bir_lowering=False)
    okind = "ExternalOutput" if debug else "Internal"

    early = stop_after in ("setup", "win", "A", "B", "C")
    INPUT_NAMES.clear()

    def din(name, shape, dt=F32):
        if early and name in ("w1", "w3", "w2"):
            return None
        INPUT_NAMES.append(name)
        return nc.dram_tensor(name, list(shape), dt, kind="ExternalInput").ap()

    def dscr(name, shape, dt):
        return nc.dram_tensor(name, list(shape), dt, kind=okind).ap()

    x_d = din("x", [T, D])
    pos_d = din("pos", [128, NT], I32)
    invf_d = din("invf", [128, 8])
    gmix_d = din("g_mix", [128, 8])
    win_d = din("w_in", [D, 4608])
    lng_d = din("lng_bc", [128, 512])
    lnb_d = din("lnb_bc", [128, 512])
    wsT_d = din("wsT", [128, 4, 128])
    bs_d = din("bs", [128, 4])
    lamv_d = din("lamv", [128, 4, 64])
    subgc_d = din("subg_col", [128, 1])
    wa_d = din("w_br_a", [512, D])
    wb_d = din("w_br_b", [512, D])
    wo_d = din("w_out", [D, D])
    gffn_d = din("g_ffn_bc", [128, D])
    wr_d = din("w_r", [D, 36])
    br_d = din("b_r", [128, 36])
    w1_d = din("w1", [NEXP, D, 512])
    w3_d = din("w3", [NEXP, D, 512])
    w2_d = din("w2", [NEXP, 512, D])
    fg_d = din("fg_bc", [128, D])
    out_d = nc.dram_tensor("out", [T, D], F32, kind="ExternalOutput").ap()

    qT_d = dscr("qT_s", [128, 4, T], BF16)
    kT_d = dscr("kT_s", [128, 4, T], BF16)
    v_d = dscr("v_s", [128, NT, 4, 130], BF16)
    yaT_d = dscr("yaT_s", [NT, 128, 512], BF16)
    ybT_d = dscr("ybT_s", [NT, 128, 512], BF16)
    x1_d = dscr("x1_s", [T, D], F32)
    xs_d = dscr("xs_s", [NSLOT, D], BF16)
    ys_d = dscr("ys_s", [NSLOT, D], BF16)
    rt_d = dscr("rt_s", [128, NT, 4], F32) if debug else None

    with ExitStack() as st:
        S = Sched(nc, st)
        A = Arena(nc, st, 192 * 1024)
        ps = st.enter_context(nc.psum_tensor("ps", [128, 8, 512], F32))

        def bank(b):
            return ps[:, b, :]

        def finish():
            lastd = {}
            for o in S.all_ops:
                if o.is_dma:
                    lastd[o.semkey] = o
            S.finalize_and_emit(final_waits=list(lastd.values()))
            return nc

        def bankbf(b):
            return ps[:, b, :].bitcast(BF16)

        dve = lambda fn, r=(), w=(): S.add("dve", fn, r, w)
        act = lambda fn, r=(), w=(): S.add("act", fn, r, w)
        pool = lambda fn, r=(), w=(): S.add("pool", fn, r, w)
        pe = lambda fn, r=(), w=(): S.add("pe", fn, r, w)

        def dma(q, out, in_, r=(), w=(), key=None):
            return S.add(q, lambda e: e.dma_start(out=out, in_=in_), r, w, dma=key)

        ident = A.bf16(128)
        identf = A.f32(128)
        ustrict = A.bf16(128)
        onesm = A.bf16(128)
        maskb = A.bf16(128)
        ctmp = A.f32(128)
        nhalf = A.f32(8)
        pool(lambda e: e.memset(nhalf, -0.5), w=["nhalf"])
        pool(lambda e: e.memset(identf, 0.0), w=["identf"])
        pool(lambda e: e.affine_select(identf, identf, [[-1, 128]], ALU.not_equal, 1.0, base=0,
                                       channel_multiplier=1), r=["identf"], w=["identf"])
        dve(lambda e: e.tensor_copy(ident, identf), r=["identf"], w=["ident"])
        pool(lambda e: e.memset(ctmp, 1.0), w=["ctmp"])
        pool(lambda e: e.affine_select(ctmp, ctmp, [[1, 128]], ALU.is_gt, 0.0, base=0,
                                       channel_multiplier=-1), r=["ctmp"], w=["ctmp"])
        dve(lambda e: e.tensor_copy(ustrict, ctmp), r=["ctmp"], w=["ustrict"])
        pool(lambda e: e.memset(ctmp, 0.0), r=["ctmp"], w=["ctmp"])
        pool(lambda e: e.affine_select(ctmp, ctmp, [[1, 128]], ALU.is_ge, -30000.0, base=0,
                                       channel_multiplier=-1), r=["ctmp"], w=["ctmp"])
        dve(lambda e: e.tensor_copy(maskb, ctmp), r=["ctmp"], w=["maskb"])
        dve(lambda e: e.memset(onesm, 1.0), w=["onesm"])

        slots_i = A.i32(NT * 2)
        wts = A.f32(NT * 2)
        slots3 = slots_i.rearrange("p (t k) -> p t k", k=2)
        wts3 = wts.rearrange("p (t k) -> p t k", k=2)
        tokid = A.i32(NT)
        pool(lambda e: e.iota(tokid, [[128, NT]], base=0, channel_multiplier=1), w=["tokid"])
        eoff_i = A.i32(NEXP)
        eoff = A.f32(NEXP)
        pool(lambda e: e.iota(eoff_i, [[CAP, NEXP]], base=0, channel_multiplier=0), w=["eoff_i"])
        dve(lambda e: e.tensor_copy(eoff, eoff_i), r=["eoff_i"], w=["eoff"])
        base_cnt = A.f32(NEXP)
        dve(lambda e: e.memset(base_cnt, 0.0), w=["base_cnt"])
        S.keep_prefix = "xs_zero_"
        ztile = A.bf16(4 * D)
        dve(lambda e: e.memset(ztile, 0.0), w=["ztile"])
        for z_ in range(NSLOT // 512):
            dma("act", xs_d[z_ * 512:(z_ + 1) * 512, :].rearrange("(n p) d -> p n d", p=128),
                ztile.rearrange("p (n d) -> p n d", d=D), r=["ztile"], w=["xs_zero_" + str(z_ % 4)], key=f"zfill{z_ % 4}")

        lamv = A.f32(256)
        lamp = A.f32(128)
        lsum = A.f32(2)
        lam_e = A.f32(2)
        neglam = A.f32(1)
        dma("sp", lamv, lamv_d.rearrange("p a b -> p (a b)"), w=["lamv"], key="lamv")
        lamv3 = lamv.rearrange("p (a b) -> p a b", b=64)
        dve(lambda e: e.tensor_tensor(lamp[:, 0:64], lamv3[:, 0, :], lamv3[:, 1, :], ALU.mult), r=["lamv"], w=["lamp"])
        dve(lambda e: e.tensor_tensor(lamp[:, 64:128], lamv3[:, 2, :], lamv3[:, 3, :], ALU.mult), r=["lamp", "lamv"], w=["lamp"])
        dve(lambda e: e.reduce_sum(lsum, lamp.rearrange("p (a b) -> p a b", b=64), axis=AX.X), r=["lamp"], w=["lsum"])
        act(lambda e: e.activation(lam_e, lsum, AF.Exp), r=["lsum"], w=["lam_e"])
        dve(lambda e: e.scalar_tensor_tensor(neglam, lam_e[:, 1:2], -LAM_INIT, lam_e[:, 0:1], ALU.add, ALU.subtract),
            r=["lam_e"], w=["neglam"])

        cos_t = A.f32(NT * 8)
        sin_t = A.f32(NT * 8)
        m0 = A.mark()
        pos_i = A.i32(NT)
        posf = A.f32(NT)
        invf = A.f32(8)
        ang = A.f32(NT * 8)
        kf = A.f32(NT * 8)
        ki = A.i32(NT * 8)
        rr = A.f32(NT * 8)
        r2 = A.f32(NT * 8)
        msk = A.f32(NT * 8)
        dma("sp", pos_i, pos_d, w=["pos_i"], key="pos_i")
        dma("sp", invf, invf_d, w=["invf"], key="invf")
        dve(lambda e: e.tensor_copy(posf, pos_i), r=["pos_i"], w=["posf"])
        ang3 = ang.rearrange("p (t j) -> p t j", j=8)
        dve(lambda e: e.tensor_tensor(ang3, posf.unsqueeze(2).broadcast_to([128, NT, 8]),
                                      invf.unsqueeze(1).broadcast_to([128, NT, 8]), ALU.mult),
            r=["posf", "invf"], w=["ang"])
        dve(lambda e: e.tensor_scalar(kf, ang, 1.0 / TWO_PI, None, ALU.mult), r=["ang"], w=["kf"])
        dve(lambda e: e.tensor_copy(ki, kf), r=["kf"], w=["ki"])
        dve(lambda e: e.tensor_copy(kf, ki), r=["ki"], w=["kf"])
        dve(lambda e: e.scalar_tensor_tensor(rr, kf, -C1, ang, ALU.mult, ALU.add), r=["kf", "ang"], w=["rr"])
        dve(lambda e: e.scalar_tensor_tensor(rr, kf, -C2, rr, ALU.mult, ALU.add), r=["kf", "rr"], w=["rr"])
        dve(lambda e: e.tensor_scalar(r2, rr, math.pi / 2, None, ALU.add), r=["rr"], w=["r2"])
        dve(lambda e: e.tensor_scalar(msk, r2, math.pi, None, ALU.is_gt), r=["r2"], w=["msk"])
        dve(lambda e: e.scalar_tensor_tensor(r2, msk, -TWO_PI, r2, ALU.mult, ALU.add), r=["msk", "r2"], w=["r2"])
        PI_SAFE = 3.1415925
        dve(lambda e: e.tensor_scalar(rr, rr, PI_SAFE, -PI_SAFE, ALU.min, ALU.max), r=["rr"], w=["rr"])
        dve(lambda e: e.tensor_scalar(r2, r2, PI_SAFE, -PI_SAFE, ALU.min, ALU.max), r=["r2"], w=["r2"])
        act(lambda e: e.activation(sin_t, rr, AF.Sin), r=["rr"], w=["sin_t"])
        act(lambda e: e.activation(cos_t, r2, AF.Sin), r=["r2"], w=["cos_t"])
        if debug:
            dbg_cs = nc.dram_tensor("dbg_cs", [128, 2, NT * 8], F32, kind="ExternalOutput").ap()
            dma("sp", dbg_cs[:, 0, :], cos_t, r=["cos_t"], key="dbgc")
            dma("sp", dbg_cs[:, 1, :], sin_t, r=["sin_t"], key="dbgs")
        S.barrier()
        A.release(m0)
        if stop_after == "setup":
            return finish()
        cos3 = cos_t.rearrange("p (t j) -> p t j", j=8)
        sin3 = sin_t.rearrange("p (t j) -> p t j", j=8)
        mark_phase = A.mark()

        def emit_rstd(ss, vtmp, rstd, n, scale, rname):
            dve(lambda e: e.tensor_scalar(vtmp, ss, scale, EPS, ALU.mult, ALU.add), r=[rname + "ss"], w=[rname + "v"])
            pool(lambda e: e.tensor_tensor(rstd, vtmp, nhalf[:, 0:n], ALU.pow), r=[rname + "v", "nhalf"], w=[rname + "rstd"])

        win = A.bf16(8 * NC_IN)
        win3 = win.rearrange("p (c n) -> p c n", n=NC_IN)
        gmix = A.f32(8)
        dma("sp", gmix, gmix_d, w=["gmix"], key="gmix")
        mA = A.mark()
        wst = [A.f32(NC_IN), A.f32(NC_IN)]
        for c in range(8):
            sl = c % 2
            dma("sp", wst[sl], win_d[c * 128:(c + 1) * 128, 0:NC_IN], w=[f"wst{sl}"], key=f"wst{sl}")
            if c % 2 == 0:
                dve(lambda e, c=c, sl=sl: e.tensor_scalar(win3[:, c, :], wst[sl], gmix[:, c:c + 1], None, ALU.mult),
                    r=[f"wst{sl}", "gmix"], w=[f"win{c}"])
            else:
                act(lambda e, c=c, sl=sl: e.activation(win3[:, c, :], wst[sl], AF.Copy, scale=gmix[:, c:c + 1]),
                    r=[f"wst{sl}", "gmix"], w=[f"win{c}"])
        S.barrier()
        A.release(mA)
        if stop_after == "win":
            dbg_w = nc.dram_tensor("dbg_w", [128, 8 * NC_IN], BF16, kind="ExternalOutput").ap()
            dma("sp", dbg_w, win, r=[f"win{c}" for c in range(8)], key="dbgw")
            return finish()
        winres = [f"win{c}" for c in range(8)]

        lng = A.f32(512)
        lnb = A.f32(512)
        wsTf = A.f32(512)
        wsT = A.bf16(512)
        bs = A.f32(4)
        dma("sp", lng, lng_d, w=["lng"], key="lng")
        dma("sp", lnb, lnb_d, w=["lnb"], key="lnb")
        dma("sp", wsTf, wsT_d.rearrange("p g t -> p (g t)"), w=["wsTf"], key="wsTf")
        dma("sp", bs, bs_d, w=["bs"], key="bs")
        pool(lambda e: e.affine_select(wsTf, wsTf, [[0, 4], [1, 128]], ALU.is_ge, 0.0, base=0,
                                       channel_multiplier=-1), r=["wsTf"], w=["wsTf"])
        dve(lambda e: e.tensor_copy(wsT, wsTf), r=["wsTf"], w=["wsT"])
        wsT3 = wsT.rearrange("p (g t) -> p g t", t=128)

        NXS = 3
        xt = [A.f32(D) for _ in range(NXS)]
        junk = A.bf16(D)
        ssA = [A.f32(1) for _ in range(2)]
        vA = [A.f32(1) for _ in range(2)]
        rsA = [A.f32(1) for _ in range(2)]
        hb = [A.bf16(D) for _ in range(2)]
        hT = [A.bf16(D) for _ in range(2)]
        x2b = [A.f32(512) for _ in range(2)]
        xhb = [A.f32(512) for _ in range(2)]
        gu = [A.f32(512) for _ in range(2)]
        gv = [A.f32(512) for _ in range(2)]
        sq = A.f32(512)
        lst = [A.f32(16) for _ in range(2)]
        vn = A.f32(512)
        vnb = [A.bf16(512) for _ in range(2)]
        qb = [A.bf16(512) for _ in range(3)]
        kb = [A.bf16(512) for _ in range(3)]
        rt = [A.f32(64 * 4) for _ in range(2)]
        vb = [A.bf16(4 * 130) for _ in range(2)]
        yab = [A.bf16(512) for _ in range(2)]
        yaT = [A.bf16(512) for _ in range(2)]
        qT = [A.bf16(512) for _ in range(2)]
        kT = [A.bf16(512) for _ in range(2)]
        for s_ in range(2):
            dve(lambda e, s_=s_: e.memset(vb[s_], 1.0), w=[f"vb{s_}"])

        def stageA0(i):
            xs = i % NXS
            s2 = i % 2
            KA1 = int(os.environ.get("KA1", "9"))
            dma("sp", xt[xs], x_d[i * 128:(i + 1) * 128, :], w=[f"xt{xs}"], key=f"xt{xs}")
            if KA1 < 2: return
            act(lambda e: e.activation(junk, xt[xs], AF.Square, accum_out=ssA[s2]), r=[f"xt{xs}"], w=["junk", f"Ass{s2}"])
            if KA1 < 3: return
            dve(lambda e: e.tensor_scalar(vA[s2], ssA[s2], 1.0 / D, EPS, ALU.mult, ALU.add), r=[f"Ass{s2}"], w=[f"Av{s2}"])
            if KA1 < 4: return
            pool(lambda e: e.tensor_tensor(rsA[s2], vA[s2], nhalf[:, 0:1], ALU.pow), r=[f"Av{s2}", "nhalf"], w=[f"Ars{s2}"])
            if KA1 < 5: return
            dve(lambda e: e.tensor_scalar(hb[s2], xt[xs], rsA[s2], None, ALU.mult), r=[f"xt{xs}", f"Ars{s2}"], w=[f"hb{s2}"])
            if KA1 < 6: return

        def stageA1(i):
            s2 = i % 2
            KA1 = 9

            def tr(e):
                last = None
                for c in range(8):
                    last = e.transpose(bankbf(0)[:, c * 128:(c + 1) * 128], hb[s2][:, c * 128:(c + 1) * 128], ident)
                return last
            pe(tr, r=[f"hb{s2}", "ident"], w=["ps0"])
            if KA1 < 7: return
            act(lambda e: e.copy(hT[s2], bankbf(0)), r=["ps0"], w=[f"hT{s2}"])

        def zmm(i, cg, bk):
            s2 = i % 2
            hT3 = hT[s2].rearrange("p (c t) -> p c t", t=128)

            def mm(e):
                last = None
                for c in range(8):
                    last = e.matmul(bank(bk), hT3[:, c, :], win3[:, c, cg * 512:(cg + 1) * 512],
                                    start=(c == 0), stop=(c == 7))
                return last
            pe(mm, r=[f"hT{s2}"] + winres, w=[f"ps{bk}"])

        def gelu_chain(i, which, bk, outbuf, oname):
            x2 = x2b[which]
            xh = xhb[which]
            n2, nh = f"x2_{which}", f"xh_{which}"
            act(lambda e: e.activation(x2, bank(bk), AF.Square), r=[f"ps{bk}"], w=[n2])
            act(lambda e: e.activation(xh, bank(bk), AF.Copy, scale=0.5), r=[f"ps{bk}"], w=[nh])
            dve(lambda e: e.tensor_scalar(x2, x2, 0.044715, 1.0, ALU.mult, ALU.add), r=[n2], w=[n2])
            dve(lambda e: e.tensor_tensor(x2, x2, xh, ALU.mult), r=[n2, nh], w=[n2])
            act(lambda e: e.activation(x2, x2, AF.Tanh, scale=2.0 * 0.7978845608028654), r=[n2], w=[n2])
            dve(lambda e: e.scalar_tensor_tensor(outbuf, x2, 1.0, xh, ALU.add, ALU.mult), r=[n2, nh], w=[oname])

        def rope(i, bk, dst, dname, tmp):
            z3 = bank(bk).rearrange("p (s d) -> p s d", d=64)
            d3 = dst.rearrange("p (s d) -> p s d", d=64)
            cb = cos3[:, i, :].unsqueeze(1).broadcast_to([128, 8, 8])
            sb = sin3[:, i, :].unsqueeze(1).broadcast_to([128, 8, 8])
            t4 = tmp.rearrange("p (a s j) -> p a s j", a=4, j=8)
            tn = dname + "_rt"
            act(lambda e: e.copy(dst, bank(bk)), r=[f"ps{bk}"], w=[dname])
            dve(lambda e: e.tensor_tensor(t4[:, 0], z3[:, :, 0:8], cb, ALU.mult), r=[f"ps{bk}", "cos_t", dname], w=[tn + "0"])
            dve(lambda e: e.tensor_tensor(t4[:, 1], z3[:, :, 8:16], sb, ALU.mult), r=[f"ps{bk}", "sin_t"], w=[tn + "1"])
            dve(lambda e: e.tensor_tensor(t4[:, 2], z3[:, :, 8:16], cb, ALU.mult), r=[f"ps{bk}", "cos_t"], w=[tn + "2"])
            dve(lambda e: e.tensor_tensor(t4[:, 3], z3[:, :, 0:8], sb, ALU.mult), r=[f"ps{bk}", "sin_t"], w=[tn + "3"])
            dve(lambda e: e.tensor_tensor(d3[:, :, 0:8], t4[:, 0], t4[:, 1], ALU.subtract), r=[tn + "0", tn + "1"], w=[dname])
            dve(lambda e: e.tensor_tensor(d3[:, :, 8:16], t4[:, 2], t4[:, 3], ALU.add), r=[tn + "2", tn + "3"], w=[dname])

        def stageA2(i):
            s2 = i % 2
            zmm(i, 0, 1)
            gelu_chain(i, 0, 1, gu[s2], f"gu{s2}")
            zmm(i, 1, 2)
            gelu_chain(i, 1, 2, gv[s2], f"gv{s2}")
            s3 = i % 3
            zmm(i, 2, 3)
            rope(i, 3, qb[s3], f"qb{s3}", rt[0])
            zmm(i, 3, 1)
            rope(i, 1, kb[s3], f"kb{s3}", rt[1])
            zmm(i, 4, 2)
            vb3 = vb[s2].rearrange("p (h d) -> p h d", d=130)
            act(lambda e: e.copy(vb3[:, :, 0:128], bank(2).rearrange("p (h d) -> p h d", d=128)),
                r=["ps2"], w=[f"vb{s2}"])
            dma("sp", v_d[:, i, :, :], vb3, r=[f"vb{s2}"], w=[f"v_d_{S.uid()}"], key=f"vb{s2}")
            g_ = gv[s2]
            g3 = g_.rearrange("p (g d) -> p g d", d=128)
            L = lst[s2]
            ln = f"lst{s2}"
            dve(lambda e: e.reduce_sum(L[:, 0:4], g3, axis=AX.X), r=[f"gv{s2}"], w=[ln + "s"])
            dve(lambda e: e.tensor_tensor(sq, g_, g_, ALU.mult), r=[f"gv{s2}"], w=["sq"])
            dve(lambda e: e.reduce_sum(L[:, 4:8], sq.rearrange("p (g d) -> p g d", d=128), axis=AX.X), r=["sq"], w=[ln + "q"])
            dve(lambda e: e.tensor_scalar(L[:, 8:12], L[:, 0:4], 1.0 / 128, None, ALU.mult), r=[ln + "s"], w=[ln + "m"])
            dve(lambda e: e.tensor_tensor(L[:, 12:16], L[:, 8:12], L[:, 8:12], ALU.mult), r=[ln + "m"], w=[ln + "v"])
            dve(lambda e: e.scalar_tensor_tensor(L[:, 12:16], L[:, 4:8], 1.0 / 128, L[:, 12:16], ALU.mult, ALU.subtract),
                r=[ln + "q", ln + "v"], w=[ln + "v"])
            dve(lambda e: e.tensor_scalar(L[:, 12:16], L[:, 12:16], EPS, None, ALU.add), r=[ln + "v"], w=[ln + "v"])
            pool(lambda e: e.tensor_tensor(L[:, 4:8], L[:, 12:16], nhalf[:, 0:4], ALU.pow), r=[ln + "v", "nhalf", ln + "q"], w=[ln + "r"])
            for g in range(4):
                dve(lambda e, g=g: e.tensor_scalar(vn[:, g * 128:(g + 1) * 128], g_[:, g * 128:(g + 1) * 128],
                                                   L[:, 8 + g:9 + g], L[:, 4 + g:5 + g], ALU.subtract, ALU.mult),
                    r=[f"gv{s2}", ln + "m", ln + "r"], w=[f"vn{g}"])
            vnr = [f"vn{g}" for g in range(4)]
            dve(lambda e: e.tensor_tensor(vn, vn, lng, ALU.mult), r=vnr + ["lng"], w=vnr)
            dve(lambda e: e.tensor_tensor(vnb[s2], vn, lnb, ALU.add), r=vnr + ["lnb"], w=[f"vnb{s2}"])

        def stageA3(i):
            s2 = i % 2

            def sp_mm(e):
                last = None
                for g in range(4):
                    last = e.matmul(bank(4)[:, g * 128:(g + 1) * 128], wsT3[:, g, :], vnb[s2][:, g * 128:(g + 1) * 128],
                                    start=True, stop=True)
                return last
            KA3 = int(os.environ.get("KA3", "9"))
            pe(sp_mm, r=[f"vnb{s2}", "wsT"], w=["ps4"])
            if KA3 < 2: return
            for g in range(4):
                dve(lambda e, g=g: e.scalar_tensor_tensor(yab[s2][:, g * 128:(g + 1) * 128], bank(4)[:, g * 128:(g + 1) * 128],
                                                          bs[:, g:g + 1], gu[s2][:, g * 128:(g + 1) * 128], ALU.add, ALU.mult),
                    r=["ps4", "bs", f"gu{s2}"], w=[f"yab{s2}_{g}"])

        def stageA4(i):
            s2 = i % 2
            s3 = i % 3
            KA3 = 9
            yres = [f"yab{s2}_{g}" for g in range(4)]

            def tr(src, bk):
                def f(e):
                    last = None
                    for c in range(4):
                        last = e.transpose(bankbf(bk)[:, c * 128:(c + 1) * 128], src[:, c * 128:(c + 1) * 128], ident)
                    return last
                return f
            pe(tr(yab[s2], 5), r=yres + ["ident"], w=["ps5"])
            pe(tr(qb[s3], 6), r=[f"qb{s3}", "ident"], w=["ps6"])
            pe(tr(kb[s3], 7), r=[f"kb{s3}", "ident"], w=["ps7"])
            act(lambda e: e.copy(yaT[s2], bankbf(5)[:, 0:512]), r=["ps5"], w=[f"yaT{s2}"])
            dve(lambda e: e.tensor_copy(qT[s2], bankbf(6)[:, 0:512]), r=["ps6"], w=[f"qT{s2}"])
            act(lambda e: e.copy(kT[s2], bankbf(7)[:, 0:512]), r=["ps7"], w=[f"kT{s2}"])
            if KA3 < 5: return
            dma("sp", yaT_d[i], yaT[s2], r=[f"yaT{s2}"], w=[f"yaT_d_{S.uid()}"], key=f"yaT{s2}")
            dma("sp", qT_d[:, :, i * 128:(i + 1) * 128], qT[s2].rearrange("p (h t) -> p h t", t=128),
                r=[f"qT{s2}"], w=[f"qT_d_{S.uid()}"], key=f"qT{s2}")
            dma("sp", kT_d[:, :, i * 128:(i + 1) * 128], kT[s2].rearrange("p (h t) -> p h t", t=128),
                r=[f"kT{s2}"], w=[f"kT_d_{S.uid()}"], key=f"kT{s2}")

        stagesA = [stageA0, stageA1, stageA2, stageA3, stageA4]
        for s_ in range(nt_a + len(stagesA) - 1):
            lists = []
            for k_, fn in enumerate(stagesA):
                if 0 <= s_ - k_ < nt_a:
                    S.begin_record()
                    fn(s_ - k_)
                    lists.append(S.end_record())
            pos_ = [0] * len(lists)
            while True:
                best, bf = None, 2.0
                for li, L_ in enumerate(lists):
                    if pos_[li] < len(L_):
                        f_ = pos_[li] / len(L_)
                        if f_ < bf:
                            best, bf = li, f_
                if best is None:
                    break
                S.add(*lists[best][pos_[best]])
                pos_[best] += 1
        S.barrier()
        A.release(mark_phase)

        if stop_after == "A":
            return finish()

        KT_sb = A.bf16(4 * T)
        KT3 = KT_sb.rearrange("p (h t) -> p h t", t=T)
        V_sb = A.bf16(NT * 4 * 130)
        V4 = V_sb.rearrange("p (i h d) -> p i h d", h=4, d=130)
        for h in range(4):
            dma("sp", KT3[:, h, :], kT_d[:, h, :], r=["kT_d"], w=[f"KT{h}"], key=f"KTl{h}")
        for c in range(4):
            dma("sp", V4[:, c * 16:(c + 1) * 16], v_d[:, c * 16:(c + 1) * 16], r=["v_d"], w=[f"V{c}"], key=f"Vl{c}")
        subgc = A.f32(1)
        dma("sp", subgc, subgc_d, w=["subgc"], key="subgc")
        dve(lambda e: e.tensor_scalar(subgc, subgc, 1.0 - LAM_INIT, None, ALU.mult), r=["subgc"], w=["subgc"])
        onesf = A.f32(128)
        dve(lambda e: e.memset(onesf, 1.0), w=["onesf"])
        QTs = [A.bf16(4 * 512) for _ in range(2)]
        NPT = 4
        pTall = A.bf16(2 * NPT * 512)
        pT4 = pTall.rearrange("p (m s q) -> p m s q", m=2, s=NPT)
        pT = [[pT4[:, m_, s_, :] for s_ in range(NPT)] for m_ in range(2)]
        racc = [A.f32(512) for _ in range(2)]
        a0B = A.f32(512)
        a1B = A.f32(512)
        l1B = A.f32(512)
        rlb = [A.f32(512) for _ in range(2)]
        t1B = A.f32(512)
        t2B = A.f32(512)
        oB = A.f32(512)
        sqB = A.f32(512)
        v4B = A.f32(4)
        rs4B = A.f32(4)
        RmB = A.f32(512)
        ybT = [A.bf16(512) for _ in range(2)]
        n_st = nt_b // 4
        blocks = [(I_, h, j) for I_ in range(n_st) for h in range(4) for j in range(4 * I_ + 4)]

        def load_q(I_):
            sl = I_ % 2
            dma("sp", QTs[sl].rearrange("p (h t) -> p h t", t=512), qT_d[:, :, I_ * 512:(I_ + 1) * 512],
                r=["qT_d"], w=[f"QT{sl}"], key=f"QT{sl}")

        def emit_qk(n):
            I_, h, j = blocks[n]
            par = n % 2
            qlo = max(0, j - 4 * I_)
            ncol = 512 - qlo * 128
            Q3 = QTs[I_ % 2].rearrange("p (h t) -> p h t", t=512)
            diag = j >= 4 * I_

            def f(e):
                last = None
                for m in range(2):
                    last = e.matmul(bank(2 * m + par)[:, 0:ncol], KT3[m * 64:(m + 1) * 64, h, j * 128:(j + 1) * 128],
                                    Q3[m * 64:(m + 1) * 64, h, qlo * 128:512], start=True, stop=not diag)
                if diag:
                    for m in range(2):
                        last = e.matmul(bank(2 * m + par)[:, 0:128], ident, maskb, start=False, stop=True)
                return last
            pe(f, r=[f"KT{h}", f"QT{I_ % 2}", "ident", "maskb"], w=[f"ps{par}", f"ps{2 + par}"])
            sl_ = n % NPT
            act(lambda e: e.activation(pT4[:, :, sl_, 0:ncol], ps[:, par:par + 3:2, 0:ncol], AF.Exp, scale=0.125),
                r=[f"ps{par}", f"ps{2 + par}"], w=[f"pT0{sl_}", f"pT1{sl_}"])

        def emit_pv(n):
            I_, h, j = blocks[n]
            par = n % NPT
            qlo = max(0, j - 4 * I_)
            ncol = 512 - qlo * 128
            jlast = 4 * I_ + 3

            def f(e):
                last = None
                for m in range(2):
                    last = e.matmul(bank(4 + m)[:, qlo * 128:512], V4[:, j, h, 0:128], pT[m][par][:, 0:ncol],
                                    start=(j == 0), stop=(j == jlast))
                last = e.matmul(bank(6)[:, qlo * 128:512], onesm, pT[1][par][:, 0:ncol], start=(j == 0), stop=(j == jlast))
                return last
            pe(f, r=[f"pT0{par}", f"pT1{par}", f"V{j // 16}", "onesm"], w=["ps4", "ps5", "ps6"])
            rc = racc[(I_ * 4 + h) % 2]
            rn = f"racc{(I_ * 4 + h) % 2}"
            if j == 0:
                dve(lambda e: e.tensor_copy(rc, pT[0][par]), r=[f"pT0{par}"], w=[rn])
            else:
                dve(lambda e: e.tensor_tensor(rc[:, qlo * 128:512], rc[:, qlo * 128:512], pT[0][par][:, 0:ncol], ALU.add),
                    r=[f"pT0{par}", rn], w=[rn])
            if j == jlast:
                offs = [0, 1, 2, 4, 6, 9, 10, 12, 13] if I_ >= 3 else ([0, 1, 2, 3, 4, 5, 6, 7, 8] if I_ == 2 else [0] * 9)
                for k_, fn in enumerate(head_steps(I_, h)):
                    pending.append((n + offs[k_], fn))

        def head_steps(I_, h):
            sl = (I_ * 4 + h) % 2
            rc = racc[(I_ * 4 + h) % 2]
            rn = f"racc{(I_ * 4 + h) % 2}"

            def s0():
                dve(lambda e: e.tensor_copy(a1B, bank(5)), r=["ps5"], w=["a1B"])
                dve(lambda e: e.tensor_copy(a0B, bank(4)), r=["ps4"], w=["a0B"])
                dve(lambda e: e.tensor_copy(l1B, bank(6)), r=["ps6"], w=["l1B"])

            def s1():
                pe(lambda e: e.matmul(bank(7), onesf, rc, start=True, stop=True), r=["onesf", rn], w=["ps7"])

            def s2a():
                dve(lambda e: e.reciprocal(rlb[1], l1B), r=["l1B"], w=["rlb1"])

            def s2b():
                dve(lambda e: e.reciprocal(rlb[0], bank(7)), r=["ps7"], w=["rlb0"])

            def s2():
                dve(lambda e: e.tensor_tensor(t2B, a1B, rlb[1], ALU.mult), r=["a1B", "rlb1"], w=["t2B"])
                dve(lambda e: e.tensor_tensor(t1B, a0B, rlb[0], ALU.mult), r=["a0B", "rlb0"], w=["t1B"])
                dve(lambda e: e.scalar_tensor_tensor(oB, t2B, neglam, t1B, ALU.mult, ALU.add), r=["t1B", "t2B", "neglam"], w=["oB"])
                dve(lambda e: e.tensor_tensor(sqB, oB, oB, ALU.mult), r=["oB"], w=["sqB"])

            def s3():
                def ssq_mm(e):
                    last = None
                    for r_ in range(4):
                        last = e.matmul(bank(7)[:, r_:r_ + 1], sqB[:, r_ * 128:(r_ + 1) * 128], onesf[:, 0:1], start=True, stop=True)
                    return last
                pe(ssq_mm, r=["sqB", "onesf"], w=["ps7"])

            def s4():
                dve(lambda e: e.tensor_scalar(v4B, bank(7)[:, 0:4], 1.0 / 128, EPS, ALU.mult, ALU.add), r=["ps7"], w=["v4B"])
                pool(lambda e: e.tensor_tensor(rs4B, v4B, nhalf[:, 0:4], ALU.pow), r=["v4B", "nhalf"], w=["rs4B"])
                for r_ in range(4):
                    dve(lambda e, r_=r_: e.tensor_scalar(RmB[:, r_ * 128:(r_ + 1) * 128], identf, rs4B[:, r_:r_ + 1], None, ALU.mult),
                        r=["rs4B", "identf"], w=[f"RmB{r_}"])

            def s5():
                def bc_mm(e):
                    last = None
                    for r_ in range(4):
                        last = e.matmul(bank(7)[:, r_ * 128:(r_ + 1) * 128], onesf, RmB[:, r_ * 128:(r_ + 1) * 128], start=True, stop=True)
                    return last
                pe(bc_mm, r=[f"RmB{r_}" for r_ in range(4)] + ["onesf"], w=["ps7"])

            def s6():
                dve(lambda e: e.scalar_tensor_tensor(ybT[sl], oB, subgc, bank(7), ALU.mult, ALU.mult), r=["oB", "subgc", "ps7"], w=[f"ybT{sl}"])
                dma("sp", ybT_d[4 * I_:4 * I_ + 4, :, h * 128:(h + 1) * 128].rearrange("r p t -> p r t"),
                    ybT[sl].rearrange("p (r t) -> p r t", t=128), r=[f"ybT{sl}"], w=[f"ybT_d_{S.uid()}"], key=f"ybT{sl}")
            return [s0, s1, s2a, s2b, s2, s3, s4, s5, s6]

        def warmup(nmm, bk):
            def f(e):
                last = None
                for _ in range(nmm):
                    last = e.matmul(bank(bk), ident, KT3[:, 0, 0:512], start=True, stop=True)
                return last
            pe(f, r=["ident", "KT0"], w=[f"ps{bk}"])

        pending = []
        if n_st > 0:
            load_q(0)
        for n in range(len(blocks) + 16):
            if n < len(blocks):
                I_, h, j = blocks[n]
                if h == 0 and j == 0:
                    if I_ + 1 < n_st:
                        load_q(I_ + 1)
                    warmup(20, n % 2)
                emit_qk(n)
            if 1 <= n <= len(blocks):
                emit_pv(n - 1)
            due = [p for p in pending if p[0] <= n - 1]
            pending[:] = [p for p in pending if p[0] > n - 1]
            for _, fn in due:
                fn()
        assert not pending
        S.barrier()
        A.release(mark_phase)
        if stop_after == "B":
            return finish()

        wg = A.bf16(8 * 2048)
        wg3 = wg.rearrange("p (c n) -> p c n", n=2048)
        wa = A.bf16(4 * 1024)
        wa3 = wa.rearrange("p (c n) -> p c n", n=1024)
        wb = A.bf16(4 * 1024)
        wb3 = wb.rearrange("p (c n) -> p c n", n=1024)
        wo = A.bf16(8 * 1024)
        wo3 = wo.rearrange("p (c n) -> p c n", n=1024)
        wr = A.f32(8 * 36)
        wr3 = wr.rearrange("p (c n) -> p c n", n=36)
        gffn = A.f32(D)
        brt = A.f32(36)
        gmixC = A.f32(8)
        dma("sp", gmixC, gmix_d, w=["gmixC"], key="gmixC")
        dma("sp", gffn, gffn_d, w=["gffn"], key="gffn")
        dma("sp", brt, br_d, w=["brt"], key="brt")
        dma("sp", wr3, wr_d.rearrange("(c p) n -> p c n", p=128), w=["wr"], key="wr")
        mC = A.mark()
        stg = [A.f32(2048), A.f32(2048)]
        nld = [0]

        def wload(src, ncol, dst, scale_ap=None, scale_f=None, dname=None):
            sl = nld[0] % 2
            nld[0] += 1
            dma("sp", stg[sl][:, 0:ncol], src, w=[f"stg{sl}"], key=f"stg{sl}")
            if sl == 0:
                if scale_ap is not None:
                    dve(lambda e: e.tensor_scalar(dst, stg[sl][:, 0:ncol], scale_ap, None, ALU.mult), r=[f"stg{sl}", "gmixC"], w=[dname])
                elif scale_f is not None:
                    dve(lambda e: e.tensor_scalar(dst, stg[sl][:, 0:ncol], scale_f, None, ALU.mult), r=[f"stg{sl}"], w=[dname])
                else:
                    dve(lambda e: e.tensor_copy(dst, stg[sl][:, 0:ncol]), r=[f"stg{sl}"], w=[dname])
            else:
                sc = scale_ap if scale_ap is not None else (scale_f if scale_f is not None else 1.0)
                act(lambda e: e.activation(dst, stg[sl][:, 0:ncol], AF.Copy, scale=sc), r=[f"stg{sl}", "gmixC"], w=[dname])
        for c in range(8):
            wload(win_d[c * 128:(c + 1) * 128, NC_IN:4608], 2048, wg3[:, c, :], scale_ap=gmixC[:, c:c + 1], dname=f"wg{c}")
        for c in range(4):
            wload(wa_d[c * 128:(c + 1) * 128, :], 1024, wa3[:, c, :], dname=f"wa{c}")
            wload(wb_d[c * 128:(c + 1) * 128, :], 1024, wb3[:, c, :], dname=f"wb{c}")
        for c in range(8):
            wload(wo_d[c * 128:(c + 1) * 128, :], 1024, wo3[:, c, :], scale_f=0.5, dname=f"wo{c}")
        S.barrier()
        A.release(mC)
        wgres = [f"wg{c}" for c in range(8)]

        xtC = [A.f32(D) for _ in range(5)]
        junkC = A.bf16(D)
        ssC = [A.f32(1) for _ in range(2)]
        vC = [A.f32(1) for _ in range(2)]
        rsC = [A.f32(1) for _ in range(2)]
        hbC = [A.bf16(D) for _ in range(2)]
        hTC = [A.bf16(D) for _ in range(2)]
        yaL = [A.bf16(512) for _ in range(3)]
        ybL = [A.bf16(512) for _ in range(3)]
        th = A.f32(2048)
        m1 = A.f32(D)
        m2 = A.f32(D)
        mbs = [A.bf16(D) for _ in range(2)]
        mTs = [A.bf16(D) for _ in range(2)]
        x1t = [A.f32(D) for _ in range(2)]
        ss2 = A.f32(1)
        v2 = A.f32(1)
        rs2 = A.f32(1)
        h2fs = [A.f32(D) for _ in range(2)]
        h2b = [A.bf16(D) for _ in range(4)]
        h2Ts = [A.f32(D) for _ in range(2)]
        Lg = A.f32(36)
        sm = A.f32(16)
        goh = A.f32(4)
        gex = A.f32(4)
        pen = A.f32(4)
        elm = A.f32(32)
        top8 = A.f32(8)
        oh1s = [A.f32(32) for _ in range(2)]
        oh2s = [A.f32(32) for _ in range(2)]
        Mbs = [A.bf16(32) for _ in range(2)]
        posC = A.f32(32)
        tmp32 = A.f32(32)

        def stageC0(i):
            xs = i % 5
            s2 = i % 2
            s3 = i % 3
            dma("sp", xtC[xs], x_d[i * 128:(i + 1) * 128, :], w=[f"xtC{xs}"], key=f"xtC{xs}")
            dma("sp", yaL[s3], yaT_d[i], r=["yaT_d"], w=[f"yaL{s3}"], key=f"yaL{s3}")
            dma("sp", ybL[s3], ybT_d[i], r=["ybT_d"], w=[f"ybL{s3}"], key=f"ybL{s3}")
            act(lambda e: e.activation(junkC, xtC[xs], AF.Square, accum_out=ssC[s2]), r=[f"xtC{xs}"], w=["junkC", f"Css{s2}"])
            dve(lambda e: e.tensor_scalar(vC[s2], ssC[s2], 1.0 / D, EPS, ALU.mult, ALU.add), r=[f"Css{s2}"], w=[f"Cv{s2}"])
            pool(lambda e: e.tensor_tensor(rsC[s2], vC[s2], nhalf[:, 0:1], ALU.pow), r=[f"Cv{s2}", "nhalf"], w=[f"Crs{s2}"])
            dve(lambda e: e.tensor_scalar(hbC[s2], xtC[xs], rsC[s2], None, ALU.mult), r=[f"xtC{xs}", f"Crs{s2}"], w=[f"hbC{s2}"])

        def stageC1(i):
            s2 = i % 2

            def tr(e):
                last = None
                for c in range(8):
                    last = e.transpose(bankbf(0)[:, c * 128:(c + 1) * 128], hbC[s2][:, c * 128:(c + 1) * 128], ident)
                return last
            pe(tr, r=[f"hbC{s2}", "ident"], w=["ps0"])
            act(lambda e: e.copy(hTC[s2], bankbf(0)), r=["ps0"], w=[f"hTC{s2}"])

        def stageC2(i):
            s2 = i % 2
            s3 = i % 3
            hT3 = hTC[s2].rearrange("p (c t) -> p c t", t=128)
            for cg in range(4):
                bk = 1 + cg % 2

                def mm(e, cg=cg, bk=bk):
                    last = None
                    for c in range(8):
                        last = e.matmul(bank(bk), hT3[:, c, :], wg3[:, c, cg * 512:(cg + 1) * 512], start=(c == 0), stop=(c == 7))
                    return last
                pe(mm, r=[f"hTC{s2}"] + wgres, w=[f"ps{bk}"])
                act(lambda e, cg=cg, bk=bk: e.activation(th[:, cg * 512:(cg + 1) * 512], bank(bk), AF.Tanh, scale=0.5),
                    r=[f"ps{bk}"], w=[f"th{cg}"])
            yl3 = yaL[s3].rearrange("p (c t) -> p c t", t=128)
            bl3 = ybL[s3].rearrange("p (c t) -> p c t", t=128)
            for half in range(2):
                def mma(e, half=half):
                    last = None
                    for c in range(4):
                        last = e.matmul(bank(3 + half), yl3[:, c, :], wa3[:, c, half * 512:(half + 1) * 512], start=(c == 0), stop=(c == 3))
                    return last
                pe(mma, r=[f"yaL{s3}"] + [f"wa{c}" for c in range(4)], w=[f"ps{3 + half}"])

                def mmb(e, half=half):
                    last = None
                    for c in range(4):
                        last = e.matmul(bank(5 + half), bl3[:, c, :], wb3[:, c, half * 512:(half + 1) * 512], start=(c == 0), stop=(c == 3))
                    return last
                pe(mmb, r=[f"ybL{s3}"] + [f"wb{c}" for c in range(4)], w=[f"ps{5 + half}"])
            for half in range(2):
                dve(lambda e, half=half: e.scalar_tensor_tensor(m1[:, half * 512:(half + 1) * 512], th[:, half * 512:(half + 1) * 512], 1.0,
                                                                bank(3 + half), ALU.add, ALU.mult),
                    r=[f"th{half}", f"ps{3 + half}"], w=[f"m1{half}"])
                dve(lambda e, half=half: e.scalar_tensor_tensor(m2[:, half * 512:(half + 1) * 512], th[:, 1024 + half * 512:1024 + (half + 1) * 512], 1.0,
                                                                bank(5 + half), ALU.add, ALU.mult),
                    r=[f"th{2 + half}", f"ps{5 + half}"], w=[f"m2{half}"])
            dve(lambda e: e.tensor_tensor(mbs[s2], m1, m2, ALU.add), r=["m10", "m11", "m20", "m21"], w=[f"mb{s2}"])

        def stageC3(i):
            s2 = i % 2

            def trm(e):
                last = None
                for c in range(8):
                    last = e.transpose(bankbf(0)[:, c * 128:(c + 1) * 128], mbs[s2][:, c * 128:(c + 1) * 128], ident)
                return last
            pe(trm, r=[f"mb{s2}", "ident"], w=["ps0"])
            act(lambda e: e.copy(mTs[s2], bankbf(0)), r=["ps0"], w=[f"mT{s2}"])

        def stageC4o(i):
            xs = i % 5
            s2 = i % 2
            s4 = i % 4
            mT3 = mTs[s2].rearrange("p (c t) -> p c t", t=128)
            for half in range(2):
                def mmo(e, half=half):
                    last = None
                    for c in range(8):
                        last = e.matmul(bank(1 + half), mT3[:, c, :], wo3[:, c, half * 512:(half + 1) * 512], start=(c == 0), stop=(c == 7))
                    return last
                pe(mmo, r=[f"mT{s2}"] + [f"wo{c}" for c in range(8)], w=[f"ps{1 + half}"])
                dve(lambda e, half=half: e.tensor_tensor(x1t[s2][:, half * 512:(half + 1) * 512], xtC[xs][:, half * 512:(half + 1) * 512],
                                                         bank(1 + half), ALU.add),
                    r=[f"xtC{xs}", f"ps{1 + half}"], w=[f"x1t{s2}_{half}"])
            x1res = [f"x1t{s2}_0", f"x1t{s2}_1"]
            dma("sp", x1_d[i * 128:(i + 1) * 128, :], x1t[s2], r=x1res, w=[f"x1_d_{S.uid()}"], key=f"x1t{s2}")
            act(lambda e: e.activation(junkC, x1t[s2], AF.Square, accum_out=ss2), r=x1res, w=["junkC", "ss2"])
            dve(lambda e: e.tensor_scalar(v2, ss2, 1.0 / D, EPS, ALU.mult, ALU.add), r=["ss2"], w=["v2"])
            pool(lambda e: e.tensor_tensor(rs2, v2, nhalf[:, 0:1], ALU.pow), r=["v2", "nhalf"], w=["rs2"])
            dve(lambda e: e.scalar_tensor_tensor(h2fs[s2], x1t[s2], rs2, gffn, ALU.mult, ALU.mult), r=x1res + ["rs2", "gffn"], w=[f"h2f{s2}"])
            act(lambda e: e.copy(h2b[s4], h2fs[s2]), r=[f"h2f{s2}"], w=[f"h2b{s4}"])

        def stageC5(i):
            s2 = i % 2
            h2f = h2fs[s2]
            h2T = h2Ts[s2]

            def trr(e):
                last = None
                for c in range(8):
                    last = e.transpose(ps[:, 3 + c // 4, (c % 4) * 128:(c % 4 + 1) * 128], h2f[:, c * 128:(c + 1) * 128], identf)
                return last
            pe(trr, r=[f"h2f{s2}", "identf"], w=["ps3", "ps4"])
            act(lambda e: e.copy(h2T[:, 0:512], bank(3)), r=["ps3"], w=[f"h2Ta{s2}"])
            act(lambda e: e.copy(h2T[:, 512:1024], bank(4)), r=["ps4"], w=[f"h2Tb{s2}"])

        def stageC6(i):
            s2 = i % 2
            oh1, oh2, Mb = oh1s[s2], oh2s[s2], Mbs[s2]
            h2T3 = h2Ts[s2].rearrange("p (c t) -> p c t", t=128)

            def mmr(e):
                last = None
                for c in range(8):
                    last = e.matmul(bank(7)[:, 0:36], h2T3[:, c, :], wr3[:, c, :], start=(c == 0), stop=(c == 7))
                return last
            pe(mmr, r=[f"h2Ta{s2}", f"h2Tb{s2}", "wr"], w=["ps7"])
            dve(lambda e: e.tensor_tensor(Lg, bank(7)[:, 0:36], brt, ALU.add), r=["ps7", "brt"], w=["Lg"])
            dve(lambda e: e.reduce_max(sm[:, 0:1], Lg[:, 0:4], axis=AX.X), r=["Lg"], w=["gmax"])
            dve(lambda e: e.tensor_scalar(goh, Lg[:, 0:4], sm[:, 0:1], None, ALU.is_equal), r=["Lg", "gmax"], w=["goh"])
            dve(lambda e: e.tensor_scalar(sm[:, 1:2], sm[:, 0:1], -1.0, None, ALU.mult), r=["gmax"], w=["negg"])
            act(lambda e: e.activation(gex, Lg[:, 0:4], AF.Exp, bias=sm[:, 1:2], accum_out=sm[:, 2:3]), r=["Lg", "negg"], w=["gex", "gsum"])
            dve(lambda e: e.reciprocal(sm[:, 3:4], sm[:, 2:3]), r=["gsum"], w=["gw"])
            dve(lambda e: e.tensor_scalar(pen, goh, -1.0, 1e30, ALU.add, ALU.mult), r=["goh"], w=["pen"])
            dve(lambda e: e.tensor_tensor(elm.rearrange("p (g e) -> p g e", e=8), Lg[:, 4:36].rearrange("p (g e) -> p g e", e=8),
                                          pen.unsqueeze(2).broadcast_to([128, 4, 8]), ALU.add), r=["Lg", "pen"], w=["elm"])
            dve(lambda e: e.max(top8, elm), r=["elm"], w=["top8"])
            dve(lambda e: e.tensor_scalar(oh1, elm, top8[:, 0:1], None, ALU.is_equal), r=["elm", "top8"], w=[f"oh1_{s2}"])
            dve(lambda e: e.tensor_scalar(oh2, elm, top8[:, 1:2], None, ALU.is_equal), r=["elm", "top8"], w=[f"oh2_{s2}"])
            dve(lambda e: e.tensor_scalar(sm[:, 4:5], top8[:, 0:1], -1.0, None, ALU.mult), r=["top8"], w=["negv1"])
            act(lambda e: e.activation(sm[:, 5:6], top8[:, 1:2], AF.Exp, bias=sm[:, 4:5]), r=["top8", "negv1"], w=["e2"])
            dve(lambda e: e.tensor_scalar(sm[:, 6:7], sm[:, 5:6], 1.0, None, ALU.add), r=["e2"], w=["den"])
            dve(lambda e: e.reciprocal(sm[:, 7:8], sm[:, 6:7]), r=["den"], w=["p1"])
            dve(lambda e: e.tensor_tensor(wts3[:, i, 0:1], sm[:, 7:8], sm[:, 3:4], ALU.mult), r=["p1", "gw"], w=[f"w1_{i}"])
            dve(lambda e: e.tensor_tensor(wts3[:, i, 1:2], wts3[:, i, 0:1], sm[:, 5:6], ALU.mult), r=[f"w1_{i}", "e2"], w=[f"w2_{i}"])
            dve(lambda e: e.tensor_tensor(Mb, oh1, oh2, ALU.add), r=[f"oh1_{s2}", f"oh2_{s2}"], w=[f"Mb_{s2}"])

        def stageC7(i):
            s2 = i % 2
            s4 = i % 4
            oh1, oh2, Mb = oh1s[s2], oh2s[s2], Mbs[s2]
            pe(lambda e: e.matmul(bank(5)[:, 0:32], ustrict, Mb, start=True, stop=True), r=["ustrict", f"Mb_{s2}"], w=["ps5"])
            pe(lambda e: e.matmul(bank(6)[:, 0:32], onesm, Mb, start=True, stop=True), r=["onesm", f"Mb_{s2}"], w=["ps6"])
            dve(lambda e: e.tensor_tensor(posC, bank(5)[:, 0:32], base_cnt, ALU.add), r=["ps5", "base_cnt"], w=["posCr"])
            dve(lambda e: e.tensor_scalar(posC, posC, float(CAP - 1), None, ALU.min), r=["posCr"], w=["posCr"])
            dve(lambda e: e.tensor_tensor(posC, posC, eoff, ALU.add), r=["posCr", "eoff"], w=["posCr"])
            dve(lambda e: e.tensor_tensor(tmp32, posC, oh1, ALU.mult), r=["posCr", f"oh1_{s2}"], w=["tmp32"])
            dve(lambda e: e.reduce_sum(sm[:, 8:9], tmp32, axis=AX.X), r=["tmp32"], w=["s1f"])
            dve(lambda e: e.tensor_tensor(tmp32, posC, oh2, ALU.mult), r=["posCr", f"oh2_{s2}", "tmp32"], w=["tmp32"])
            dve(lambda e: e.reduce_sum(sm[:, 9:10], tmp32, axis=AX.X), r=["tmp32"], w=["s2f"])
            dve(lambda e: e.tensor_copy(slots3[:, i, 0:1], sm[:, 8:9]), r=["s1f"], w=[f"sl1_{i}"])
            dve(lambda e: e.tensor_copy(slots3[:, i, 1:2], sm[:, 9:10]), r=["s2f"], w=[f"sl2_{i}"])
            dve(lambda e: e.tensor_tensor(base_cnt, base_cnt, bank(6)[:, 0:32], ALU.add), r=["ps6", "base_cnt"], w=["base_cnt"])
            for k in range(2):
                S.add("pool", lambda e, k=k: e.indirect_dma_start(
                    out=xs_d, out_offset=bass.IndirectOffsetOnAxis(ap=slots3[:, i, k:k + 1], axis=0),
                    in_=h2b[s4], in_offset=None),
                    reads=[f"sl{k + 1}_{i}", f"h2b{s4}"] + [f"xs_zero_{z}" for z in range(4)], writes=[f"xs_d{k}"], dma=f"scat{k}_{s4}")
            if debug:
                dma("sp", rt_d[:, i, 0:2], wts3[:, i, :], r=[f"w1_{i}", f"w2_{i}"], key="dbgrt")

        stagesC = [stageC0, stageC1, stageC2, stageC3, stageC4o, stageC5, stageC6, stageC7]
        groupsC = [[0, 1, 2, 6], [3, 4, 5, 7]]

        def merge_emit(lists):
            pos_ = [0] * len(lists)
            while True:
                best, bf = None, 2.0
                for li, L_ in enumerate(lists):
                    if pos_[li] < len(L_):
                        f_ = pos_[li] / len(L_)
                        if f_ < bf:
                            best, bf = li, f_
                if best is None:
                    break
                S.add(*lists[best][pos_[best]])
                pos_[best] += 1

        for s_ in range(nt_c + len(stagesC) - 1):
            for grp in groupsC:
                lists = []
                for k_ in grp:
                    if 0 <= s_ - k_ < nt_c:
                        S.begin_record()
                        stagesC[k_](s_ - k_)
                        lists.append(S.end_record())
                merge_emit(lists)
        S.barrier()
        A.release(mark_phase)
        if stop_after == "C":
            return finish()

        w1s = A.f32(8 * 512)
        w3s = A.f32(8 * 512)
        w2s = A.f32(4 * 1024)
        w1b = [A.bf16(8 * 512) for _ in range(2)]
        w3b = [A.bf16(8 * 512) for _ in range(2)]
        w2b = [A.bf16(4 * 1024) for _ in range(2)]
        xg = [A.bf16(D) for _ in range(3)]
        XT = [A.bf16(8 * CAP) for _ in range(2)]
        AT = [A.bf16(4 * CAP) for _ in range(2)]
        thD = [A.f32(CAP // 2) for _ in range(2)]
        a1D = [A.f32(CAP // 2) for _ in range(2)]
        ysb = [A.bf16(D) for _ in range(3)]
        HC = CAP // 2

        def load_w(e_):
            for c in range(8):
                dma("sp", w1s[:, c * 512:(c + 1) * 512], w1_d[e_, c * 128:(c + 1) * 128, :], w=[f"w1s{c}"], key="w1s")
                dma("sp", w3s[:, c * 512:(c + 1) * 512], w3_d[e_, c * 128:(c + 1) * 128, :], w=[f"w3s{c}"], key="w3s")
                dma("sp", w2s[:, c * 512:(c + 1) * 512], w2_d[e_, (c // 2) * 128:(c // 2 + 1) * 128, (c % 2) * 512:(c % 2 + 1) * 512],
                    w=[f"w2s{c}"], key="w2s")

        def cast_w_chunk(e_, c):
            sl = e_ % 2
            act(lambda e: e.copy(w1b[sl][:, c * 512:(c + 1) * 512], w1s[:, c * 512:(c + 1) * 512]), r=[f"w1s{cc}" for cc in range(8)], w=[f"w1b{sl}_{c}"])
            dve(lambda e: e.tensor_copy(w3b[sl][:, c * 512:(c + 1) * 512], w3s[:, c * 512:(c + 1) * 512]), r=[f"w3s{cc}" for cc in range(8)], w=[f"w3b{sl}_{c}"])
            dve(lambda e: e.tensor_scalar(w2b[sl][:, c * 512:(c + 1) * 512], w2s[:, c * 512:(c + 1) * 512], 0.5, None, ALU.mult),
                r=[f"w2s{cc}" for cc in range(8)], w=[f"w2b{sl}_{c}"])

        def cast_w(e_):
            for c in range(8):
                cast_w_chunk(e_, c)

        nblk_ct = [0]

        def xblock(e_, b):
            sl = e_ % 2
            XT3 = XT[sl].rearrange("p (c t) -> p c t", t=CAP)
            g = nblk_ct[0] % 3
            nblk_ct[0] += 1
            blk = e_ * NBLK + b
            tb = 0 if b % 2 == 0 else 7
            dma("sp", xg[g], xs_d[blk * 128:(blk + 1) * 128, :], w=[f"xg{g}"], key=f"xg{g}")

            def trx(e):
                last = None
                for c in range(8):
                    last = e.transpose(bankbf(tb)[:, c * 128:(c + 1) * 128], xg[g][:, c * 128:(c + 1) * 128], ident)
                return last
            pe(trx, r=[f"xg{g}", "ident"], w=[f"ps{tb}"])
            if b % 2 == 0:
                act(lambda e: e.copy(XT3[:, :, b * 128:(b + 1) * 128], bankbf(tb).rearrange("p (c t) -> p c t", t=128)),
                    r=[f"ps{tb}"], w=[f"XT{sl}_{b}"])
            else:
                dve(lambda e: e.tensor_copy(XT3[:, :, b * 128:(b + 1) * 128], bankbf(tb).rearrange("p (c t) -> p c t", t=128)),
                    r=[f"ps{tb}"], w=[f"XT{sl}_{b}"])

        def gate_up_step(e_, k):
            sl = e_ % 2
            XT3 = XT[sl].rearrange("p (c t) -> p c t", t=CAP)
            AT3 = AT[sl].rearrange("p (c t) -> p c t", t=CAP)
            w1b3 = w1b[sl].rearrange("p (c n) -> p c n", n=512)
            w3b3 = w3b[sl].rearrange("p (c n) -> p c n", n=512)
            xtres = [f"XT{sl}_{b}" for b in range(NBLK)]
            dc, half = k // 2, k % 2
            bG = 1 + k % 2
            bU = 3 + k % 2
            t2 = k % 2

            def mmg(e):
                last = None
                for c in range(8):
                    last = e.matmul(bank(bG)[:, 0:HC], w1b3[:, c, dc * 128:(dc + 1) * 128], XT3[:, c, half * HC:(half + 1) * HC],
                                    start=(c == 0), stop=(c == 7))
                return last
            pe(mmg, r=xtres + [f"w1b{sl}_{c}" for c in range(8)], w=[f"ps{bG}"])

            def mmu(e):
                last = None
                for c in range(8):
                    last = e.matmul(bank(bU)[:, 0:HC], w3b3[:, c, dc * 128:(dc + 1) * 128], XT3[:, c, half * HC:(half + 1) * HC],
                                    start=(c == 0), stop=(c == 7))
                return last
            pe(mmu, r=xtres + [f"w3b{sl}_{c}" for c in range(8)], w=[f"ps{bU}"])
            act(lambda e: e.activation(thD[t2], bank(bG)[:, 0:HC], AF.Tanh, scale=0.5), r=[f"ps{bG}"], w=[f"thD{t2}"])
            dve(lambda e: e.scalar_tensor_tensor(a1D[t2], thD[t2], 1.0, bank(bG)[:, 0:HC], ALU.add, ALU.mult),
                r=[f"thD{t2}", f"ps{bG}"], w=[f"a1D{t2}"])
            dve(lambda e: e.tensor_tensor(AT3[:, dc, half * HC:(half + 1) * HC], a1D[t2], bank(bU)[:, 0:HC], ALU.mult),
                r=[f"a1D{t2}", f"ps{bU}"], w=[f"AT{sl}_{k}"])

        def down(e_):
            sl = e_ % 2
            AT3 = AT[sl].rearrange("p (c t) -> p c t", t=CAP)
            w2b3 = w2b[sl].rearrange("p (c n) -> p c n", n=1024)
            atres = [f"AT{sl}_{k}" for k in range(8)]
            for b in range(NBLK):
                blk = e_ * NBLK + b
                ysl = blk % 3
                for cg in range(2):
                    def mmy(e, b=b, cg=cg):
                        last = None
                        for dc in range(4):
                            last = e.matmul(bank(5 + cg), AT3[:, dc, b * 128:(b + 1) * 128], w2b3[:, dc, cg * 512:(cg + 1) * 512],
                                            start=(dc == 0), stop=(dc == 3))
                        return last
                    pe(mmy, r=atres + [f"w2b{sl}_{c}" for c in range(8)], w=[f"ps{5 + cg}"])
                    if cg == 0:
                        act(lambda e, ysl=ysl: e.copy(ysb[ysl][:, 0:512], bank(5)), r=["ps5"], w=[f"ysb{ysl}_0"])
                    else:
                        dve(lambda e, ysl=ysl: e.tensor_copy(ysb[ysl][:, 512:1024], bank(6)), r=["ps6"], w=[f"ysb{ysl}_1"])
                dma("pool", ys_d[blk * 128:(blk + 1) * 128, :], ysb[ysl], r=[f"ysb{ysl}_0", f"ysb{ysl}_1"], w=[f"ys_d_{S.uid()}"], key=f"ysb{ysl}")

        assert NBLK == 8
        if n_exp > 0:
            load_w(0)
            cast_w(0)
            if n_exp > 1:
                load_w(1)
            for b in range(NBLK):
                xblock(0, b)
        for e_ in range(n_exp):
            for k in range(8):
                gate_up_step(e_, k)
                if e_ + 1 < n_exp:
                    xblock(e_ + 1, k)
                    cast_w_chunk(e_ + 1, k)
            if e_ + 2 < n_exp:
                load_w(e_ + 2)
            down(e_)
        S.barrier()
        A.release(mark_phase)
        if stop_after == "D":
            return finish()

        fg = A.f32(D)
        dma("sp", fg, fg_d, w=["fg"], key="fg")
        x1L = [A.f32(D) for _ in range(3)]
        y1L = [A.bf16(D) for _ in range(3)]
        y2L = [A.bf16(D) for _ in range(3)]
        tE = [A.f32(D) for _ in range(2)]
        x2E = [A.f32(D) for _ in range(2)]
        junkE = A.bf16(D)
        ssE = [A.f32(1) for _ in range(2)]
        vE = [A.f32(1) for _ in range(2)]
        rsE = [A.f32(1) for _ in range(2)]
        oE = [A.f32(D) for _ in range(2)]

        def stageE1(i):
            s3 = i % 3
            dma("sp", x1L[s3], x1_d[i * 128:(i + 1) * 128, :], r=["x1_d"], w=[f"x1L{s3}"], key=f"x1L{s3}")
            for k, yL in ((0, y1L), (1, y2L)):
                S.add("pool", lambda e, k=k, yL=yL: e.indirect_dma_start(
                    out=yL[s3], out_offset=None, in_=ys_d,
                    in_offset=bass.IndirectOffsetOnAxis(ap=slots3[:, i, k:k + 1], axis=0)),
                    reads=["ys_d", "slots"], writes=[f"y{k}L{s3}"], dma=f"y{k}L{s3}")

        def stageE2(i):
            s3 = i % 3
            s2 = i % 2
            dve(lambda e: e.scalar_tensor_tensor(tE[s2], y1L[s3], wts3[:, i, 0:1], x1L[s3], ALU.mult, ALU.add),
                r=[f"y0L{s3}", f"x1L{s3}", "wts"], w=[f"tE{s2}"])
            dve(lambda e: e.scalar_tensor_tensor(x2E[s2], y2L[s3], wts3[:, i, 1:2], tE[s2], ALU.mult, ALU.add),
                r=[f"y1L{s3}", f"tE{s2}", "wts"], w=[f"x2E{s2}"])
            act(lambda e: e.activation(junkE, x2E[s2], AF.Square, accum_out=ssE[s2]), r=[f"x2E{s2}"], w=["junkE", f"ssE{s2}"])
            dve(lambda e: e.tensor_scalar(vE[s2], ssE[s2], 1.0 / D, EPS, ALU.mult, ALU.add), r=[f"ssE{s2}"], w=[f"vE{s2}"])
            pool(lambda e: e.tensor_tensor(rsE[s2], vE[s2], nhalf[:, 0:1], ALU.pow), r=[f"vE{s2}", "nhalf"], w=[f"rsE{s2}"])
            dve(lambda e: e.scalar_tensor_tensor(oE[s2], x2E[s2], rsE[s2], fg, ALU.mult, ALU.mult), r=[f"x2E{s2}", f"rsE{s2}", "fg"], w=[f"oE{s2}"])
            dma("sp", out_d[i * 128:(i + 1) * 128, :], oE[s2], r=[f"oE{s2}"], w=[f"out_d_{S.uid()}"], key=f"oE{s2}")

        for s_ in range(NT + 1):
            if s_ < NT:
                stageE1(s_)
            if s_ >= 1:
                stageE2(s_ - 1)
        return finish()


def _bc(v, n=128):
    v = np.asarray(v, dtype=np.float32).reshape(1, -1)
    return np.ascontiguousarray(np.broadcast_to(v, (n, v.shape[1])))


def core_inputs(I, b):
    f = np.float32
    invf = (np.float32(500000.0) ** (-np.arange(8, dtype=np.float32) / np.float32(8))).astype(f)
    return {
        "x": np.ascontiguousarray(I["x"][b]),
        "pos": np.ascontiguousarray(I["positions"][b].reshape(NT, 128).T.astype(np.int32)),
        "invf": _bc(invf),
        "g_mix": np.ascontiguousarray(I["norm_mix_g"][0].reshape(8, 128).T),
        "w_in": I["w_in"][0],
        "lng_bc": _bc(I["gm_ln_g"][0].reshape(-1)),
        "lnb_bc": _bc(I["gm_ln_b"][0].reshape(-1)),
        "wsT": np.ascontiguousarray(I["gm_w_s"][0].transpose(2, 0, 1)),
        "bs": np.ascontiguousarray(I["gm_b_s"][0].T),
        "lamv": np.ascontiguousarray(np.broadcast_to(
            np.stack([I["lam_q1"][0], I["lam_k1"][0], I["lam_q2"][0], I["lam_k2"][0]])[None], (128, 4, 64))).astype(f),
        "subg_col": np.ascontiguousarray(I["da_subln_g"][0].reshape(128, 1).astype(np.float32)),
        "w_br_a": I["w_br_a"][0],
        "w_br_b": I["w_br_b"][0],
        "w_out": I["w_out"][0],
        "g_ffn_bc": _bc(I["norm_ffn_g"][0]),
        "w_r": np.ascontiguousarray(np.concatenate([I["w_router_group"][0], I["w_router_expert"][0]], axis=1)),
        "b_r": _bc(np.concatenate([I["b_router_group"][0], I["b_router_expert"][0]])),
        "w1": I["w_exp_gate"][0],
        "w3": I["w_exp_up"][0],
        "w2": I["w_exp_down"][0],
        "fg_bc": _bc(I["final_norm_g"]),
    }


_CACHE = {}


def kernel(**inputs):
    I = {k: np.asarray(v) for k, v in inputs.items()}
    if "nc" not in _CACHE:
        _CACHE["nc"] = build_program()
    nc = _CACHE["nc"]
    in_maps = [core_inputs(I, b) for b in range(8)]
    res = run_bass_kernel_spmd(nc, in_maps, core_ids=list(range(8)))
    return np.stack([np.asarray(r["out"], dtype=np.float32) for r in res.results], axis=0)
```

```python
import math
from contextlib import ExitStack

import numpy as np
import concourse.bass as bass
import concourse.mybir as mybir
from concourse.bass_utils import run_bass_kernel_spmd

F32 = mybir.dt.float32
BF16 = mybir.dt.bfloat16
I32 = mybir.dt.int32
U32 = mybir.dt.uint32
ALU = mybir.AluOpType
AF = mybir.ActivationFunctionType
AX = mybir.AxisListType

ENGS = ("pe", "act", "dve", "pool", "sp")
import os as _os
NOSELF = tuple(x for x in _os.environ.get('KNOSELF', '').split(',') if x)


class _Op:
    __slots__ = ("eng", "fn", "deps", "semkey", "sigval", "needs_sig", "is_dma", "idx")

    def __init__(self, eng, fn, semkey, is_dma):
        self.eng = eng
        self.fn = fn
        self.deps = {}
        self.semkey = semkey
        self.sigval = None
        self.needs_sig = is_dma
        self.is_dma = is_dma


class Sched:
    def __init__(self, nc, stack):
        self.nc = nc
        self.stack = stack
        self.ops = {e: [] for e in ENGS}
        self.lastw = {}
        self.readers = {}
        self.sems = {}
        self.semcount = {}
        self.all_ops = []
        self.keep_prefix = None
        self._uid = 0

    def uid(self):
        self._uid += 1
        return self._uid

    def _sem(self, key):
        if key not in self.sems:
            name = "s_" + str(key).replace(" ", "").replace("(", "").replace(")", "").replace(",", "_").replace("'", "")
            self.sems[key] = self.stack.enter_context(self.nc.semaphore(name[:40]))
            self.semcount[key] = 0
        return self.sems[key]

    def begin_record(self):
        self._rec = []

    def end_record(self):
        r, self._rec = self._rec, None
        return r

    def add(self, eng, fn, reads=(), writes=(), dma=None):
        if getattr(self, "_rec", None) is not None:
            self._rec.append((eng, fn, tuple(reads), tuple(writes), dma))
            return None
        is_dma = dma is not None
        semkey = ("dma", dma) if is_dma else ("eng", eng)
        op = _Op(eng, fn, semkey, is_dma)
        self._sem(semkey)
        deps = {}

        def dep_on(o):
            if o is None or o is op:
                return
            if (not o.is_dma) and o.eng == "pe" and eng == "pe" and not is_dma:
                return
            if NOSELF and (not o.is_dma) and (not is_dma) and o.eng == eng and eng in NOSELF:
                return
            cur = deps.get(o.semkey)
            if cur is None or cur.idx < o.idx:
                deps[o.semkey] = o

        for r in reads:
            dep_on(self.lastw.get(r))
            if r.startswith("ps"):
                for o in self.readers.get(r, {}).values():
                    if o.eng != eng:
                        dep_on(o)
        for r in writes:
            dep_on(self.lastw.get(r))
            for o in self.readers.get(r, {}).values():
                dep_on(o)
        op.idx = len(self.all_ops)
        self.all_ops.append(op)
        for r in reads:
            self.readers.setdefault(r, {})[semkey] = op
        for r in writes:
            self.lastw[r] = op
            self.readers[r] = {}
        for o in deps.values():
            o.needs_sig = True
        op.deps = deps
        self.ops[eng].append(op)
        return op

    def barrier(self):
        tok = ("__barrier__",)
        last = []
        for e in ENGS:
            for o in reversed(self.ops[e]):
                if o.fn is not None:
                    last.append(o)
                    break
        lastdma = {}
        for o in self.all_ops:
            if o.is_dma:
                lastdma[o.semkey] = o
        keep_res = {r: o for r, o in self.lastw.items() if r.startswith(self.keep_prefix)} if self.keep_prefix else {}
        excl = {o.semkey for o in keep_res.values()}
        every = {o.semkey: o for o in last if not o.is_dma}
        every.update({k: o for k, o in lastdma.items() if k not in excl})
        for e in ENGS:
            op = _Op(e, None, ("eng", e), False)
            op.idx = len(self.all_ops)
            self.all_ops.append(op)
            op.deps = {k: o for k, o in every.items() if not (k == ("eng", "pe") and e == "pe")}
            for o in op.deps.values():
                o.needs_sig = True
            self.ops[e].append(op)
        self.lastw = dict(keep_res)
        self.readers = {}

    def finalize_and_emit(self, final_waits=()):
        nc = self.nc
        for o in self.all_ops:
            if o.fn is None:
                continue
            if o.needs_sig:
                inc = 16 if o.is_dma else 1
                self.semcount[o.semkey] += inc
                o.sigval = self.semcount[o.semkey]
        engmap = {"pe": "tensor", "act": "scalar", "dve": "vector", "pool": "gpsimd", "sp": "sync"}
        final = [(self.sems[o.semkey], o.sigval) for o in final_waits]

        def run(engname, eng):
            known = {}
            for o in self.ops[engname]:
                for k, d in o.deps.items():
                    v = d.sigval
                    assert v is not None, (k, d.eng)
                    if known.get(k, 0) >= v:
                        continue
                    known[k] = v
                    eng.wait_ge(self.sems[k], v)
                if o.fn is None:
                    continue
                inst = o.fn(eng)
                if o.needs_sig:
                    assert inst is not None
                    inst.then_inc(self.sems[o.semkey], 16 if o.is_dma else 1)
            if engname == "sp":
                for s, v in final:
                    eng.wait_ge(s, v)

        with nc.Block() as block:
            for engname in ENGS:
                getattr(block, engmap[engname])(lambda eng, _n=engname: run(_n, eng))


D = 1024
T = 8192
NT = T // 128
NC_IN = 2560
NEXP = 32
CAP = 1024
NBLK = CAP // 128
NSLOT = NEXP * CAP
EPS = 1e-6
LAM_INIT = 0.8 - 0.6 * math.exp(0.0)
TWO_PI = 2.0 * math.pi
C1 = 6.28125
C2 = TWO_PI - C1


INPUT_NAMES = []
import os
STAGES = os.environ.get('KSTAGES', '123')


class Arena:
    def __init__(self, nc, st, nbytes):
        self.t = st.enter_context(nc.sbuf_tensor("arena", [128, nbytes // 4], F32))
        self.off = 0
        self.cap = nbytes

    def _take(self, nbytes):
        nbytes = (nbytes + 31) // 32 * 32
        o = self.off
        self.off += nbytes
        assert self.off <= self.cap, ("SBUF arena overflow", self.off, self.cap)
        return o

    def f32(self, n):
        o = self._take(n * 4)
        return self.t[:, o // 4:o // 4 + n]

    def i32(self, n):
        return self.f32(n).bitcast(I32)

    def bf16(self, n):
        n2 = (n + 1) // 2 * 2
        o = self._take(n2 * 2)
        return self.t[:, o // 4:o // 4 + n2 // 2].bitcast(BF16)[:, 0:n]

    def mark(self):
        return self.off

    def release(self, m):
        self.off = m


def build_program(debug=False, stop_after=None, nt_a=NT, nt_b=NT, nt_c=NT, n_exp=NEXP):
    nc = bass.Bass("TRN2", target_bir_lowering=False)
    okind = "ExternalOutput" if debug else "Internal"

    early = stop_after in ("setup", "win", "A", "B", "C")
    INPUT_NAMES.clear()

    def din(name, shape, dt=F32):
        if early and name in ("w1", "w3", "w2"):
            return None
        INPUT_NAMES.append(name)
        return nc.dram_tensor(name, list(shape), dt, kind="ExternalInput").ap()

    def dscr(name, shape, dt):
        return nc.dram_tensor(name, list(shape), dt, kind=okind).ap()

    x_d = din("x", [T, D])
    pos_d = din("pos", [128, NT], I32)
    invf_d = din("invf", [128, 8])
    gmix_d = din("g_mix", [128, 8])
    win_d = din("w_in", [D, 4608])
    lng_d = din("lng_bc", [128, 512])
    lnb_d = din("lnb_bc", [128, 512])
    wsT_d = din("wsT", [128, 4, 128])
    bs_d = din("bs", [128, 4])
    lamv_d = din("lamv", [128, 4, 64])
    subgc_d = din("subg_col", [128, 1])
    wa_d = din("w_br_a", [512, D])
    wb_d = din("w_br_b", [512, D])
    wo_d = din("w_out", [D, D])
    gffn_d = din("g_ffn_bc", [128, D])
    wr_d = din("w_r", [D, 36])
    br_d = din("b_r", [128, 36])
    w1_d = din("w1", [NEXP, D, 512])
    w3_d = din("w3", [NEXP, D, 512])
    w2_d = din("w2", [NEXP, 512, D])
    fg_d = din("fg_bc", [128, D])
    out_d = nc.dram_tensor("out", [T, D], F32, kind="ExternalOutput").ap()

    qT_d = dscr("qT_s", [128, 4, T], BF16)
    kT_d = dscr("kT_s", [128, 4, T], BF16)
    v_d = dscr("v_s", [128, NT, 4, 130], BF16)
    yaT_d = dscr("yaT_s", [NT, 128, 512], BF16)
    ybT_d = dscr("ybT_s", [NT, 128, 512], BF16)
    x1_d = dscr("x1_s", [T, D], F32)
    xs_d = dscr("xs_s", [NSLOT, D], BF16)
    ys_d = dscr("ys_s", [NSLOT, D], BF16)
    rt_d = dscr("rt_s", [128, NT, 4], F32) if debug else None

    with ExitStack() as st:
        S = Sched(nc, st)
        A = Arena(nc, st, 192 * 1024)
        ps = st.enter_context(nc.psum_tensor("ps", [128, 8, 512], F32))

        def bank(b):
            return ps[:, b, :]

        def finish():
            lastd = {}
            for o in S.all_ops:
                if o.is_dma:
                    lastd[o.semkey] = o
            S.finalize_and_emit(final_waits=list(lastd.values()))
            return nc

        def bankbf(b):
            return ps[:, b, :].bitcast(BF16)

        dve = lambda fn, r=(), w=(): S.add("dve", fn, r, w)
        act = lambda fn, r=(), w=(): S.add("act", fn, r, w)
        pool = lambda fn, r=(), w=(): S.add("pool", fn, r, w)
        pe = lambda fn, r=(), w=(): S.add("pe", fn, r, w)

        def dma(q, out, in_, r=(), w=(), key=None):
            return S.add(q, lambda e: e.dma_start(out=out, in_=in_), r, w, dma=key)

        ident = A.bf16(128)
        identf = A.f32(128)
        ustrict = A.bf16(128)
        onesm = A.bf16(128)
        maskb = A.bf16(128)
        ctmp = A.f32(128)
        nhalf = A.f32(8)
        pool(lambda e: e.memset(nhalf, -0.5), w=["nhalf"])
        pool(lambda e: e.memset(identf, 0.0), w=["identf"])
        pool(lambda e: e.affine_select(identf, identf, [[-1, 128]], ALU.not_equal, 1.0, base=0,
                                       channel_multiplier=1), r=["identf"], w=["identf"])
        dve(lambda e: e.tensor_copy(ident, identf), r=["identf"], w=["ident"])
        pool(lambda e: e.memset(ctmp, 1.0), w=["ctmp"])
        pool(lambda e: e.affine_select(ctmp, ctmp, [[1, 128]], ALU.is_gt, 0.0, base=0,
                                       channel_multiplier=-1), r=["ctmp"], w=["ctmp"])
        dve(lambda e: e.tensor_copy(ustrict, ctmp), r=["ctmp"], w=["ustrict"])
        pool(lambda e: e.memset(ctmp, 0.0), r=["ctmp"], w=["ctmp"])
        pool(lambda e: e.affine_select(ctmp, ctmp, [[1, 128]], ALU.is_ge, -30000.0, base=0,
                                       channel_multiplier=-1), r=["ctmp"], w=["ctmp"])
        dve(lambda e: e.tensor_copy(maskb, ctmp), r=["ctmp"], w=["maskb"])
        dve(lambda e: e.memset(onesm, 1.0), w=["onesm"])

        slots_i = A.i32(NT * 2)
        wts = A.f32(NT * 2)
        slots3 = slots_i.rearrange("p (t k) -> p t k", k=2)
        wts3 = wts.rearrange("p (t k) -> p t k", k=2)
        tokid = A.i32(NT)
        pool(lambda e: e.iota(tokid, [[128, NT]], base=0, channel_multiplier=1), w=["tokid"])
        eoff_i = A.i32(NEXP)
        eoff = A.f32(NEXP)
        pool(lambda e: e.iota(eoff_i, [[CAP, NEXP]], base=0, channel_multiplier=0), w=["eoff_i"])
        dve(lambda e: e.tensor_copy(eoff, eoff_i), r=["eoff_i"], w=["eoff"])
        base_cnt = A.f32(NEXP)
        dve(lambda e: e.memset(base_cnt, 0.0), w=["base_cnt"])
        S.keep_prefix = "xs_zero_"
        ztile = A.bf16(4 * D)
        dve(lambda e: e.memset(ztile, 0.0), w=["ztile"])
        for z_ in range(NSLOT // 512):
            dma("act", xs_d[z_ * 512:(z_ + 1) * 512, :].rearrange("(n p) d -> p n d", p=128),
                ztile.rearrange("p (n d) -> p n d", d=D), r=["ztile"], w=["xs_zero_" + str(z_ % 4)], key=f"zfill{z_ % 4}")

        lamv = A.f32(256)
        lamp = A.f32(128)
        lsum = A.f32(2)
        lam_e = A.f32(2)
        neglam = A.f32(1)
        dma("sp", lamv, lamv_d.rearrange("p a b -> p (a b)"), w=["lamv"], key="lamv")
        lamv3 = lamv.rearrange("p (a b) -> p a b", b=64)
        dve(lambda e: e.tensor_tensor(lamp[:, 0:64], lamv3[:, 0, :], lamv3[:, 1, :], ALU.mult), r=["lamv"], w=["lamp"])
        dve(lambda e: e.tensor_tensor(lamp[:, 64:128], lamv3[:, 2, :], lamv3[:, 3, :], ALU.mult), r=["lamp", "lamv"], w=["lamp"])
        dve(lambda e: e.reduce_sum(lsum, lamp.rearrange("p (a b) -> p a b", b=64), axis=AX.X), r=["lamp"], w=["lsum"])
        act(lambda e: e.activation(lam_e, lsum, AF.Exp), r=["lsum"], w=["lam_e"])
        dve(lambda e: e.scalar_tensor_tensor(neglam, lam_e[:, 1:2], -LAM_INIT, lam_e[:, 0:1], ALU.add, ALU.subtract),
            r=["lam_e"], w=["neglam"])

        cos_t = A.f32(NT * 8)
        sin_t = A.f32(NT * 8)
        m0 = A.mark()
        pos_i = A.i32(NT)
        posf = A.f32(NT)
        invf = A.f32(8)
        ang = A.f32(NT * 8)
        kf = A.f32(NT * 8)
        ki = A.i32(NT * 8)
        rr = A.f32(NT * 8)
        r2 = A.f32(NT * 8)
        msk = A.f32(NT * 8)
        dma("sp", pos_i, pos_d, w=["pos_i"], key="pos_i")
        dma("sp", invf, invf_d, w=["invf"], key="invf")
        dve(lambda e: e.tensor_copy(posf, pos_i), r=["pos_i"], w=["posf"])
        ang3 = ang.rearrange("p (t j) -> p t j", j=8)
        dve(lambda e: e.tensor_tensor(ang3, posf.unsqueeze(2).broadcast_to([128, NT, 8]),
                                      invf.unsqueeze(1).broadcast_to([128, NT, 8]), ALU.mult),
            r=["posf", "invf"], w=["ang"])
        dve(lambda e: e.tensor_scalar(kf, ang, 1.0 / TWO_PI, None, ALU.mult), r=["ang"], w=["kf"])
        dve(lambda e: e.tensor_copy(ki, kf), r=["kf"], w=["ki"])
        dve(lambda e: e.tensor_copy(kf, ki), r=["ki"], w=["kf"])
        dve(lambda e: e.scalar_tensor_tensor(rr, kf, -C1, ang, ALU.mult, ALU.add), r=["kf", "ang"], w=["rr"])
        dve(lambda e: e.scalar_tensor_tensor(rr, kf, -C2, rr, ALU.mult, ALU.add), r=["kf", "rr"], w=["rr"])
        dve(lambda e: e.tensor_scalar(r2, rr, math.pi / 2, None, ALU.add), r=["rr"], w=["r2"])
        dve(lambda e: e.tensor_scalar(msk, r2, math.pi, None, ALU.is_gt), r=["r2"], w=["msk"])
        dve(lambda e: e.scalar_tensor_tensor(r2, msk, -TWO_PI, r2, ALU.mult, ALU.add), r=["msk", "r2"], w=["r2"])
        PI_SAFE = 3.1415925
        dve(lambda e: e.tensor_scalar(rr, rr, PI_SAFE, -PI_SAFE, ALU.min, ALU.max), r=["rr"], w=["rr"])
        dve(lambda e: e.tensor_scalar(r2, r2, PI_SAFE, -PI_SAFE, ALU.min, ALU.max), r=["r2"], w=["r2"])
        act(lambda e: e.activation(sin_t, rr, AF.Sin), r=["rr"], w=["sin_t"])
        act(lambda e: e.activation(cos_t, r2, AF.Sin), r=["r2"], w=["cos_t"])
        if debug:
            dbg_cs = nc.dram_tensor("dbg_cs", [128, 2, NT * 8], F32, kind="ExternalOutput").ap()
            dma("sp", dbg_cs[:, 0, :], cos_t, r=["cos_t"], key="dbgc")
            dma("sp", dbg_cs[:, 1, :], sin_t, r=["sin_t"], key="dbgs")
        S.barrier()
        A.release(m0)
        if stop_after == "setup":
            return finish()
        cos3 = cos_t.rearrange("p (t j) -> p t j", j=8)
        sin3 = sin_t.rearrange("p (t j) -> p t j", j=8)
        mark_phase = A.mark()

        def emit_rstd(ss, vtmp, rstd, n, scale, rname):
            dve(lambda e: e.tensor_scalar(vtmp, ss, scale, EPS, ALU.mult, ALU.add), r=[rname + "ss"], w=[rname + "v"])
            pool(lambda e: e.tensor_tensor(rstd, vtmp, nhalf[:, 0:n], ALU.pow), r=[rname + "v", "nhalf"], w=[rname + "rstd"])

        win = A.bf16(8 * NC_IN)
        win3 = win.rearrange("p (c n) -> p c n", n=NC_IN)
        gmix = A.f32(8)
        dma("sp", gmix, gmix_d, w=["gmix"], key="gmix")
        mA = A.mark()
        wst = [A.f32(NC_IN), A.f32(NC_IN)]
        for c in range(8):
            sl = c % 2
            dma("sp", wst[sl], win_d[c * 128:(c + 1) * 128, 0:NC_IN], w=[f"wst{sl}"], key=f"wst{sl}")
            if c % 2 == 0:
                dve(lambda e, c=c, sl=sl: e.tensor_scalar(win3[:, c, :], wst[sl], gmix[:, c:c + 1], None, ALU.mult),
                    r=[f"wst{sl}", "gmix"], w=[f"win{c}"])
            else:
                act(lambda e, c=c, sl=sl: e.activation(win3[:, c, :], wst[sl], AF.Copy, scale=gmix[:, c:c + 1]),
                    r=[f"wst{sl}", "gmix"], w=[f"win{c}"])
        S.barrier()
        A.release(mA)
        if stop_after == "win":
            dbg_w = nc.dram_tensor("dbg_w", [128, 8 * NC_IN], BF16, kind="ExternalOutput").ap()
            dma("sp", dbg_w, win, r=[f"win{c}" for c in range(8)], key="dbgw")
            return finish()
        winres = [f"win{c}" for c in range(8)]

        lng = A.f32(512)
        lnb = A.f32(512)
        wsTf = A.f32(512)
        wsT = A.bf16(512)
        bs = A.f32(4)
        dma("sp", lng, lng_d, w=["lng"], key="lng")
        dma("sp", lnb, lnb_d, w=["lnb"], key="lnb")
        dma("sp", wsTf, wsT_d.rearrange("p g t -> p (g t)"), w=["wsTf"], key="wsTf")
        dma("sp", bs, bs_d, w=["bs"], key="bs")
        pool(lambda e: e.affine_select(wsTf, wsTf, [[0, 4], [1, 128]], ALU.is_ge, 0.0, base=0,
                                       channel_multiplier=-1), r=["wsTf"], w=["wsTf"])
        dve(lambda e: e.tensor_copy(wsT, wsTf), r=["wsTf"], w=["wsT"])
        wsT3 = wsT.rearrange("p (g t) -> p g t", t=128)

        NXS = 3
        xt = [A.f32(D) for _ in range(NXS)]
        junk = A.bf16(D)
        ssA = [A.f32(1) for _ in range(2)]
        vA = [A.f32(1) for _ in range(2)]
        rsA = [A.f32(1) for _ in range(2)]
        hb = [A.bf16(D) for _ in range(2)]
        hT = [A.bf16(D) for _ in range(2)]
        x2b = [A.f32(512) for _ in range(2)]
        xhb = [A.f32(512) for _ in range(2)]
        gu = [A.f32(512) for _ in range(2)]
        gv = [A.f32(512) for _ in range(2)]
        sq = A.f32(512)
        lst = [A.f32(16) for _ in range(2)]
        vn = A.f32(512)
        vnb = [A.bf16(512) for _ in range(2)]
        qb = [A.bf16(512) for _ in range(3)]
        kb = [A.bf16(512) for _ in range(3)]
        rt = [A.f32(64 * 4) for _ in range(2)]
        vb = [A.bf16(4 * 130) for _ in range(2)]
        yab = [A.bf16(512) for _ in range(2)]
        yaT = [A.bf16(512) for _ in range(2)]
        qT = [A.bf16(512) for _ in range(2)]
        kT = [A.bf16(512) for _ in range(2)]
        for s_ in range(2):
            dve(lambda e, s_=s_: e.memset(vb[s_], 1.0), w=[f"vb{s_}"])

        def stageA0(i):
            xs = i % NXS
            s2 = i % 2
            KA1 = int(os.environ.get("KA1", "9"))
            dma("sp", xt[xs], x_d[i * 128:(i + 1) * 128, :], w=[f"xt{xs}"], key=f"xt{xs}")
            if KA1 < 2: return
            act(lambda e: e.activation(junk, xt[xs], AF.Square, accum_out=ssA[s2]), r=[f"xt{xs}"], w=["junk", f"Ass{s2}"])
            if KA1 < 3: return
            dve(lambda e: e.tensor_scalar(vA[s2], ssA[s2], 1.0 / D, EPS, ALU.mult, ALU.add), r=[f"Ass{s2}"], w=[f"Av{s2}"])
            if KA1 < 4: return
            pool(lambda e: e.tensor_tensor(rsA[s2], vA[s2], nhalf[:, 0:1], ALU.pow), r=[f"Av{s2}", "nhalf"], w=[f"Ars{s2}"])
            if KA1 < 5: return
            dve(lambda e: e.tensor_scalar(hb[s2], xt[xs], rsA[s2], None, ALU.mult), r=[f"xt{xs}", f"Ars{s2}"], w=[f"hb{s2}"])
            if KA1 < 6: return

        def stageA1(i):
            s2 = i % 2
            KA1 = 9

            def tr(e):
                last = None
                for c in range(8):
                    last = e.transpose(bankbf(0)[:, c * 128:(c + 1) * 128], hb[s2][:, c * 128:(c + 1) * 128], ident)
                return last
            pe(tr, r=[f"hb{s2}", "ident"], w=["ps0"])
            if KA1 < 7: return
            act(lambda e: e.copy(hT[s2], bankbf(0)), r=["ps0"], w=[f"hT{s2}"])

        def zmm(i, cg, bk):
            s2 = i % 2
            hT3 = hT[s2].rearrange("p (c t) -> p c t", t=128)

            def mm(e):
                last = None
                for c in range(8):
                    last = e.matmul(bank(bk), hT3[:, c, :], win3[:, c, cg * 512:(cg + 1) * 512],
                                    start=(c == 0), stop=(c == 7))
                return last
            pe(mm, r=[f"hT{s2}"] + winres, w=[f"ps{bk}"])

        def gelu_chain(i, which, bk, outbuf, oname):
            x2 = x2b[which]
            xh = xhb[which]
            n2, nh = f"x2_{which}", f"xh_{which}"
            act(lambda e: e.activation(x2, bank(bk), AF.Square), r=[f"ps{bk}"], w=[n2])
            act(lambda e: e.activation(xh, bank(bk), AF.Copy, scale=0.5), r=[f"ps{bk}"], w=[nh])
            dve(lambda e: e.tensor_scalar(x2, x2, 0.044715, 1.0, ALU.mult, ALU.add), r=[n2], w=[n2])
            dve(lambda e: e.tensor_tensor(x2, x2, xh, ALU.mult), r=[n2, nh], w=[n2])
            act(lambda e: e.activation(x2, x2, AF.Tanh, scale=2.0 * 0.7978845608028654), r=[n2], w=[n2])
            dve(lambda e: e.scalar_tensor_tensor(outbuf, x2, 1.0, xh, ALU.add, ALU.mult), r=[n2, nh], w=[oname])

        def rope(i, bk, dst, dname, tmp):
            z3 = bank(bk).rearrange("p (s d) -> p s d", d=64)
            d3 = dst.rearrange("p (s d) -> p s d", d=64)
            cb = cos3[:, i, :].unsqueeze(1).broadcast_to([128, 8, 8])
            sb = sin3[:, i, :].unsqueeze(1).broadcast_to([128, 8, 8])
            t4 = tmp.rearrange("p (a s j) -> p a s j", a=4, j=8)
            tn = dname + "_rt"
            act(lambda e: e.copy(dst, bank(bk)), r=[f"ps{bk}"], w=[dname])
            dve(lambda e: e.tensor_tensor(t4[:, 0], z3[:, :, 0:8], cb, ALU.mult), r=[f"ps{bk}", "cos_t", dname], w=[tn + "0"])
            dve(lambda e: e.tensor_tensor(t4[:, 1], z3[:, :, 8:16], sb, ALU.mult), r=[f"ps{bk}", "sin_t"], w=[tn + "1"])
            dve(lambda e: e.tensor_tensor(t4[:, 2], z3[:, :, 8:16], cb, ALU.mult), r=[f"ps{bk}", "cos_t"], w=[tn + "2"])
            dve(lambda e: e.tensor_tensor(t4[:, 3], z3[:, :, 0:8], sb, ALU.mult), r=[f"ps{bk}", "sin_t"], w=[tn + "3"])
            dve(lambda e: e.tensor_tensor(d3[:, :, 0:8], t4[:, 0], t4[:, 1], ALU.subtract), r=[tn + "0", tn + "1"], w=[dname])
            dve(lambda e: e.tensor_tensor(d3[:, :, 8:16], t4[:, 2], t4[:, 3], ALU.add), r=[tn + "2", tn + "3"], w=[dname])

        def stageA2(i):
            s2 = i % 2
            zmm(i, 0, 1)
            gelu_chain(i, 0, 1, gu[s2], f"gu{s2}")
            zmm(i, 1, 2)
            gelu_chain(i, 1, 2, gv[s2], f"gv{s2}")
            s3 = i % 3
            zmm(i, 2, 3)
            rope(i, 3, qb[s3], f"qb{s3}", rt[0])
            zmm(i, 3, 1)
            rope(i, 1, kb[s3], f"kb{s3}", rt[1])
            zmm(i, 4, 2)
            vb3 = vb[s2].rearrange("p (h d) -> p h d", d=130)
            act(lambda e: e.copy(vb3[:, :, 0:128], bank(2).rearrange("p (h d) -> p h d", d=128)),
                r=["ps2"], w=[f"vb{s2}"])
            dma("sp", v_d[:, i, :, :], vb3, r=[f"vb{s2}"], w=[f"v_d_{S.uid()}"], key=f"vb{s2}")
            g_ = gv[s2]
            g3 = g_.rearrange("p (g d) -> p g d", d=128)
            L = lst[s2]
            ln = f"lst{s2}"
            dve(lambda e: e.reduce_sum(L[:, 0:4], g3, axis=AX.X), r=[f"gv{s2}"], w=[ln + "s"])
            dve(lambda e: e.tensor_tensor(sq, g_, g_, ALU.mult), r=[f"gv{s2}"], w=["sq"])
            dve(lambda e: e.reduce_sum(L[:, 4:8], sq.rearrange("p (g d) -> p g d", d=128), axis=AX.X), r=["sq"], w=[ln + "q"])
            dve(lambda e: e.tensor_scalar(L[:, 8:12], L[:, 0:4], 1.0 / 128, None, ALU.mult), r=[ln + "s"], w=[ln + "m"])
            dve(lambda e: e.tensor_tensor(L[:, 12:16], L[:, 8:12], L[:, 8:12], ALU.mult), r=[ln + "m"], w=[ln + "v"])
            dve(lambda e: e.scalar_tensor_tensor(L[:, 12:16], L[:, 4:8], 1.0 / 128, L[:, 12:16], ALU.mult, ALU.subtract),
                r=[ln + "q", ln + "v"], w=[ln + "v"])
            dve(lambda e: e.tensor_scalar(L[:, 12:16], L[:, 12:16], EPS, None, ALU.add), r=[ln + "v"], w=[ln + "v"])
            pool(lambda e: e.tensor_tensor(L[:, 4:8], L[:, 12:16], nhalf[:, 0:4], ALU.pow), r=[ln + "v", "nhalf", ln + "q"], w=[ln + "r"])
            for g in range(4):
                dve(lambda e, g=g: e.tensor_scalar(vn[:, g * 128:(g + 1) * 128], g_[:, g * 128:(g + 1) * 128],
                                                   L[:, 8 + g:9 + g], L[:, 4 + g:5 + g], ALU.subtract, ALU.mult),
                    r=[f"gv{s2}", ln + "m", ln + "r"], w=[f"vn{g}"])
            vnr = [f"vn{g}" for g in range(4)]
            dve(lambda e: e.tensor_tensor(vn, vn, lng, ALU.mult), r=vnr + ["lng"], w=vnr)
            dve(lambda e: e.tensor_tensor(vnb[s2], vn, lnb, ALU.add), r=vnr + ["lnb"], w=[f"vnb{s2}"])

        def stageA3(i):
            s2 = i % 2

            def sp_mm(e):
                last = None
                for g in range(4):
                    last = e.matmul(bank(4)[:, g * 128:(g + 1) * 128], wsT3[:, g, :], vnb[s2][:, g * 128:(g + 1) * 128],
                                    start=True, stop=True)
                return last
            KA3 = int(os.environ.get("KA3", "9"))
            pe(sp_mm, r=[f"vnb{s2}", "wsT"], w=["ps4"])
            if KA3 < 2: return
            for g in range(4):
                dve(lambda e, g=g: e.scalar_tensor_tensor(yab[s2][:, g * 128:(g + 1) * 128], bank(4)[:, g * 128:(g + 1) * 128],
                                                          bs[:, g:g + 1], gu[s2][:, g * 128:(g + 1) * 128], ALU.add, ALU.mult),
                    r=["ps4", "bs", f"gu{s2}"], w=[f"yab{s2}_{g}"])

        def stageA4(i):
            s2 = i % 2
            s3 = i % 3
            KA3 = 9
            yres = [f"yab{s2}_{g}" for g in range(4)]

            def tr(src, bk):
                def f(e):
                    last = None
                    for c in range(4):
                        last = e.transpose(bankbf(bk)[:, c * 128:(c + 1) * 128], src[:, c * 128:(c + 1) * 128], ident)
                    return last
                return f
            pe(tr(yab[s2], 5), r=yres + ["ident"], w=["ps5"])
            pe(tr(qb[s3], 6), r=[f"qb{s3}", "ident"], w=["ps6"])
            pe(tr(kb[s3], 7), r=[f"kb{s3}", "ident"], w=["ps7"])
            act(lambda e: e.copy(yaT[s2], bankbf(5)[:, 0:512]), r=["ps5"], w=[f"yaT{s2}"])
            dve(lambda e: e.tensor_copy(qT[s2], bankbf(6)[:, 0:512]), r=["ps6"], w=[f"qT{s2}"])
            act(lambda e: e.copy(kT[s2], bankbf(7)[:, 0:512]), r=["ps7"], w=[f"kT{s2}"])
            if KA3 < 5: return
            dma("sp", yaT_d[i], yaT[s2], r=[f"yaT{s2}"], w=[f"yaT_d_{S.uid()}"], key=f"yaT{s2}")
            dma("sp", qT_d[:, :, i * 128:(i + 1) * 128], qT[s2].rearrange("p (h t) -> p h t", t=128),
                r=[f"qT{s2}"], w=[f"qT_d_{S.uid()}"], key=f"qT{s2}")
            dma("sp", kT_d[:, :, i * 128:(i + 1) * 128], kT[s2].rearrange("p (h t) -> p h t", t=128),
                r=[f"kT{s2}"], w=[f"kT_d_{S.uid()}"], key=f"kT{s2}")

        stagesA = [stageA0, stageA1, stageA2, stageA3, stageA4]
        for s_ in range(nt_a + len(stagesA) - 1):
            lists = []
            for k_, fn in enumerate(stagesA):
                if 0 <= s_ - k_ < nt_a:
                    S.begin_record()
                    fn(s_ - k_)
                    lists.append(S.end_record())
            pos_ = [0] * len(lists)
            while True:
                best, bf = None, 2.0
                for li, L_ in enumerate(lists):
                    if pos_[li] < len(L_):
                        f_ = pos_[li] / len(L_)
                        if f_ < bf:
                            best, bf = li, f_
                if best is None:
                    break
                S.add(*lists[best][pos_[best]])
                pos_[best] += 1
        S.barrier()
        A.release(mark_phase)

        if stop_after == "A":
            return finish()

        KT_sb = A.bf16(4 * T)
        KT3 = KT_sb.rearrange("p (h t) -> p h t", t=T)
        V_sb = A.bf16(NT * 4 * 130)
        V4 = V_sb.rearrange("p (i h d) -> p i h d", h=4, d=130)
        for h in range(4):
            dma("sp", KT3[:, h, :], kT_d[:, h, :], r=["kT_d"], w=[f"KT{h}"], key=f"KTl{h}")
        for c in range(4):
            dma("sp", V4[:, c * 16:(c + 1) * 16], v_d[:, c * 16:(c + 1) * 16], r=["v_d"], w=[f"V{c}"], key=f"Vl{c}")
        subgc = A.f32(1)
        dma("sp", subgc, subgc_d, w=["subgc"], key="subgc")
        dve(lambda e: e.tensor_scalar(subgc, subgc, 1.0 - LAM_INIT, None, ALU.mult), r=["subgc"], w=["subgc"])
        onesf = A.f32(128)
        dve(lambda e: e.memset(onesf, 1.0), w=["onesf"])
        QTs = [A.bf16(4 * 512) for _ in range(2)]
        NPT = 4
        pTall = A.bf16(2 * NPT * 512)
        pT4 = pTall.rearrange("p (m s q) -> p m s q", m=2, s=NPT)
        pT = [[pT4[:, m_, s_, :] for s_ in range(NPT)] for m_ in range(2)]
        racc = [A.f32(512) for _ in range(2)]
        a0B = A.f32(512)
        a1B = A.f32(512)
        l1B = A.f32(512)
        rlb = [A.f32(512) for _ in range(2)]
        t1B = A.f32(512)
        t2B = A.f32(512)
        oB = A.f32(512)
        sqB = A.f32(512)
        v4B = A.f32(4)
        rs4B = A.f32(4)
        RmB = A.f32(512)
        ybT = [A.bf16(512) for _ in range(2)]
        n_st = nt_b // 4
        blocks = [(I_, h, j) for I_ in range(n_st) for h in range(4) for j in range(4 * I_ + 4)]

        def load_q(I_):
            sl = I_ % 2
            dma("sp", QTs[sl].rearrange("p (h t) -> p h t", t=512), qT_d[:, :, I_ * 512:(I_ + 1) * 512],
                r=["qT_d"], w=[f"QT{sl}"], key=f"QT{sl}")

        def emit_qk(n):
            I_, h, j = blocks[n]
            par = n % 2
            qlo = max(0, j - 4 * I_)
            ncol = 512 - qlo * 128
            Q3 = QTs[I_ % 2].rearrange("p (h t) -> p h t", t=512)
            diag = j >= 4 * I_

            def f(e):
                last = None
                for m in range(2):
                    last = e.matmul(bank(2 * m + par)[:, 0:ncol], KT3[m * 64:(m + 1) * 64, h, j * 128:(j + 1) * 128],
                                    Q3[m * 64:(m + 1) * 64, h, qlo * 128:512], start=True, stop=not diag)
                if diag:
                    for m in range(2):
                        last = e.matmul(bank(2 * m + par)[:, 0:128], ident, maskb, start=False, stop=True)
                return last
            pe(f, r=[f"KT{h}", f"QT{I_ % 2}", "ident", "maskb"], w=[f"ps{par}", f"ps{2 + par}"])
            sl_ = n % NPT
            act(lambda e: e.activation(pT4[:, :, sl_, 0:ncol], ps[:, par:par + 3:2, 0:ncol], AF.Exp, scale=0.125),
                r=[f"ps{par}", f"ps{2 + par}"], w=[f"pT0{sl_}", f"pT1{sl_}"])

        def emit_pv(n):
            I_, h, j = blocks[n]
            par = n % NPT
            qlo = max(0, j - 4 * I_)
            ncol = 512 - qlo * 128
            jlast = 4 * I_ + 3

            def f(e):
                last = None
                for m in range(2):
                    last = e.matmul(bank(4 + m)[:, qlo * 128:512], V4[:, j, h, 0:128], pT[m][par][:, 0:ncol],
                                    start=(j == 0), stop=(j == jlast))
                last = e.matmul(bank(6)[:, qlo * 128:512], onesm, pT[1][par][:, 0:ncol], start=(j == 0), stop=(j == jlast))
                return last
            pe(f, r=[f"pT0{par}", f"pT1{par}", f"V{j // 16}", "onesm"], w=["ps4", "ps5", "ps6"])
            rc = racc[(I_ * 4 + h) % 2]
            rn = f"racc{(I_ * 4 + h) % 2}"
            if j == 0:
                dve(lambda e: e.tensor_copy(rc, pT[0][par]), r=[f"pT0{par}"], w=[rn])
            else:
                dve(lambda e: e.tensor_tensor(rc[:, qlo * 128:512], rc[:, qlo * 128:512], pT[0][par][:, 0:ncol], ALU.add),
                    r=[f"pT0{par}", rn], w=[rn])
            if j == jlast:
                offs = [0, 1, 2, 4, 6, 9, 10, 12, 13] if I_ >= 3 else ([0, 1, 2, 3, 4, 5, 6, 7, 8] if I_ == 2 else [0] * 9)
                for k_, fn in enumerate(head_steps(I_, h)):
                    pending.append((n + offs[k_], fn))

        def head_steps(I_, h):
            sl = (I_ * 4 + h) % 2
            rc = racc[(I_ * 4 + h) % 2]
            rn = f"racc{(I_ * 4 + h) % 2}"

            def s0():
                dve(lambda e: e.tensor_copy(a1B, bank(5)), r=["ps5"], w=["a1B"])
                dve(lambda e: e.tensor_copy(a0B, bank(4)), r=["ps4"], w=["a0B"])
                dve(lambda e: e.tensor_copy(l1B, bank(6)), r=["ps6"], w=["l1B"])

            def s1():
                pe(lambda e: e.matmul(bank(7), onesf, rc, start=True, stop=True), r=["onesf", rn], w=["ps7"])

            def s2a():
                dve(lambda e: e.reciprocal(rlb[1], l1B), r=["l1B"], w=["rlb1"])

            def s2b():
                dve(lambda e: e.reciprocal(rlb[0], bank(7)), r=["ps7"], w=["rlb0"])

            def s2():
                dve(lambda e: e.tensor_tensor(t2B, a1B, rlb[1], ALU.mult), r=["a1B", "rlb1"], w=["t2B"])
                dve(lambda e: e.tensor_tensor(t1B, a0B, rlb[0], ALU.mult), r=["a0B", "rlb0"], w=["t1B"])
                dve(lambda e: e.scalar_tensor_tensor(oB, t2B, neglam, t1B, ALU.mult, ALU.add), r=["t1B", "t2B", "neglam"], w=["oB"])
                dve(lambda e: e.tensor_tensor(sqB, oB, oB, ALU.mult), r=["oB"], w=["sqB"])

            def s3():
                def ssq_mm(e):
                    last = None
                    for r_ in range(4):
                        last = e.matmul(bank(7)[:, r_:r_ + 1], sqB[:, r_ * 128:(r_ + 1) * 128], onesf[:, 0:1], start=True, stop=True)
                    return last
                pe(ssq_mm, r=["sqB", "onesf"], w=["ps7"])

            def s4():
                dve(lambda e: e.tensor_scalar(v4B, bank(7)[:, 0:4], 1.0 / 128, EPS, ALU.mult, ALU.add), r=["ps7"], w=["v4B"])
                pool(lambda e: e.tensor_tensor(rs4B, v4B, nhalf[:, 0:4], ALU.pow), r=["v4B", "nhalf"], w=["rs4B"])
                for r_ in range(4):
                    dve(lambda e, r_=r_: e.tensor_scalar(RmB[:, r_ * 128:(r_ + 1) * 128], identf, rs4B[:, r_:r_ + 1], None, ALU.mult),
                        r=["rs4B", "identf"], w=[f"RmB{r_}"])

            def s5():
                def bc_mm(e):
                    last = None
                    for r_ in range(4):
                        last = e.matmul(bank(7)[:, r_ * 128:(r_ + 1) * 128], onesf, RmB[:, r_ * 128:(r_ + 1) * 128], start=True, stop=True)
                    return last
                pe(bc_mm, r=[f"RmB{r_}" for r_ in range(4)] + ["onesf"], w=["ps7"])

            def s6():
                dve(lambda e: e.scalar_tensor_tensor(ybT[sl], oB, subgc, bank(7), ALU.mult, ALU.mult), r=["oB", "subgc", "ps7"], w=[f"ybT{sl}"])
                dma("sp", ybT_d[4 * I_:4 * I_ + 4, :, h * 128:(h + 1) * 128].rearrange("r p t -> p r t"),
                    ybT[sl].rearrange("p (r t) -> p r t", t=128), r=[f"ybT{sl}"], w=[f"ybT_d_{S.uid()}"], key=f"ybT{sl}")
            return [s0, s1, s2a, s2b, s2, s3, s4, s5, s6]

        def warmup(nmm, bk):
            def f(e):
                last = None
                for _ in range(nmm):
                    last = e.matmul(bank(bk), ident, KT3[:, 0, 0:512], start=True, stop=True)
                return last
            pe(f, r=["ident", "KT0"], w=[f"ps{bk}"])

        pending = []
        if n_st > 0:
            load_q(0)
        for n in range(len(blocks) + 16):
            if n < len(blocks):
                I_, h, j = blocks[n]
                if h == 0 and j == 0:
                    if I_ + 1 < n_st:
                        load_q(I_ + 1)
                    warmup(20, n % 2)
                emit_qk(n)
            if 1 <= n <= len(blocks):
                emit_pv(n - 1)
            due = [p for p in pending if p[0] <= n - 1]
            pending[:] = [p for p in pending if p[0] > n - 1]
            for _, fn in due:
                fn()
        assert not pending
        S.barrier()
        A.release(mark_phase)
        if stop_after == "B":
            return finish()

        wg = A.bf16(8 * 2048)
        wg3 = wg.rearrange("p (c n) -> p c n", n=2048)
        wa = A.bf16(4 * 1024)
        wa3 = wa.rearrange("p (c n) -> p c n", n=1024)
        wb = A.bf16(4 * 1024)
        wb3 = wb.rearrange("p (c n) -> p c n", n=1024)
        wo = A.bf16(8 * 1024)
        wo3 = wo.rearrange("p (c n) -> p c n", n=1024)
        wr = A.f32(8 * 36)
        wr3 = wr.rearrange("p (c n) -> p c n", n=36)
        gffn = A.f32(D)
        brt = A.f32(36)
        gmixC = A.f32(8)
        dma("sp", gmixC, gmix_d, w=["gmixC"], key="gmixC")
        dma("sp", gffn, gffn_d, w=["gffn"], key="gffn")
        dma("sp", brt, br_d, w=["brt"], key="brt")
        dma("sp", wr3, wr_d.rearrange("(c p) n -> p c n", p=128), w=["wr"], key="wr")
        mC = A.mark()
        stg = [A.f32(2048), A.f32(2048)]
        nld = [0]

        def wload(src, ncol, dst, scale_ap=None, scale_f=None, dname=None):
            sl = nld[0] % 2
            nld[0] += 1
            dma("sp", stg[sl][:, 0:ncol], src, w=[f"stg{sl}"], key=f"stg{sl}")
            if sl == 0:
                if scale_ap is not None:
                    dve(lambda e: e.tensor_scalar(dst, stg[sl][:, 0:ncol], scale_ap, None, ALU.mult), r=[f"stg{sl}", "gmixC"], w=[dname])
                elif scale_f is not None:
                    dve(lambda e: e.tensor_scalar(dst, stg[sl][:, 0:ncol], scale_f, None, ALU.mult), r=[f"stg{sl}"], w=[dname])
                else:
                    dve(lambda e: e.tensor_copy(dst, stg[sl][:, 0:ncol]), r=[f"stg{sl}"], w=[dname])
            else:
                sc = scale_ap if scale_ap is not None else (scale_f if scale_f is not None else 1.0)
                act(lambda e: e.activation(dst, stg[sl][:, 0:ncol], AF.Copy, scale=sc), r=[f"stg{sl}", "gmixC"], w=[dname])
        for c in range(8):
            wload(win_d[c * 128:(c + 1) * 128, NC_IN:4608], 2048, wg3[:, c, :], scale_ap=gmixC[:, c:c + 1], dname=f"wg{c}")
        for c in range(4):
            wload(wa_d[c * 128:(c + 1) * 128, :], 1024, wa3[:, c, :], dname=f"wa{c}")
            wload(wb_d[c * 128:(c + 1) * 128, :], 1024, wb3[:, c, :], dname=f"wb{c}")
        for c in range(8):
            wload(wo_d[c * 128:(c + 1) * 128, :], 1024, wo3[:, c, :], scale_f=0.5, dname=f"wo{c}")
        S.barrier()
        A.release(mC)
        wgres = [f"wg{c}" for c in range(8)]

        xtC = [A.f32(D) for _ in range(5)]
        junkC = A.bf16(D)
        ssC = [A.f32(1) for _ in range(2)]
        vC = [A.f32(1) for _ in range(2)]
        rsC = [A.f32(1) for _ in range(2)]
        hbC = [A.bf16(D) for _ in range(2)]
        hTC = [A.bf16(D) for _ in range(2)]
        yaL = [A.bf16(512) for _ in range(3)]
        ybL = [A.bf16(512) for _ in range(3)]
        th = A.f32(2048)
        m1 = A.f32(D)
        m2 = A.f32(D)
        mbs = [A.bf16(D) for _ in range(2)]
        mTs = [A.bf16(D) for _ in range(2)]
        x1t = [A.f32(D) for _ in range(2)]
        ss2 = A.f32(1)
        v2 = A.f32(1)
        rs2 = A.f32(1)
        h2fs = [A.f32(D) for _ in range(2)]
        h2b = [A.bf16(D) for _ in range(4)]
        h2Ts = [A.f32(D) for _ in range(2)]
        Lg = A.f32(36)
        sm = A.f32(16)
        goh = A.f32(4)
        gex = A.f32(4)
        pen = A.f32(4)
        elm = A.f32(32)
        top8 = A.f32(8)
        oh1s = [A.f32(32) for _ in range(2)]
        oh2s = [A.f32(32) for _ in range(2)]
        Mbs = [A.bf16(32) for _ in range(2)]
        posC = A.f32(32)
        tmp32 = A.f32(32)

        def stageC0(i):
            xs = i % 5
            s2 = i % 2
            s3 = i % 3
            dma("sp", xtC[xs], x_d[i * 128:(i + 1) * 128, :], w=[f"xtC{xs}"], key=f"xtC{xs}")
            dma("sp", yaL[s3], yaT_d[i], r=["yaT_d"], w=[f"yaL{s3}"], key=f"yaL{s3}")
            dma("sp", ybL[s3], ybT_d[i], r=["ybT_d"], w=[f"ybL{s3}"], key=f"ybL{s3}")
            act(lambda e: e.activation(junkC, xtC[xs], AF.Square, accum_out=ssC[s2]), r=[f"xtC{xs}"], w=["junkC", f"Css{s2}"])
            dve(lambda e: e.tensor_scalar(vC[s2], ssC[s2], 1.0 / D, EPS, ALU.mult, ALU.add), r=[f"Css{s2}"], w=[f"Cv{s2}"])
            pool(lambda e: e.tensor_tensor(rsC[s2], vC[s2], nhalf[:, 0:1], ALU.pow), r=[f"Cv{s2}", "nhalf"], w=[f"Crs{s2}"])
            dve(lambda e: e.tensor_scalar(hbC[s2], xtC[xs], rsC[s2], None, ALU.mult), r=[f"xtC{xs}", f"Crs{s2}"], w=[f"hbC{s2}"])

        def stageC1(i):
            s2 = i % 2

            def tr(e):
                last = None
                for c in range(8):
                    last = e.transpose(bankbf(0)[:, c * 128:(c + 1) * 128], hbC[s2][:, c * 128:(c + 1) * 128], ident)
                return last
            pe(tr, r=[f"hbC{s2}", "ident"], w=["ps0"])
            act(lambda e: e.copy(hTC[s2], bankbf(0)), r=["ps0"], w=[f"hTC{s2}"])

        def stageC2(i):
            s2 = i % 2
            s3 = i % 3
            hT3 = hTC[s2].rearrange("p (c t) -> p c t", t=128)
            for cg in range(4):
                bk = 1 + cg % 2

                def mm(e, cg=cg, bk=bk):
                    last = None
                    for c in range(8):
                        last = e.matmul(bank(bk), hT3[:, c, :], wg3[:, c, cg * 512:(cg + 1) * 512], start=(c == 0), stop=(c == 7))
                    return last
                pe(mm, r=[f"hTC{s2}"] + wgres, w=[f"ps{bk}"])
                act(lambda e, cg=cg, bk=bk: e.activation(th[:, cg * 512:(cg + 1) * 512], bank(bk), AF.Tanh, scale=0.5),
                    r=[f"ps{bk}"], w=[f"th{cg}"])
            yl3 = yaL[s3].rearrange("p (c t) -> p c t", t=128)
            bl3 = ybL[s3].rearrange("p (c t) -> p c t", t=128)
            for half in range(2):
                def mma(e, half=half):
                    last = None
                    for c in range(4):
                        last = e.matmul(bank(3 + half), yl3[:, c, :], wa3[:, c, half * 512:(half + 1) * 512], start=(c == 0), stop=(c == 3))
                    return last
                pe(mma, r=[f"yaL{s3}"] + [f"wa{c}" for c in range(4)], w=[f"ps{3 + half}"])

                def mmb(e, half=half):
                    last = None
                    for c in range(4):
                        last = e.matmul(bank(5 + half), bl3[:, c, :], wb3[:, c, half * 512:(half + 1) * 512], start=(c == 0), stop=(c == 3))
                    return last
                pe(mmb, r=[f"ybL{s3}"] + [f"wb{c}" for c in range(4)], w=[f"ps{5 + half}"])
            for half in range(2):
                dve(lambda e, half=half: e.scalar_tensor_tensor(m1[:, half * 512:(half + 1) * 512], th[:, half * 512:(half + 1) * 512], 1.0,
                                                                bank(3 + half), ALU.add, ALU.mult),
                    r=[f"th{half}", f"ps{3 + half}"], w=[f"m1{half}"])
                dve(lambda e, half=half: e.scalar_tensor_tensor(m2[:, half * 512:(half + 1) * 512], th[:, 1024 + half * 512:1024 + (half + 1) * 512], 1.0,
                                                                bank(5 + half), ALU.add, ALU.mult),
                    r=[f"th{2 + half}", f"ps{5 + half}"], w=[f"m2{half}"])
            dve(lambda e: e.tensor_tensor(mbs[s2], m1, m2, ALU.add), r=["m10", "m11", "m20", "m21"], w=[f"mb{s2}"])

        def stageC3(i):
            s2 = i % 2

            def trm(e):
                last = None
                for c in range(8):
                    last = e.transpose(bankbf(0)[:, c * 128:(c + 1) * 128], mbs[s2][:, c * 128:(c + 1) * 128], ident)
                return last
            pe(trm, r=[f"mb{s2}", "ident"], w=["ps0"])
            act(lambda e: e.copy(mTs[s2], bankbf(0)), r=["ps0"], w=[f"mT{s2}"])

        def stageC4o(i):
            xs = i % 5
            s2 = i % 2
            s4 = i % 4
            mT3 = mTs[s2].rearrange("p (c t) -> p c t", t=128)
            for half in range(2):
                def mmo(e, half=half):
                    last = None
                    for c in range(8):
                        last = e.matmul(bank(1 + half), mT3[:, c, :], wo3[:, c, half * 512:(half + 1) * 512], start=(c == 0), stop=(c == 7))
                    return last
                pe(mmo, r=[f"mT{s2}"] + [f"wo{c}" for c in range(8)], w=[f"ps{1 + half}"])
                dve(lambda e, half=half: e.tensor_tensor(x1t[s2][:, half * 512:(half + 1) * 512], xtC[xs][:, half * 512:(half + 1) * 512],
                                                         bank(1 + half), ALU.add),
                    r=[f"xtC{xs}", f"ps{1 + half}"], w=[f"x1t{s2}_{half}"])
            x1res = [f"x1t{s2}_0", f"x1t{s2}_1"]
            dma("sp", x1_d[i * 128:(i + 1) * 128, :], x1t[s2], r=x1res, w=[f"x1_d_{S.uid()}"], key=f"x1t{s2}")
            act(lambda e: e.activation(junkC, x1t[s2], AF.Square, accum_out=ss2), r=x1res, w=["junkC", "ss2"])
            dve(lambda e: e.tensor_scalar(v2, ss2, 1.0 / D, EPS, ALU.mult, ALU.add), r=["ss2"], w=["v2"])
            pool(lambda e: e.tensor_tensor(rs2, v2, nhalf[:, 0:1], ALU.pow), r=["v2", "nhalf"], w=["rs2"])
            dve(lambda e: e.scalar_tensor_tensor(h2fs[s2], x1t[s2], rs2, gffn, ALU.mult, ALU.mult), r=x1res + ["rs2", "gffn"], w=[f"h2f{s2}"])
            act(lambda e: e.copy(h2b[s4], h2fs[s2]), r=[f"h2f{s2}"], w=[f"h2b{s4}"])

        def stageC5(i):
            s2 = i % 2
            h2f = h2fs[s2]
            h2T = h2Ts[s2]

            def trr(e):
                last = None
                for c in range(8):
                    last = e.transpose(ps[:, 3 + c // 4, (c % 4) * 128:(c % 4 + 1) * 128], h2f[:, c * 128:(c + 1) * 128], identf)
                return last
            pe(trr, r=[f"h2f{s2}", "identf"], w=["ps3", "ps4"])
            act(lambda e: e.copy(h2T[:, 0:512], bank(3)), r=["ps3"], w=[f"h2Ta{s2}"])
            act(lambda e: e.copy(h2T[:, 512:1024], bank(4)), r=["ps4"], w=[f"h2Tb{s2}"])

        def stageC6(i):
            s2 = i % 2
            oh1, oh2, Mb = oh1s[s2], oh2s[s2], Mbs[s2]
            h2T3 = h2Ts[s2].rearrange("p (c t) -> p c t", t=128)

            def mmr(e):
                last = None
                for c in range(8):
                    last = e.matmul(bank(7)[:, 0:36], h2T3[:, c, :], wr3[:, c, :], start=(c == 0), stop=(c == 7))
                return last
            pe(mmr, r=[f"h2Ta{s2}", f"h2Tb{s2}", "wr"], w=["ps7"])
            dve(lambda e: e.tensor_tensor(Lg, bank(7)[:, 0:36], brt, ALU.add), r=["ps7", "brt"], w=["Lg"])
            dve(lambda e: e.reduce_max(sm[:, 0:1], Lg[:, 0:4], axis=AX.X), r=["Lg"], w=["gmax"])
            dve(lambda e: e.tensor_scalar(goh, Lg[:, 0:4], sm[:, 0:1], None, ALU.is_equal), r=["Lg", "gmax"], w=["goh"])
            dve(lambda e: e.tensor_scalar(sm[:, 1:2], sm[:, 0:1], -1.0, None, ALU.mult), r=["gmax"], w=["negg"])
            act(lambda e: e.activation(gex, Lg[:, 0:4], AF.Exp, bias=sm[:, 1:2], accum_out=sm[:, 2:3]), r=["Lg", "negg"], w=["gex", "gsum"])
            dve(lambda e: e.reciprocal(sm[:, 3:4], sm[:, 2:3]), r=["gsum"], w=["gw"])
            dve(lambda e: e.tensor_scalar(pen, goh, -1.0, 1e30, ALU.add, ALU.mult), r=["goh"], w=["pen"])
            dve(lambda e: e.tensor_tensor(elm.rearrange("p (g e) -> p g e", e=8), Lg[:, 4:36].rearrange("p (g e) -> p g e", e=8),
                                          pen.unsqueeze(2).broadcast_to([128, 4, 8]), ALU.add), r=["Lg", "pen"], w=["elm"])
            dve(lambda e: e.max(top8, elm), r=["elm"], w=["top8"])
            dve(lambda e: e.tensor_scalar(oh1, elm, top8[:, 0:1], None, ALU.is_equal), r=["elm", "top8"], w=[f"oh1_{s2}"])
            dve(lambda e: e.tensor_scalar(oh2, elm, top8[:, 1:2], None, ALU.is_equal), r=["elm", "top8"], w=[f"oh2_{s2}"])
            dve(lambda e: e.tensor_scalar(sm[:, 4:5], top8[:, 0:1], -1.0, None, ALU.mult), r=["top8"], w=["negv1"])
            act(lambda e: e.activation(sm[:, 5:6], top8[:, 1:2], AF.Exp, bias=sm[:, 4:5]), r=["top8", "negv1"], w=["e2"])
            dve(lambda e: e.tensor_scalar(sm[:, 6:7], sm[:, 5:6], 1.0, None, ALU.add), r=["e2"], w=["den"])
            dve(lambda e: e.reciprocal(sm[:, 7:8], sm[:, 6:7]), r=["den"], w=["p1"])
            dve(lambda e: e.tensor_tensor(wts3[:, i, 0:1], sm[:, 7:8], sm[:, 3:4], ALU.mult), r=["p1", "gw"], w=[f"w1_{i}"])
            dve(lambda e: e.tensor_tensor(wts3[:, i, 1:2], wts3[:, i, 0:1], sm[:, 5:6], ALU.mult), r=[f"w1_{i}", "e2"], w=[f"w2_{i}"])
            dve(lambda e: e.tensor_tensor(Mb, oh1, oh2, ALU.add), r=[f"oh1_{s2}", f"oh2_{s2}"], w=[f"Mb_{s2}"])

        def stageC7(i):
            s2 = i % 2
            s4 = i % 4
            oh1, oh2, Mb = oh1s[s2], oh2s[s2], Mbs[s2]
            pe(lambda e: e.matmul(bank(5)[:, 0:32], ustrict, Mb, start=True, stop=True), r=["ustrict", f"Mb_{s2}"], w=["ps5"])
            pe(lambda e: e.matmul(bank(6)[:, 0:32], onesm, Mb, start=True, stop=True), r=["onesm", f"Mb_{s2}"], w=["ps6"])
            dve(lambda e: e.tensor_tensor(posC, bank(5)[:, 0:32], base_cnt, ALU.add), r=["ps5", "base_cnt"], w=["posCr"])
            dve(lambda e: e.tensor_scalar(posC, posC, float(CAP - 1), None, ALU.min), r=["posCr"], w=["posCr"])
            dve(lambda e: e.tensor_tensor(posC, posC, eoff, ALU.add), r=["posCr", "eoff"], w=["posCr"])
            dve(lambda e: e.tensor_tensor(tmp32, posC, oh1, ALU.mult), r=["posCr", f"oh1_{s2}"], w=["tmp32"])
            dve(lambda e: e.reduce_sum(sm[:, 8:9], tmp32, axis=AX.X), r=["tmp32"], w=["s1f"])
            dve(lambda e: e.tensor_tensor(tmp32, posC, oh2, ALU.mult), r=["posCr", f"oh2_{s2}", "tmp32"], w=["tmp32"])
            dve(lambda e: e.reduce_sum(sm[:, 9:10], tmp32, axis=AX.X), r=["tmp32"], w=["s2f"])
            dve(lambda e: e.tensor_copy(slots3[:, i, 0:1], sm[:, 8:9]), r=["s1f"], w=[f"sl1_{i}"])
            dve(lambda e: e.tensor_copy(slots3[:, i, 1:2], sm[:, 9:10]), r=["s2f"], w=[f"sl2_{i}"])
            dve(lambda e: e.tensor_tensor(base_cnt, base_cnt, bank(6)[:, 0:32], ALU.add), r=["ps6", "base_cnt"], w=["base_cnt"])
            for k in range(2):
                S.add("pool", lambda e, k=k: e.indirect_dma_start(
                    out=xs_d, out_offset=bass.IndirectOffsetOnAxis(ap=slots3[:, i, k:k + 1], axis=0),
                    in_=h2b[s4], in_offset=None),
                    reads=[f"sl{k + 1}_{i}", f"h2b{s4}"] + [f"xs_zero_{z}" for z in range(4)], writes=[f"xs_d{k}"], dma=f"scat{k}_{s4}")
            if debug:
                dma("sp", rt_d[:, i, 0:2], wts3[:, i, :], r=[f"w1_{i}", f"w2_{i}"], key="dbgrt")

        stagesC = [stageC0, stageC1, stageC2, stageC3, stageC4o, stageC5, stageC6, stageC7]
        groupsC = [[0, 1, 2, 6], [3, 4, 5, 7]]

        def merge_emit(lists):
            pos_ = [0] * len(lists)
            while True:
                best, bf = None, 2.0
                for li, L_ in enumerate(lists):
                    if pos_[li] < len(L_):
                        f_ = pos_[li] / len(L_)
                        if f_ < bf:
                            best, bf = li, f_
                if best is None:
                    break
                S.add(*lists[best][pos_[best]])
                pos_[best] += 1

        for s_ in range(nt_c + len(stagesC) - 1):
            for grp in groupsC:
                lists = []
                for k_ in grp:
                    if 0 <= s_ - k_ < nt_c:
                        S.begin_record()
                        stagesC[k_](s_ - k_)
                        lists.append(S.end_record())
                merge_emit(lists)
        S.barrier()
        A.release(mark_phase)
        if stop_after == "C":
            return finish()

        w1s = A.f32(8 * 512)
        w3s = A.f32(8 * 512)
        w2s = A.f32(4 * 1024)
        w1b = [A.bf16(8 * 512) for _ in range(2)]
        w3b = [A.bf16(8 * 512) for _ in range(2)]
        w2b = [A.bf16(4 * 1024) for _ in range(2)]
        xg = [A.bf16(D) for _ in range(3)]
        XT = [A.bf16(8 * CAP) for _ in range(2)]
        AT = [A.bf16(4 * CAP) for _ in range(2)]
        thD = [A.f32(CAP // 2) for _ in range(2)]
        a1D = [A.f32(CAP // 2) for _ in range(2)]
        ysb = [A.bf16(D) for _ in range(3)]
        HC = CAP // 2

        def load_w(e_):
            for c in range(8):
                dma("sp", w1s[:, c * 512:(c + 1) * 512], w1_d[e_, c * 128:(c + 1) * 128, :], w=[f"w1s{c}"], key="w1s")
                dma("sp", w3s[:, c * 512:(c + 1) * 512], w3_d[e_, c * 128:(c + 1) * 128, :], w=[f"w3s{c}"], key="w3s")
                dma("sp", w2s[:, c * 512:(c + 1) * 512], w2_d[e_, (c // 2) * 128:(c // 2 + 1) * 128, (c % 2) * 512:(c % 2 + 1) * 512],
                    w=[f"w2s{c}"], key="w2s")

        def cast_w_chunk(e_, c):
            sl = e_ % 2
            act(lambda e: e.copy(w1b[sl][:, c * 512:(c + 1) * 512], w1s[:, c * 512:(c + 1) * 512]), r=[f"w1s{cc}" for cc in range(8)], w=[f"w1b{sl}_{c}"])
            dve(lambda e: e.tensor_copy(w3b[sl][:, c * 512:(c + 1) * 512], w3s[:, c * 512:(c + 1) * 512]), r=[f"w3s{cc}" for cc in range(8)], w=[f"w3b{sl}_{c}"])
            dve(lambda e: e.tensor_scalar(w2b[sl][:, c * 512:(c + 1) * 512], w2s[:, c * 512:(c + 1) * 512], 0.5, None, ALU.mult),
                r=[f"w2s{cc}" for cc in range(8)], w=[f"w2b{sl}_{c}"])

        def cast_w(e_):
            for c in range(8):
                cast_w_chunk(e_, c)

        nblk_ct = [0]

        def xblock(e_, b):
            sl = e_ % 2
            XT3 = XT[sl].rearrange("p (c t) -> p c t", t=CAP)
            g = nblk_ct[0] % 3
            nblk_ct[0] += 1
            blk = e_ * NBLK + b
            tb = 0 if b % 2 == 0 else 7
            dma("sp", xg[g], xs_d[blk * 128:(blk + 1) * 128, :], w=[f"xg{g}"], key=f"xg{g}")

            def trx(e):
                last = None
                for c in range(8):
                    last = e.transpose(bankbf(tb)[:, c * 128:(c + 1) * 128], xg[g][:, c * 128:(c + 1) * 128], ident)
                return last
            pe(trx, r=[f"xg{g}", "ident"], w=[f"ps{tb}"])
            if b % 2 == 0:
                act(lambda e: e.copy(XT3[:, :, b * 128:(b + 1) * 128], bankbf(tb).rearrange("p (c t) -> p c t", t=128)),
                    r=[f"ps{tb}"], w=[f"XT{sl}_{b}"])
            else:
                dve(lambda e: e.tensor_copy(XT3[:, :, b * 128:(b + 1) * 128], bankbf(tb).rearrange("p (c t) -> p c t", t=128)),
                    r=[f"ps{tb}"], w=[f"XT{sl}_{b}"])

        def gate_up_step(e_, k):
            sl = e_ % 2
            XT3 = XT[sl].rearrange("p (c t) -> p c t", t=CAP)
            AT3 = AT[sl].rearrange("p (c t) -> p c t", t=CAP)
            w1b3 = w1b[sl].rearrange("p (c n) -> p c n", n=512)
            w3b3 = w3b[sl].rearrange("p (c n) -> p c n", n=512)
            xtres = [f"XT{sl}_{b}" for b in range(NBLK)]
            dc, half = k // 2, k % 2
            bG = 1 + k % 2
            bU = 3 + k % 2
            t2 = k % 2

            def mmg(e):
                last = None
                for c in range(8):
                    last = e.matmul(bank(bG)[:, 0:HC], w1b3[:, c, dc * 128:(dc + 1) * 128], XT3[:, c, half * HC:(half + 1) * HC],
                                    start=(c == 0), stop=(c == 7))
                return last
            pe(mmg, r=xtres + [f"w1b{sl}_{c}" for c in range(8)], w=[f"ps{bG}"])

            def mmu(e):
                last = None
                for c in range(8):
                    last = e.matmul(bank(bU)[:, 0:HC], w3b3[:, c, dc * 128:(dc + 1) * 128], XT3[:, c, half * HC:(half + 1) * HC],
                                    start=(c == 0), stop=(c == 7))
                return last
            pe(mmu, r=xtres + [f"w3b{sl}_{c}" for c in range(8)], w=[f"ps{bU}"])
            act(lambda e: e.activation(thD[t2], bank(bG)[:, 0:HC], AF.Tanh, scale=0.5), r=[f"ps{bG}"], w=[f"thD{t2}"])
            dve(lambda e: e.scalar_tensor_tensor(a1D[t2], thD[t2], 1.0, bank(bG)[:, 0:HC], ALU.add, ALU.mult),
                r=[f"thD{t2}", f"ps{bG}"], w=[f"a1D{t2}"])
            dve(lambda e: e.tensor_tensor(AT3[:, dc, half * HC:(half + 1) * HC], a1D[t2], bank(bU)[:, 0:HC], ALU.mult),
                r=[f"a1D{t2}", f"ps{bU}"], w=[f"AT{sl}_{k}"])

        def down(e_):
            sl = e_ % 2
            AT3 = AT[sl].rearrange("p (c t) -> p c t", t=CAP)
            w2b3 = w2b[sl].rearrange("p (c n) -> p c n", n=1024)
            atres = [f"AT{sl}_{k}" for k in range(8)]
            for b in range(NBLK):
                blk = e_ * NBLK + b
                ysl = blk % 3
                for cg in range(2):
                    def mmy(e, b=b, cg=cg):
                        last = None
                        for dc in range(4):
                            last = e.matmul(bank(5 + cg), AT3[:, dc, b * 128:(b + 1) * 128], w2b3[:, dc, cg * 512:(cg + 1) * 512],
                                            start=(dc == 0), stop=(dc == 3))
                        return last
                    pe(mmy, r=atres + [f"w2b{sl}_{c}" for c in range(8)], w=[f"ps{5 + cg}"])
                    if cg == 0:
                        act(lambda e, ysl=ysl: e.copy(ysb[ysl][:, 0:512], bank(5)), r=["ps5"], w=[f"ysb{ysl}_0"])
                    else:
                        dve(lambda e, ysl=ysl: e.tensor_copy(ysb[ysl][:, 512:1024], bank(6)), r=["ps6"], w=[f"ysb{ysl}_1"])
                dma("pool", ys_d[blk * 128:(blk + 1) * 128, :], ysb[ysl], r=[f"ysb{ysl}_0", f"ysb{ysl}_1"], w=[f"ys_d_{S.uid()}"], key=f"ysb{ysl}")

        assert NBLK == 8
        if n_exp > 0:
            load_w(0)
            cast_w(0)
            if n_exp > 1:
                load_w(1)
            for b in range(NBLK):
                xblock(0, b)
        for e_ in range(n_exp):
            for k in range(8):
                gate_up_step(e_, k)
                if e_ + 1 < n_exp:
                    xblock(e_ + 1, k)
                    cast_w_chunk(e_ + 1, k)
            if e_ + 2 < n_exp:
                load_w(e_ + 2)
            down(e_)
        S.barrier()
        A.release(mark_phase)
        if stop_after == "D":
            return finish()

        fg = A.f32(D)
        dma("sp", fg, fg_d, w=["fg"], key="fg")
        x1L = [A.f32(D) for _ in range(3)]
        y1L = [A.bf16(D) for _ in range(3)]
        y2L = [A.bf16(D) for _ in range(3)]
        tE = [A.f32(D) for _ in range(2)]
        x2E = [A.f32(D) for _ in range(2)]
        junkE = A.bf16(D)
        ssE = [A.f32(1) for _ in range(2)]
        vE = [A.f32(1) for _ in range(2)]
        rsE = [A.f32(1) for _ in range(2)]
        oE = [A.f32(D) for _ in range(2)]

        def stageE1(i):
            s3 = i % 3
            dma("sp", x1L[s3], x1_d[i * 128:(i + 1) * 128, :], r=["x1_d"], w=[f"x1L{s3}"], key=f"x1L{s3}")
            for k, yL in ((0, y1L), (1, y2L)):
                S.add("pool", lambda e, k=k, yL=yL: e.indirect_dma_start(
                    out=yL[s3], out_offset=None, in_=ys_d,
                    in_offset=bass.IndirectOffsetOnAxis(ap=slots3[:, i, k:k + 1], axis=0)),
                    reads=["ys_d", "slots"], writes=[f"y{k}L{s3}"], dma=f"y{k}L{s3}")

        def stageE2(i):
            s3 = i % 3
            s2 = i % 2
            dve(lambda e: e.scalar_tensor_tensor(tE[s2], y1L[s3], wts3[:, i, 0:1], x1L[s3], ALU.mult, ALU.add),
                r=[f"y0L{s3}", f"x1L{s3}", "wts"], w=[f"tE{s2}"])
            dve(lambda e: e.scalar_tensor_tensor(x2E[s2], y2L[s3], wts3[:, i, 1:2], tE[s2], ALU.mult, ALU.add),
                r=[f"y1L{s3}", f"tE{s2}", "wts"], w=[f"x2E{s2}"])
            act(lambda e: e.activation(junkE, x2E[s2], AF.Square, accum_out=ssE[s2]), r=[f"x2E{s2}"], w=["junkE", f"ssE{s2}"])
            dve(lambda e: e.tensor_scalar(vE[s2], ssE[s2], 1.0 / D, EPS, ALU.mult, ALU.add), r=[f"ssE{s2}"], w=[f"vE{s2}"])
            pool(lambda e: e.tensor_tensor(rsE[s2], vE[s2], nhalf[:, 0:1], ALU.pow), r=[f"vE{s2}", "nhalf"], w=[f"rsE{s2}"])
            dve(lambda e: e.scalar_tensor_tensor(oE[s2], x2E[s2], rsE[s2], fg, ALU.mult, ALU.mult), r=[f"x2E{s2}", f"rsE{s2}", "fg"], w=[f"oE{s2}"])
            dma("act", out_d[i * 128:(i + 1) * 128, :], oE[s2], r=[f"oE{s2}"], w=[f"out_d_{S.uid()}"], key=f"oE{s2}")

        for s_ in range(NT + 1):
            if s_ < NT:
                stageE1(s_)
            if s_ >= 1:
                stageE2(s_ - 1)
        return finish()


def _bc(v, n=128):
    v = np.asarray(v, dtype=np.float32).reshape(1, -1)
    return np.ascontiguousarray(np.broadcast_to(v, (n, v.shape[1])))


def core_inputs(I, b):
    f = np.float32
    invf = (np.float32(500000.0) ** (-np.arange(8, dtype=np.float32) / np.float32(8))).astype(f)
    return {
        "x": np.ascontiguousarray(I["x"][b]),
        "pos": np.ascontiguousarray(I["positions"][b].reshape(NT, 128).T.astype(np.int32)),
        "invf": _bc(invf),
        "g_mix": np.ascontiguousarray(I["norm_mix_g"][0].reshape(8, 128).T),
        "w_in": I["w_in"][0],
        "lng_bc": _bc(I["gm_ln_g"][0].reshape(-1)),
        "lnb_bc": _bc(I["gm_ln_b"][0].reshape(-1)),
        "wsT": np.ascontiguousarray(I["gm_w_s"][0].transpose(2, 0, 1)),
        "bs": np.ascontiguousarray(I["gm_b_s"][0].T),
        "lamv": np.ascontiguousarray(np.broadcast_to(
            np.stack([I["lam_q1"][0], I["lam_k1"][0], I["lam_q2"][0], I["lam_k2"][0]])[None], (128, 4, 64))).astype(f),
        "subg_col": np.ascontiguousarray(I["da_subln_g"][0].reshape(128, 1).astype(np.float32)),
        "w_br_a": I["w_br_a"][0],
        "w_br_b": I["w_br_b"][0],
        "w_out": I["w_out"][0],
        "g_ffn_bc": _bc(I["norm_ffn_g"][0]),
        "w_r": np.ascontiguousarray(np.concatenate([I["w_router_group"][0], I["w_router_expert"][0]], axis=1)),
        "b_r": _bc(np.concatenate([I["b_router_group"][0], I["b_router_expert"][0]])),
        "w1": I["w_exp_gate"][0],
        "w3": I["w_exp_up"][0],
        "w2": I["w_exp_down"][0],
        "fg_bc": _bc(I["final_norm_g"]),
    }


_CACHE = {}


def kernel(**inputs):
    I = {k: np.asarray(v) for k, v in inputs.items()}
    if "nc" not in _CACHE:
        _CACHE["nc"] = build_program()
    nc = _CACHE["nc"]
    in_maps = [core_inputs(I, b) for b in range(8)]
    res = run_bass_kernel_spmd(nc, in_maps, core_ids=list(range(8)))
    return np.stack([np.asarray(r["out"], dtype=np.float32) for r in res.results], axis=0)
```
